# Optimizing a Trainium2 kernel written in Bass

```python
import math
import jax, jax.numpy as jnp
from jax import lax
import numpy as np

D_MODEL = 1024
BATCH = 8
SEQ = 4096
DEPTH = 1

GRID_W = 64
CTX_LEN = 256
D_MIX = D_MODEL
D_S5 = D_MIX // 2
S5_GROUP = 16
S5_GROUPS = D_S5 // S5_GROUP
S5_STATE = 64
D_FOURIER = D_MIX - D_S5
FOURIER_GROUPS = 4
FOURIER_GROUP = D_FOURIER // FOURIER_GROUPS
N_EXPERT_GROUPS = 4
EXPERTS_PER_GROUP = 8
N_EXPERTS = N_EXPERT_GROUPS * EXPERTS_PER_GROUP
TOP_K_INNER = 2
D_EXPERT = D_MODEL // 4
EPS = 1e-6
DT_MIN = 1e-3
DT_MAX = 1e-1

kernel_name = "hymba_s5_fnet_hmoe_prefix_block"


def rms_norm(x, g):
    xf = x.astype(jnp.float32)
    y = xf * lax.rsqrt(jnp.mean(xf * xf, axis=-1, keepdims=True) + EPS)
    return (y * g.astype(jnp.float32)).astype(x.dtype)


def adaln(cond, w, b):
    m = jax.nn.silu(cond) @ w + b
    return [p[:, None, :] for p in jnp.split(m, 6, axis=-1)]


def modulate(h, shift, scale):
    return h * (1.0 + scale) + shift


def to_lbgh(u):
    b, l, _ = u.shape
    return u.reshape(b, l, S5_GROUPS, S5_GROUP).transpose(1, 0, 2, 3)


def s5_discretize(lam_re, lam_im, b_re, b_im, log_step):
    f32 = jnp.float32
    dt = jnp.exp(log_step.astype(f32))[:, None]
    lr, li = lam_re.astype(f32), lam_im.astype(f32)
    mag = jnp.exp(lr * dt)
    abar_re, abar_im = mag * jnp.cos(li * dt), mag * jnp.sin(li * dt)
    den = lr * lr + li * li
    nr, ni = abar_re - 1.0, abar_im
    q_re = (nr * lr + ni * li) / den
    q_im = (ni * lr - nr * li) / den
    br, bi = b_re.astype(f32), b_im.astype(f32)
    bbar_re = q_re[..., None] * br - q_im[..., None] * bi
    bbar_im = q_re[..., None] * bi + q_im[..., None] * br
    return abar_re, abar_im, bbar_re, bbar_im


def _linear_recurrence_combine(e1, e2):
    a1r, a1i, b1r, b1i = e1
    a2r, a2i, b2r, b2i = e2
    return (a2r * a1r - a2i * a1i,
            a2r * a1i + a2i * a1r,
            a2r * b1r - a2i * b1i + b2r,
            a2r * b1i + a2i * b1r + b2i)


def s5_scan(u, disc, h0_re, h0_im, reverse):
    abar_re, abar_im, bbar_re, bbar_im = disc
    bu_re = jnp.einsum('lbgh,gph->lbgp', u, bbar_re)
    bu_im = jnp.einsum('lbgh,gph->lbgp', u, bbar_im)
    first = -1 if reverse else 0
    bu_re = bu_re.at[first].add(abar_re * h0_re - abar_im * h0_im)
    bu_im = bu_im.at[first].add(abar_re * h0_im + abar_im * h0_re)
    n = u.shape[0]
    a_re = jnp.broadcast_to(abar_re, (n, 1) + abar_re.shape)
    a_im = jnp.broadcast_to(abar_im, (n, 1) + abar_im.shape)
    _, _, h_re, h_im = lax.associative_scan(
        _linear_recurrence_combine, (a_re, a_im, bu_re, bu_im), reverse=reverse, axis=0)
    return h_re, h_im


def s5_bidirectional(u, lp, h0, need_output):
    y = None
    finals = []
    for d, reverse in enumerate((False, True)):
        disc = s5_discretize(lp["lam_re"][d], lp["lam_im"][d], lp["b_re"][d],
                             lp["b_im"][d], lp["log_step"][d])
        h_re, h_im = s5_scan(u, disc, h0[d][0], h0[d][1], reverse)
        last = 0 if reverse else -1
        finals.append((h_re[last], h_im[last]))
        if need_output:
            yd = (jnp.einsum('lbgp,ghp->lbgh', h_re, lp["c_re"][d].astype(jnp.float32))
                  - jnp.einsum('lbgp,ghp->lbgh', h_im, lp["c_im"][d].astype(jnp.float32)))
            y = yd if y is None else y + yd
    return y, finals


def fourier_mixer(v, w_four):
    b, l, _ = v.shape
    vg = v.reshape(b, l, FOURIER_GROUPS, FOURIER_GROUP).astype(jnp.float32)
    f = jnp.fft.fft2(vg, axes=(1, 3), norm="ortho").real.astype(v.dtype)
    return jnp.einsum('blgc,gcd->blgd', f, w_four).reshape(b, l, D_FOURIER)


def mixer_inputs(h_stream, shift, scale, lp):
    h = modulate(rms_norm(h_stream, lp["norm1_g"]), shift, scale)
    z = h @ lp["w_in"]
    return to_lbgh(z[..., :D_S5]), z[..., D_S5:]


def mixer_output(y_s5, u, v, lp):
    b, l = v.shape[0], v.shape[1]
    y = y_s5 + lp["s5_d"].reshape(S5_GROUPS, S5_GROUP) * u
    y = y.transpose(1, 0, 2, 3).reshape(b, l, D_S5).astype(v.dtype)
    y = jax.nn.gelu(y)
    y = y * jax.nn.sigmoid(y @ lp["s5_w_glu"])
    f = fourier_mixer(v, lp["fourier_w"])
    merged = jnp.concatenate([rms_norm(y, lp["mix_norm_s5_g"]),
                              rms_norm(f, lp["mix_norm_f_g"])], axis=-1)
    return merged @ lp["w_out"]


def hmoe(h, lp):
    def per_sample(t):
        f32 = jnp.float32
        g_logits = (t @ lp["w_group"] + lp["b_group"]).astype(f32)
        g_prob = jax.nn.softmax(g_logits, axis=-1)
        g_w, g_idx = lax.top_k(g_prob, 1)
        e_logits = (jnp.einsum('td,dge->tge', t, lp["w_router"]) + lp["b_router"]).astype(f32)
        sel = jax.nn.one_hot(g_idx[:, 0], N_EXPERT_GROUPS, dtype=f32)
        e_sel = jnp.sum(e_logits * sel[:, :, None], axis=1)
        top_v, top_i = lax.top_k(e_sel, TOP_K_INNER)
        weights = g_w * jax.nn.softmax(top_v, axis=-1)
        expert_id = g_idx * EXPERTS_PER_GROUP + top_i
        gates = jnp.sum(jax.nn.one_hot(expert_id, N_EXPERTS, dtype=f32) * weights[..., None], axis=1)
        hg = jnp.einsum('td,edf->tef', t, lp["w_gate"])
        hu = jnp.einsum('td,edf->tef', t, lp["w_up"])
        a = jax.nn.silu(hg) * hu * gates[..., None].astype(t.dtype)
        return jnp.einsum('tef,efd->td', a, lp["w_down"])
    return lax.map(per_sample, h)


def setup_inputs(seed: int = 0) -> dict:
    key = jax.random.key(seed)
    ks = jax.random.split(key, 30)
    f32 = jnp.float32

    def nrm(k, shape, s):
        return s * jax.random.normal(k, shape, f32)

    G, P, H = S5_GROUPS, S5_STATE, S5_GROUP
    lam_im0 = math.pi * jnp.arange(P, dtype=f32)
    return {
        "x": nrm(ks[0], (BATCH, SEQ, D_MODEL), 1.0),
        "c": nrm(ks[1], (BATCH, D_MODEL), 1.0),
        "ctx": nrm(ks[2], (BATCH, CTX_LEN, D_MODEL), 1.0),
        "c_ctx": nrm(ks[3], (D_MODEL,), 1.0),
        "w_ada": nrm(ks[4], (DEPTH, D_MODEL, 6 * D_MODEL), 0.5 * D_MODEL ** -0.5),
        "b_ada": nrm(ks[5], (DEPTH, 6 * D_MODEL), 0.02),
        "norm1_g": 1.0 + nrm(ks[6], (DEPTH, D_MODEL), 0.02),
        "norm2_g": 1.0 + nrm(ks[7], (DEPTH, D_MODEL), 0.02),
        "w_in": nrm(ks[8], (DEPTH, D_MODEL, D_MIX), D_MODEL ** -0.5),
        "s5_lam_re": -0.5 + nrm(ks[9], (DEPTH, 2, G, P), 0.01),
        "s5_lam_im": lam_im0 + nrm(ks[10], (DEPTH, 2, G, P), 0.01),
        "s5_b_re": nrm(ks[11], (DEPTH, 2, G, P, H), (2.0 * H) ** -0.5),
        "s5_b_im": nrm(ks[12], (DEPTH, 2, G, P, H), (2.0 * H) ** -0.5),
        "s5_c_re": nrm(ks[13], (DEPTH, 2, G, H, P), (2.0 * P) ** -0.5),
        "s5_c_im": nrm(ks[14], (DEPTH, 2, G, H, P), (2.0 * P) ** -0.5),
        "s5_log_step": jax.random.uniform(ks[15], (DEPTH, 2, G), f32,
                                          math.log(DT_MIN), math.log(DT_MAX)),
        "s5_d": nrm(ks[16], (DEPTH, D_S5), 1.0),
        "s5_w_glu": nrm(ks[17], (DEPTH, D_S5, D_S5), D_S5 ** -0.5),
        "fourier_w": nrm(ks[18], (DEPTH, FOURIER_GROUPS, FOURIER_GROUP, FOURIER_GROUP),
                          FOURIER_GROUP ** -0.5),
        "mix_norm_s5_g": 1.0 + nrm(ks[19], (DEPTH, D_S5), 0.02),
        "mix_norm_f_g": 1.0 + nrm(ks[20], (DEPTH, D_FOURIER), 0.02),
        "w_out": nrm(ks[21], (DEPTH, D_MIX, D_MODEL), D_MIX ** -0.5),
        "moe_w_group": nrm(ks[22], (DEPTH, D_MODEL, N_EXPERT_GROUPS), D_MODEL ** -0.5),
        "moe_b_group": nrm(ks[23], (DEPTH, N_EXPERT_GROUPS), 0.01),
        "moe_w_router": nrm(ks[24], (DEPTH, D_MODEL, N_EXPERT_GROUPS, EXPERTS_PER_GROUP),
                             D_MODEL ** -0.5),
        "moe_b_router": nrm(ks[25], (DEPTH, N_EXPERT_GROUPS, EXPERTS_PER_GROUP), 0.01),
        "moe_w_gate": nrm(ks[26], (DEPTH, N_EXPERTS, D_MODEL, D_EXPERT), D_MODEL ** -0.5),
        "moe_w_up": nrm(ks[27], (DEPTH, N_EXPERTS, D_MODEL, D_EXPERT), D_MODEL ** -0.5),
        "moe_w_down": nrm(ks[28], (DEPTH, N_EXPERTS, D_EXPERT, D_MODEL), D_EXPERT ** -0.5),
        "final_norm_g": 1.0 + nrm(ks[29], (D_MODEL,), 0.02),
    }


def reference(x, c, ctx, c_ctx, w_ada, b_ada, norm1_g, norm2_g, w_in,
              s5_lam_re, s5_lam_im, s5_b_re, s5_b_im, s5_c_re, s5_c_im, s5_log_step,
              s5_d, s5_w_glu, fourier_w, mix_norm_s5_g, mix_norm_f_g, w_out,
              moe_w_group, moe_b_group, moe_w_router, moe_b_router,
              moe_w_gate, moe_w_up, moe_w_down, final_norm_g):
    n_lat = x.shape[1]
    rows = n_lat // GRID_W
    assert rows * GRID_W == n_lat
    batch = x.shape[0]
    ctx_h = ctx
    for layer in range(DEPTH):
        last = layer == DEPTH - 1
        lp = {
            "norm1_g": norm1_g[layer], "norm2_g": norm2_g[layer], "w_in": w_in[layer],
            "lam_re": s5_lam_re[layer], "lam_im": s5_lam_im[layer],
            "b_re": s5_b_re[layer], "b_im": s5_b_im[layer],
            "c_re": s5_c_re[layer], "c_im": s5_c_im[layer],
            "log_step": s5_log_step[layer], "s5_d": s5_d[layer], "s5_w_glu": s5_w_glu[layer],
            "fourier_w": fourier_w[layer], "mix_norm_s5_g": mix_norm_s5_g[layer],
            "mix_norm_f_g": mix_norm_f_g[layer], "w_out": w_out[layer],
            "w_group": moe_w_group[layer], "b_group": moe_b_group[layer],
            "w_router": moe_w_router[layer], "b_router": moe_b_router[layer],
            "w_gate": moe_w_gate[layer], "w_up": moe_w_up[layer], "w_down": moe_w_down[layer],
        }
        sh1, sc1, g1, sh2, sc2, g2 = adaln(c, w_ada[layer], b_ada[layer])
        csh1, csc1, cg1, csh2, csc2, cg2 = adaln(c_ctx[None, :], w_ada[layer], b_ada[layer])

        u_c, v_c = mixer_inputs(ctx_h, csh1, csc1, lp)
        zero = jnp.zeros((batch, S5_GROUPS, S5_STATE), jnp.float32)
        y_c, ctx_finals = s5_bidirectional(u_c, lp, ((zero, zero), (zero, zero)),
                                           need_output=not last)

        u_x, v_x = mixer_inputs(x, sh1, sc1, lp)
        y_x, _ = s5_bidirectional(u_x, lp, ctx_finals, need_output=True)
        x = x + g1 * mixer_output(y_x, u_x, v_x, lp)
        x = x + g2 * hmoe(modulate(rms_norm(x, lp["norm2_g"]), sh2, sc2), lp)

        if not last:
            ctx_h = ctx_h + cg1 * mixer_output(y_c, u_c, v_c, lp)
            ctx_h = ctx_h + cg2 * hmoe(modulate(rms_norm(ctx_h, lp["norm2_g"]), csh2, csc2), lp)
    return rms_norm(x, final_norm_g)
```

```python
import math
import numpy as np
import ml_dtypes
import concourse.bass as bass
import concourse.mybir as mybir
from concourse.bass_utils import run_bass_kernel_spmd

F32 = mybir.dt.float32
BF16 = mybir.dt.bfloat16
I32 = mybir.dt.int32
AF = mybir.ActivationFunctionType
ALU = mybir.AluOpType
AX = mybir.AxisListType

L = 4096
D = 1024
NCH = 512
NCX = 32
NE = 544
EPS = 1e-6
TWO_PI = 2.0 * math.pi
ARENA = 106000

DEBUG = False
SELF_ORDERED = ("pe",)


class Sched:
    def __init__(self, nc, sems, dma_sems):
        self.nc = nc
        self.eng = {n: dict(prog=[], sem=sems[n], count=0, known={}) for n in ("pe", "act", "dve", "pool", "sp")}
        self.dma_sems = dma_sems
        self.dma_counts = [0] * len(dma_sems)
        self.dma_rr = 0
        self.dma_rr_sw = 0
        self.last_w = {}
        self.readers = {}

    def _deps(self, reads, writes):
        toks = []
        for k in reads:
            t = self.last_w.get(k)
            if t:
                toks.append(t)
        for k in writes:
            t = self.last_w.get(k)
            if t:
                toks.append(t)
            toks += list(self.readers.get(k, {}).values())
        return toks

    def _waits(self, ename, toks):
        E = self.eng[ename]
        need = {}
        for (sid, sem, val, src) in toks:
            if src == ename and ename in SELF_ORDERED:
                continue
            if E["known"].get(sid, 0) >= val:
                continue
            if need.get(sid, (None, 0))[1] < val:
                need[sid] = (sem, val)
        out = []
        for sid, (sem, val) in need.items():
            E["known"][sid] = val
            out.append((sem, val))
        return out

    def _commit(self, tok, reads, writes):
        for k in writes:
            self.last_w[k] = tok
            self.readers[k] = {}
        for k in reads:
            d = self.readers.setdefault(k, {})
            o = d.get(tok[0])
            if o is None or o[2] < tok[2]:
                d[tok[0]] = tok

    def op(self, ename, fn, reads=(), writes=()):
        E = self.eng[ename]
        waits = self._waits(ename, self._deps(reads, writes))
        E["count"] += 1
        val = E["count"]
        sem = E["sem"]

        def run(e, fn=fn, waits=waits, sem=sem):
            for (s, v) in waits:
                e.wait_ge(s, v)
            fn(e).then_inc(sem, 1)
        E["prog"].append(run)
        tok = (id(sem), sem, val, ename)
        self._commit(tok, reads, writes)
        return tok

    def dma(self, q, out, in_, reads=(), writes=(), **kw):
        E = self.eng[q]
        nsw = 12
        nhw = len(self.dma_sems) - nsw
        if q == "pool":
            i = nhw + self.dma_rr_sw
            self.dma_rr_sw = (self.dma_rr_sw + 1) % nsw
        else:
            i = self.dma_rr
            self.dma_rr = (i + 1) % nhw
        sem = self.dma_sems[i]
        prev = self.dma_counts[i]
        toks = self._deps(reads, writes)
        if prev > 0:
            toks.append((id(sem), sem, 16 * prev, "dma"))
        waits = self._waits(q, toks)
        self.dma_counts[i] += 1
        val = 16 * self.dma_counts[i]

        def run(e, waits=waits, sem=sem, out=out, in_=in_, kw=kw):
            for (s, v) in waits:
                e.wait_ge(s, v)
            e.dma_start(out=out, in_=in_, **kw).then_inc(sem, 16)
        E["prog"].append(run)
        tok = (id(sem), sem, val, "dma")
        self._commit(tok, reads, writes)
        return tok

    def barrier(self):
        toks = []
        for n, E in self.eng.items():
            if E["count"] > 0:
                toks.append((id(E["sem"]), E["sem"], E["count"], "bar"))
        for i, s in enumerate(self.dma_sems):
            if self.dma_counts[i] > 0:
                toks.append((id(s), s, 16 * self.dma_counts[i], "bar"))
        for n, E in self.eng.items():
            waits = self._waits(n, toks)
            if waits:
                def run(e, waits=waits):
                    for (s, v) in waits:
                        e.wait_ge(s, v)
                E["prog"].append(run)

    def finish(self):
        toks = []
        for i, s in enumerate(self.dma_sems):
            if self.dma_counts[i] > 0:
                toks.append((id(s), s, 16 * self.dma_counts[i], "bar"))
        waits = self._waits("sp", toks)

        def run(e, waits=waits):
            for (s, v) in waits:
                e.wait_ge(s, v)
        self.eng["sp"]["prog"].append(run)


class Arena:
    def __init__(self, ap):
        self.ap = ap
        self.top = 0

    def alloc(self, shape, dtype, parts=128):
        n = 1
        for d in shape:
            n *= d
        nb = n * (2 if dtype == BF16 else 4)
        nb = (nb + 63) // 64 * 64
        off = self.top
        self.top += nb // 2
        assert self.top <= ARENA, f"arena overflow {self.top}"
        v = self.ap[0:parts, off:off + nb // 2]
        if dtype != BF16:
            v = v.bitcast(dtype)
        v = v[:, 0:n]
        if len(shape) == 2:
            return v.rearrange("p (a b) -> p a b", a=shape[0])
        if len(shape) == 3:
            return v.rearrange("p (a b c) -> p a b c", a=shape[0], b=shape[1])
        if len(shape) == 4:
            return v.rearrange("p (a b c d) -> p a b c d", a=shape[0], b=shape[1], c=shape[2])
        return v


def build_nc(dbg=(), stop_after=99):
    nc = bass.Bass("TRN2", target_bir_lowering=False)

    def din(name, shape, dt=F32):
        return nc.dram_tensor(name, list(shape), dt, kind="ExternalInput").ap()

    x = din("x", [L, D])
    ctx = din("ctx", [256, D])
    rows_in = din("rows_in", [128, 128])
    w_ada = din("w_ada", [D, 6 * D])
    b_ada = din("b_ada", [6 * D])
    w_in = din("w_in", [D, D])
    lam_re = din("lam_re", [2, 32, 64])
    lam_im = din("lam_im", [2, 32, 64])
    b_re = din("b_re", [2, 32, 64, 16])
    b_im = din("b_im", [2, 32, 64, 16])
    c_re = din("c_re", [2, 32, 16, 64])
    c_im = din("c_im", [2, 32, 16, 64])
    log_step = din("log_step", [2, 32])
    s5_d = din("s5_d", [512])
    w_glu = din("w_glu", [512, 512])
    four_w = din("four_w", [4, 128, 128])
    w_out = din("w_out", [D, D])
    w_rt = din("w_rt", [D, 36])
    b_rt = din("b_rt", [36])
    w_gate = din("w_gate", [32, D, 256])
    w_up = din("w_up", [32, D, 256])
    w_down = din("w_down", [32, 256, D])
    fin_g = din("fin_g", [D])
    c_ident = din("c_ident", [128, 128])
    c_masks = din("c_masks", [128, 256])
    c_kidx = din("c_kidx", [128, 18])
    c_rep16 = din("c_rep16", [16, 128])
    c_dftc = din("c_dftc", [128, 256])
    c_tab = din("c_tab", [128, 2, 64, 128], BF16)
    y = nc.dram_tensor("y", [L, D], F32, kind="ExternalOutput").ap()
    x1_scr = nc.dram_tensor("x1_scr", [L, D], F32, kind="Internal").ap()
    tT_scr = nc.dram_tensor("tT_scr", [8, 128, L], BF16, kind="Internal").ap()
    fT_scr = nc.dram_tensor("fT_scr", [32, 128, 4, 128], BF16, kind="Internal").ap()
    y5_scr = nc.dram_tensor("y5_scr", [L, 512], F32, kind="Internal").ap()
    dbg_out = {}
    for name, shape in dbg:
        dbg_out[name] = nc.dram_tensor(name, list(shape), F32, kind="ExternalOutput").ap()

    from contextlib import ExitStack
    with ExitStack() as es:
        arena_t = es.enter_context(nc.sbuf_tensor("arena", [128, ARENA], BF16))
        ps_t = es.enter_context(nc.psum_tensor("ps", [128, 4096], F32))
        sems = {n: es.enter_context(nc.semaphore("sem_" + n)) for n in ("pe", "act", "dve", "pool", "sp")}
        dma_sems = [es.enter_context(nc.semaphore(f"dsem{i}")) for i in range(40)]
        block = es.enter_context(nc.Block())
        K = Sched(nc, sems, dma_sems)
        A = Arena(arena_t)

        def PS(b, n=512, off=0):
            return ps_t[:, b * 512 + off: b * 512 + off + n]

        def PSB(b):
            return ps_t[:, b * 512:(b + 1) * 512].bitcast(BF16)

        def pk(b):
            return ("ps", b)

        def cp(eng, out, in_, reads, writes):
            if eng == "act":
                K.op("act", lambda e: e.activation(out=out, in_=in_, func=AF.Identity), reads=reads, writes=writes)
            else:
                K.op(eng, lambda e: e.tensor_copy(out=out, in_=in_), reads=reads, writes=writes)

        def dump(name, src_ap, key):
            if name in dbg_out:
                K.dma("sp", dbg_out[name], src_ap, reads=[key], writes=[("dbg", name)])

        ident_f = A.alloc([128], F32)
        ident_b = A.alloc([128], BF16)
        masks = A.alloc([256], F32)
        kidx = A.alloc([18], F32)
        rep16 = A.alloc([128], F32)
        dftc = A.alloc([256], F32)
        cols = A.alloc([128], F32)
        scol = A.alloc([16], F32)
        modc = A.alloc([48, 2], F32)
        vecs = A.alloc([8, 8], F32)
        g1_bc = A.alloc([1024], F32)
        g2_bc = A.alloc([1024], F32)
        fin_bc = A.alloc([1024], F32)
        brt_bc = A.alloc([36], F32)
        Dcol = A.alloc([32], F32)
        AcAs = A.alloc([4, 256], BF16)
        wrt = A.alloc([8, 36], F32)
        small = A.alloc([64], F32)
        persist_top = A.top

        class _Stop(Exception):
            pass

        def chk(k):
            if stop_after == k:
                raise _Stop()

        try:
            K.dma("sp", ident_f, c_ident, writes=["ident_f"])
            K.dma("sp", masks, c_masks, writes=["masks"])
            K.dma("sp", kidx, c_kidx, writes=["kidx"])
            K.dma("sp", rep16[0:16, :], c_rep16, writes=["rep16"])
            K.dma("sp", dftc, c_dftc, writes=["dftc"])
            K.dma("sp", g1_bc, b_ada[2048:3072].partition_broadcast(128), writes=["g1_bc"])
            K.dma("sp", g2_bc, b_ada[5120:6144].partition_broadcast(128), writes=["g2_bc"])
            K.dma("sp", fin_bc, fin_g.partition_broadcast(128), writes=["fin_bc"])
            K.dma("sp", brt_bc, b_rt.partition_broadcast(128), writes=["brt_bc"])
            K.dma("sp", wrt, w_rt.rearrange("(kc p) n -> p kc n", p=128), writes=["wrt"])
            K.op("dve", lambda e: e.tensor_copy(out=ident_b, in_=ident_f), reads=["ident_f"], writes=["ident_b"])

            p2start = A.top
            U = A.alloc([32, 576], BF16)
            PQ = A.alloc([32, 1024], BF16)
            p2mark = A.top
            w_in_bf = A.alloc([8, 1024], BF16)
            w_in_r = w_in.rearrange("(kc p) n -> p kc n", p=128)
            for kc in range(8):
                K.dma("pool", w_in_bf[:, kc, :], w_in_r[:, kc, :], writes=["w_in_bf"])
            mark = A.top
            rows_sb = A.alloc([128], F32)
            srep = A.alloc([8, 128], F32)
            wa = [A.alloc([8, 512], F32) for _ in range(2)]
            K.dma("sp", rows_sb, rows_in, writes=["rows_sb"])
            K.op("pe", lambda e: e.transpose(PS(0, 128), rows_sb, ident_f), reads=["rows_sb", "ident_f"], writes=[pk(0)])
            K.op("dve", lambda e: e.tensor_copy(out=cols, in_=PS(0, 128)), reads=[pk(0)], writes=["cols"])
            K.op("act", lambda e: e.activation(out=scol, in_=cols[:, 0:16], func=AF.Silu), reads=["cols"], writes=["scol"])
            K.op("dve", lambda e: e.tensor_copy(out=srep, in_=scol[:, 0:8].unsqueeze(2).to_broadcast([128, 8, 128])),
                 reads=["scol"], writes=["srep"])
            wa_rr = [w_ada.rearrange("(kc p) n -> p kc n", p=128)[:, :, cc * 512:(cc + 1) * 512] for cc in range(12)]
            for cc in range(12):
                buf = wa[cc % 2]
                K.dma("sp" if cc % 2 == 0 else "act", buf, wa_rr[cc], writes=[("wa", cc % 2)])
                for fb in range(4):
                    j = cc * 4 + fb
                    for kc in range(8):
                        K.op("pe", lambda e, buf=buf, fb=fb, kc=kc, j=j: e.matmul(
                            PS(1, 2, 2 * j), lhsT=buf[:, kc, fb * 128:(fb + 1) * 128], rhs=scol[:, kc:16:8],
                            start=(kc == 0), stop=(kc == 7)),
                            reads=[("wa", cc % 2), "scol"], writes=[pk(1)])
                if cc in (4, 5, 10, 11):
                    tgt = g1_bc if cc in (4, 5) else g2_bc
                    tk = "g1_bc" if cc in (4, 5) else "g2_bc"
                    half = cc % 2 if cc in (4, 5) else (cc - 10)
                    for kc in range(8):
                        K.op("pe", lambda e, buf=buf, kc=kc: e.matmul(
                            PS(2), lhsT=srep[:, kc, :], rhs=buf[:, kc, :], start=(kc == 0), stop=(kc == 7)),
                            reads=[("wa", cc % 2), "srep"], writes=[pk(2)])
                    K.op("dve", lambda e, tgt=tgt, half=half: e.tensor_tensor(
                        out=tgt[:, half * 512:(half + 1) * 512], in0=PS(2), in1=tgt[:, half * 512:(half + 1) * 512], op=ALU.add),
                        reads=[pk(2), tk], writes=[tk])
            K.op("dve", lambda e: e.tensor_tensor(
                out=modc, in0=PS(1, 96).rearrange("p (j t) -> p j t", t=2),
                in1=cols[:, 32:80].unsqueeze(2).to_broadcast([128, 48, 2]), op=ALU.add),
                reads=[pk(1), "cols"], writes=["modc"])
            def vec_ops(e_name, dst, g_lo, sc_j, which):
                K.op("dve", lambda e: e.scalar_tensor_tensor(
                    out=vecs[:, dst, :], in0=modc[:, sc_j:sc_j + 8, which], scalar=1.0, in1=cols[:, g_lo:g_lo + 8],
                    op0=ALU.add, op1=ALU.mult), reads=["modc", "cols"], writes=["vecs"])
            vec_ops("dve", 0, 16, 8, 0)
            vec_ops("dve", 2, 16, 8, 1)
            vec_ops("dve", 4, 24, 32, 0)
            K.op("dve", lambda e: e.tensor_copy(out=vecs[:, 1, :], in_=modc[:, 0:8, 0]), reads=["modc"], writes=["vecs"])
            K.op("dve", lambda e: e.tensor_copy(out=vecs[:, 3, :], in_=modc[:, 0:8, 1]), reads=["modc"], writes=["vecs"])
            K.op("dve", lambda e: e.tensor_copy(out=vecs[:, 5, :], in_=modc[:, 24:32, 0]), reads=["modc"], writes=["vecs"])
            if "d_mod" in dbg_out:
                K.dma("sp", dbg_out["d_mod"], modc.rearrange("p j t -> p (j t)"), reads=["modc"], writes=[("dbg", 0)])
                K.dma("sp", dbg_out["d_g1"], g1_bc, reads=["g1_bc"], writes=[("dbg", 1)])
            drow = A.alloc([32], F32)
            with nc.allow_non_contiguous_dma(reason="tiny s5_d gather"):
                K.dma("sp", drow[0:16, :], s5_d.rearrange("(g h) -> h g", h=16), writes=["drow"], allow_slow_non_contiguous=True)
            K.op("pe", lambda e: e.matmul(PS(3, 32), lhsT=rep16[0:16, :], rhs=drow[0:16, :], start=True, stop=True),
                 reads=["rep16", "drow"], writes=[pk(3)])
            K.op("dve", lambda e: e.tensor_copy(out=Dcol, in_=PS(3, 32)), reads=[pk(3)], writes=["Dcol"])
            wf = A.alloc([4, 128], F32)
            K.dma("sp", wf, four_w.rearrange("g m d -> m g d"), writes=["wf"])
            for g in range(4):
                for t in range(2):
                    K.op("pe", lambda e, g=g, t=t: e.matmul(
                        PS(4, 128, (g * 2 + t) * 128 % 512) if False else ps_t[:, 4 * 512 + (g * 2 + t) * 128: 4 * 512 + (g * 2 + t) * 128 + 128],
                        lhsT=dftc[:, t * 128:(t + 1) * 128], rhs=wf[:, g, :], start=True, stop=True),
                        reads=["dftc", "wf"], writes=[pk(4 + (g * 2 + t) // 4)])
            K.op("dve", lambda e: e.tensor_copy(out=AcAs.rearrange("p g n -> p (g n)"), in_=ps_t[:, 4 * 512:6 * 512]),
                 reads=[pk(4), pk(5)], writes=["AcAs"])
            K.barrier()
            A.top = mark

            chk(1)
            xt = [A.alloc([1024], F32) for _ in range(4)]
            junk2 = [A.alloc([1024], BF16) for _ in range(2)]
            junk = A.alloc([1024], BF16)
            xs = [A.alloc([1024], BF16) for _ in range(2)]
            hT = [A.alloc([8, 128], BF16) for _ in range(2)]
            Zs = [A.alloc([32, 128], BF16) for _ in range(2)]
            vT = [A.alloc([4, 128], BF16) for _ in range(2)]
            stat = A.alloc([40, 4], F32)
            x_r = x.rearrange("(c s) d -> s c d", s=8)
            ctx_r = ctx.rearrange("(c s) d -> s c d", s=8)

            def rms_prep(ti, src_key, src_ap, rows, nfeat, st, junk_ap, out_bf, out_key, eng_scale="dve", junk_key="junk"):
                K.op("act", lambda e: e.activation(out=junk_ap, in_=src_ap, func=AF.Square, accum_out=st[0:rows, 0:1]),
                     reads=[src_key], writes=[junk_key, ("stat", ti)])
                K.op("act", lambda e: e.activation(out=st[0:rows, 1:2], in_=st[0:rows, 0:1], func=AF.Sqrt,
                                                   scale=1.0 / nfeat, bias=small[0:rows, 0:1]),
                     reads=[("stat", ti), "small"], writes=[("stat", ti)])
                K.op("dve", lambda e: e.reciprocal(out=st[0:rows, 2:3], in_=st[0:rows, 1:2]),
                     reads=[("stat", ti)], writes=[("stat", ti)])
                K.op(eng_scale, lambda e: e.tensor_scalar(out=out_bf, in0=src_ap, scalar1=st[0:rows, 2:3], scalar2=None, op0=ALU.mult),
                     reads=[src_key, ("stat", ti)], writes=[out_key])

            K.op("dve", lambda e: e.memset(small[:, 0:1], EPS), writes=["small"])

            tiles = [("c", s, 0) for s in range(8)] + [("x", s, j) for j in range(4) for s in range(8)]
            def p2_tile(ti):
                kind, s, j = tiles[ti]
                rows = 32 if kind == "c" else 128
                b2 = ti % 2
                b4 = ti % 4
                rms_prep(ti, ("xt", b4), xt[b4][0:rows, :], rows, 1024.0, stat[:, ti, :], junk2[b2][0:rows, :], xs[b2][0:rows, :], ("xs", b2), junk_key=("junk2", b2))
                yield
                pb = 0 + b2
                for kc in range(8):
                    K.op("pe", lambda e, kc=kc, b2=b2, pb=pb, rows=rows: e.transpose(
                        PSB(pb)[:, kc * 128: kc * 128 + rows], xs[b2][0:rows, kc * 128:(kc + 1) * 128], ident_b[0:rows, 0:rows]),
                        reads=[("xs", b2), "ident_b"], writes=[pk(pb)])
                yield
                av, sv = (2, 3) if kind == "c" else (0, 1)
                psv = PSB(pb).rearrange("p (k t) -> p k t", k=8)[:, :, 0:rows]
                K.op("dve", lambda e, psv=psv, b2=b2, rows=rows, av=av: e.tensor_tensor(
                    out=hT[b2][:, :, 0:rows], in0=psv, in1=vecs[:, av, :].unsqueeze(2).to_broadcast([128, 8, rows]), op=ALU.mult),
                    reads=[pk(pb), "vecs"], writes=[("hT", b2)])
                K.op("pool", lambda e, b2=b2, rows=rows, sv=sv: e.tensor_tensor(
                    out=hT[b2][:, :, 0:rows], in0=hT[b2][:, :, 0:rows], in1=vecs[:, sv, :].unsqueeze(2).to_broadcast([128, 8, rows]), op=ALU.add),
                    reads=[("hT", b2), "vecs"], writes=[("hT", b2)])
                yield
                zb = (ti // 8) % 2
                pz = 2 + b2
                for kc in range(8):
                    K.op("pe", lambda e, kc=kc, b2=b2, pz=pz, rows=rows: e.matmul(
                        PS(pz)[0:rows, :], lhsT=hT[b2][:, kc, 0:rows], rhs=w_in_bf[:, kc, 0:512], start=(kc == 0), stop=(kc == 7)),
                        reads=[("hT", b2), "w_in_bf"], writes=[pk(pz)])
                K.op("act", lambda e, zb=zb, s=s, pz=pz, rows=rows: e.activation(
                    out=Zs[zb][0:rows].rearrange("p g (s h) -> p g s h", s=8)[:, :, s, :],
                    in_=PS(pz)[0:rows, :].rearrange("p (g h) -> p g h", g=32), func=AF.Identity),
                     reads=[pk(pz)], writes=[("Zs", zb, s)])
                yield
                if kind == "x":
                    pv = 4 + b2
                    for g in range(4):
                        for kc in range(8):
                            K.op("pe", lambda e, g=g, kc=kc, b2=b2, pv=pv: e.matmul(
                                PS(pv, 128, g * 128), lhsT=w_in_bf[:, kc, 512 + g * 128: 512 + (g + 1) * 128], rhs=hT[b2][:, kc, :],
                                start=(kc == 0), stop=(kc == 7)), reads=[("hT", b2), "w_in_bf"], writes=[pk(pv)])
                    K.op("act", lambda e, b2=b2, pv=pv: e.activation(out=vT[b2].rearrange("p g t -> p (g t)"), in_=PS(pv), func=AF.Identity),
                         reads=[pk(pv)], writes=[("vT", b2)])
                    tl = j * 8 + s
                    for g in range(4):
                        K.op("pe", lambda e, g=g, b2=b2: e.matmul(
                            PS(6 + g // 2, 256, (g % 2) * 256), lhsT=vT[b2][:, g, :], rhs=AcAs[:, g, :], start=True, stop=True),
                            reads=[("vT", b2), "AcAs"], writes=[pk(6 + g // 2)])
                    K.op("dve", lambda e, tl=tl: e.tensor_copy(
                        out=PQ[:, tl, :].rearrange("p (r g m) -> p g r m", r=2, g=4),
                        in_=ps_t[:, 6 * 512:8 * 512].rearrange("p (g r m) -> p g r m", g=4, r=2)),
                         reads=[pk(6), pk(7)], writes=[("PQ", tl)])
                yield
                if s == 7:
                    nrow = rows
                    if kind == "c":
                        col_sets = [(0, 32), (544, 576)]
                    else:
                        col_sets = [(32 + 128 * j, 32 + 128 * (j + 1))]
                    for g8 in range(4):
                        pu = 6 + (g8 % 2)
                        for gi in range(8):
                            g = g8 * 8 + gi
                            K.op("pe", lambda e, g=g, gi=gi, zb=zb, pu=pu, nrow=nrow: e.transpose(
                                PSB(pu)[:, gi * 128: gi * 128 + nrow], Zs[zb][0:nrow, g, :], ident_b[0:nrow, 0:nrow]),
                                reads=[("Zs", zb, ss_) for ss_ in range(8)] + ["ident_b"], writes=[pk(pu)])
                        for (c0, c1) in col_sets:
                            cp("act" if g8 % 2 == 0 else "dve", U[:, g8 * 8:(g8 + 1) * 8, c0:c1],
                               PSB(pu).rearrange("p (g c) -> p g c", g=8)[:, :, 0:nrow], [pk(pu)], [("U", g8)])

            def p2_load(ti):
                kind, s, j = tiles[ti]
                rows = 32 if kind == "c" else 128
                src = ctx_r[s, :, :] if kind == "c" else x_r[s, j * 128:(j + 1) * 128, :]
                K.dma("sp", xt[ti % 4][0:rows, :], src, writes=[("xt", ti % 4)])
            p2_load(0); p2_load(1)
            for pr in range(len(tiles) // 2):
                if 2 * pr + 3 < len(tiles):
                    p2_load(2 * pr + 2); p2_load(2 * pr + 3)
                gens = [p2_tile(2 * pr), p2_tile(2 * pr + 1)]
                alive = [True, True]
                while any(alive):
                    for gi_, g_ in enumerate(gens):
                        if alive[gi_]:
                            try:
                                next(g_)
                            except StopIteration:
                                alive[gi_] = False
            K.barrier()
            A.top = p2mark

            chk(2)
            p3mark = A.top
            fnTt = [A.alloc([4, 128], BF16) for _ in range(2)]
            tab = A.alloc([2, 64, 128], BF16)
            fsb = [A.alloc([512], F32) for _ in range(2)]
            fmn = [A.alloc([512], F32) for _ in range(2)]
            fnb = [A.alloc([512], BF16) for _ in range(2)]
            fjunk = A.alloc([512], BF16)
            fstat = A.alloc([32, 4], F32)
            PQv = PQ.rearrange("p t (r n) -> p t r n", r=2)
            for r in range(2):
                K.dma("sp" if r == 0 else "act", tab[:, r], c_tab[:, r], writes=["tab"])
            def dft_a(ot):
                b2 = ot % 2
                jo, so = ot // 8, ot % 8
                pA, pB = 2 * b2, 2 * b2 + 1
                plan = []
                for it in range(32):
                    ji, si = it // 8, it % 8
                    m = (ji * so + jo * si) % 4
                    ss = si * 8 + so
                    if m == 0:
                        plan += [(pA, 0, 0, it, ss), (pB, 1, 1, it, ss)]
                    elif m == 1:
                        plan += [(pB, 1, 0, it, ss), (pB, 0, 1, it, ss)]
                    elif m == 2:
                        plan += [(pB, 0, 0, it, ss), (pA, 1, 1, it, ss)]
                    else:
                        plan += [(pA, 1, 0, it, ss), (pA, 0, 1, it, ss)]
                la = [p for p in plan if p[0] == pA]; lb = [p for p in plan if p[0] == pB]
                plan = []
                for q in range(max(len(la), len(lb))):
                    if q < len(la):
                        plan.append(la[q])
                    if q < len(lb):
                        plan.append(lb[q])
                cnt = {pA: len(la), pB: len(lb)}
                seen = {pA: 0, pB: 0}
                for (bk, tr, qr, it, ss) in plan:
                    st_ = (seen[bk] == 0)
                    seen[bk] += 1
                    sp_ = (seen[bk] == cnt[bk])
                    K.op("pe", lambda e, bk=bk, tr=tr, qr=qr, it=it, ss=ss, st_=st_, sp_=sp_: e.matmul(
                        PS(bk), lhsT=tab[:, tr, ss, :], rhs=PQv[:, it, qr, :], start=st_, stop=sp_),
                        reads=["tab", ("PQ", it)], writes=[pk(bk)])
                cp("act", fmn[b2], PS(pB), [pk(pB)], [("fmn", b2)])
                K.op("dve", lambda e, b2=b2, pA=pA: e.tensor_tensor(out=fsb[b2], in0=PS(pA), in1=fmn[b2], op=ALU.subtract),
                     reads=[pk(pA), ("fmn", b2)], writes=[("fsb", b2)])
                if ot == 0 and "d_f" in dbg_out:
                    K.dma("sp", dbg_out["d_f"], fsb[0], reads=[("fsb", 0)], writes=[("dbg", 5)])
                K.op("act", lambda e, b2=b2, ot=ot: e.activation(out=fjunk, in_=fsb[b2], func=AF.Square, accum_out=fstat[:, ot, 0:1]),
                     reads=[("fsb", b2)], writes=["fjunk", ("fstat", ot)])
                K.op("act", lambda e, ot=ot: e.activation(out=fstat[:, ot, 1:2], in_=fstat[:, ot, 0:1], func=AF.Sqrt, scale=1.0 / 512.0, bias=small[:, 0:1]),
                     reads=[("fstat", ot), "small"], writes=[("fstat", ot)])
                K.op("dve", lambda e, ot=ot: e.reciprocal(out=fstat[:, ot, 2:3], in_=fstat[:, ot, 1:2]), reads=[("fstat", ot)], writes=[("fstat", ot)])
                K.op("dve", lambda e, b2=b2, ot=ot: e.tensor_scalar(out=fnb[b2], in0=fsb[b2], scalar1=fstat[:, ot, 2:3], scalar2=None, op0=ALU.mult),
                     reads=[("fsb", b2), ("fstat", ot)], writes=[("fnb", b2)])

            def dft_b(ot):
                b2 = ot % 2
                pt = 4 + b2
                for fb in range(4):
                    K.op("pe", lambda e, fb=fb, b2=b2, pt=pt: e.transpose(PSB(pt)[:, fb * 128:(fb + 1) * 128], fnb[b2][:, fb * 128:(fb + 1) * 128], ident_b),
                         reads=[("fnb", b2), "ident_b"], writes=[pk(pt)])
                K.op("dve", lambda e, b2=b2, pt=pt: e.tensor_copy(out=fnTt[b2].rearrange("p f t -> p (f t)"), in_=PSB(pt)[:, 0:512]),
                     reads=[pk(pt)], writes=[("fnTt", b2)])
                K.dma("act", fT_scr[ot], fnTt[b2], reads=[("fnTt", b2)], writes=[("fTscr", ot)])
            for ot in range(32):
                dft_a(ot)
                if ot > 0:
                    dft_b(ot - 1)
            dft_b(31)
            K.barrier()
            A.top = p2start + 32 * 576

            chk(3)
            GamRe = A.alloc([32, 128], BF16)
            GamIm = A.alloc([32, 128], BF16)
            Mi = A.alloc([32, 128], BF16)
            ovl = A.top
            PhiT = A.alloc([32, 2, 128], BF16)
            RS = 32
            ring = A.alloc([RS, 4, 32], F32)
            rtmp = A.alloc([4, 32], F32); rC = A.alloc([2, 32], F32)
            L4 = A.alloc([4, 32], F32)
            p4mark = A.top
            Lrows = A.alloc([2, 128], F32)
            LR = A.alloc([32], F32); LI = A.alloc([32], F32); LS = A.alloc([32], F32)
            Bre = A.alloc([32, 16], F32); Bim = A.alloc([32, 16], F32)
            Crows = A.alloc([2, 4, 128], F32)
            Cre = A.alloc([32, 16], F32); Cim = A.alloc([32, 16], F32)
            T = {n: A.alloc([32, 18], F32) for n in ("AK", "ANG", "ANGC", "TF", "SINK", "COSK", "MAGP", "MAGN", "LKR", "LKI")}
            TI = A.alloc([32, 18], I32)
            S = {n: A.alloc([32], F32) for n in ("dt", "a", "th", "nr", "den", "rden", "qre", "qim", "t1", "t2")}
            W = {n: A.alloc([32, 8], F32) for n in ("ire", "iim", "wre", "wim", "w2re", "w2im", "t1", "t2")}
            PH1 = A.alloc([16, 8, 16], F32); PH2 = A.alloc([16, 8, 16], F32)
            PhRe = A.alloc([32, 128], BF16); PhIm = A.alloc([32, 128], BF16)
            MT1 = PH1.rearrange("p g s h -> p (g s h)")[:, 0:512].rearrange("p (g m) -> p g m", g=4)
            MT2 = PH2.rearrange("p g s h -> p (g s h)")[:, 0:512].rearrange("p (g m) -> p g m", g=4)

            with nc.allow_non_contiguous_dma(reason="s5 small param layouts"):
                K.dma("sp", Lrows[0:32, 0, :].rearrange("g (d p) -> g d p", d=2), lam_re.rearrange("d g p -> g d p"), writes=["Lrows"])
                K.dma("sp", Lrows[0:32, 1, :].rearrange("g (d p) -> g d p", d=2), lam_im.rearrange("d g p -> g d p"), writes=["Lrows"])
                for d in range(2):
                    K.dma("sp", LS[64 * d:64 * d + 64, :], log_step[d].partition_broadcast(64), writes=["LS"])
                    K.dma("sp", Bre[64 * d:64 * d + 64], b_re[d].rearrange("g p h -> p g h"), writes=["Bre"])
                    K.dma("act", Bim[64 * d:64 * d + 64], b_im[d].rearrange("g p h -> p g h"), writes=["Bim"])
                    for gb in range(4):
                        K.dma("sp", Crows[:, 0, gb, 64 * d:64 * d + 64], c_re[d, gb * 8:(gb + 1) * 8].rearrange("gl h p -> (gl h) p"), writes=["Crows"])
                        K.dma("act", Crows[:, 1, gb, 64 * d:64 * d + 64], c_im[d, gb * 8:(gb + 1) * 8].rearrange("gl h p -> (gl h) p"), writes=["Crows"])
            for r, dst, key in ((0, LR, "LR"), (1, LI, "LI")):
                K.op("pe", lambda e, r=r: e.transpose(PS(0, 32), Lrows[0:32, r, :], ident_f[0:32, 0:32]), reads=["Lrows", "ident_f"], writes=[pk(0)])
                cp("dve", dst, PS(0, 32), [pk(0)], [key])
            for r, dst, key in ((0, Cre, "Cre"), (1, Cim, "Cim")):
                for gb in range(4):
                    K.op("pe", lambda e, r=r, gb=gb: e.transpose(PS(1, 128, gb * 128), Crows[:, r, gb, :], ident_f), reads=["Crows", "ident_f"], writes=[pk(1)])
                cp("dve", dst.rearrange("p g h -> p (g h)"), PS(1), [pk(1)], [key])

            def dve(fn, reads, writes, eng="dve"):
                K.op(eng, fn, reads=reads, writes=writes)
            chk(3.1)
            K.op("act", lambda e: e.activation(out=S["dt"], in_=LS, func=AF.Exp), reads=["LS"], writes=["dt"])
            dve(lambda e: e.tensor_tensor(out=S["a"], in0=LR, in1=S["dt"], op=ALU.mult), ["LR", "dt"], ["a"])
            dve(lambda e: e.tensor_tensor(out=S["th"], in0=LI, in1=S["dt"], op=ALU.mult), ["LI", "dt"], ["th"])
            kidx_b = kidx.unsqueeze(1).to_broadcast([128, 32, 18])
            dve(lambda e: e.tensor_tensor(out=T["AK"], in0=S["a"].unsqueeze(2).to_broadcast([128, 32, 18]), in1=kidx_b, op=ALU.mult), ["a", "kidx"], ["AK"])
            dve(lambda e: e.tensor_tensor(out=T["ANG"], in0=S["th"].unsqueeze(2).to_broadcast([128, 32, 18]), in1=kidx_b, op=ALU.mult), ["th", "kidx"], ["ANG"])
            dve(lambda e: e.tensor_scalar(out=T["ANGC"], in0=T["ANG"], scalar1=math.pi / 2, scalar2=None, op0=ALU.add), ["ANG"], ["ANGC"])
            for src, dst in (("ANG", "SINK"), ("ANGC", "COSK")):
                dve(lambda e, src=src: e.tensor_scalar(out=TI, in0=T[src], scalar1=1.0 / TWO_PI, scalar2=None, op0=ALU.mult), [src], ["TI"])
                dve(lambda e: e.tensor_copy(out=T["TF"], in_=TI), ["TI"], ["TF"])
                dve(lambda e, src=src: e.scalar_tensor_tensor(out=T["TF"], in0=T["TF"], scalar=-TWO_PI, in1=T[src], op0=ALU.mult, op1=ALU.add), ["TF", src], ["TF"])
                dve(lambda e: e.tensor_scalar(out=T["TF"], in0=T["TF"], scalar1=-3.14159, scalar2=3.14159, op0=ALU.max, op1=ALU.min), ["TF"], ["TF"])
                K.op("act", lambda e, dst=dst: e.activation(out=T[dst], in_=T["TF"], func=AF.Sin), reads=["TF"], writes=[dst])
            K.op("act", lambda e: e.activation(out=T["MAGP"], in_=T["AK"], func=AF.Exp), reads=["AK"], writes=["MAGP"])
            K.op("act", lambda e: e.activation(out=T["MAGN"], in_=T["AK"], func=AF.Exp, scale=-1.0), reads=["AK"], writes=["MAGN"])
            dve(lambda e: e.tensor_tensor(out=T["LKR"], in0=T["MAGP"], in1=T["COSK"], op=ALU.mult), ["MAGP", "COSK"], ["LKR"])
            dve(lambda e: e.tensor_tensor(out=T["LKI"], in0=T["MAGP"], in1=T["SINK"], op=ALU.mult), ["MAGP", "SINK"], ["LKI"])
            l1r = T["LKR"][:, :, 16]; l1i = T["LKI"][:, :, 16]
            dve(lambda e: e.tensor_scalar(out=S["nr"], in0=l1r, scalar1=-1.0, scalar2=None, op0=ALU.add), ["LKR"], ["nr"])
            dve(lambda e: e.tensor_tensor(out=S["den"], in0=LR, in1=LR, op=ALU.mult), ["LR"], ["den"])
            dve(lambda e: e.tensor_tensor(out=S["t1"], in0=LI, in1=LI, op=ALU.mult), ["LI"], ["t1"])
            dve(lambda e: e.tensor_tensor(out=S["den"], in0=S["den"], in1=S["t1"], op=ALU.add), ["den", "t1"], ["den"])
            dve(lambda e: e.reciprocal(out=S["rden"], in_=S["den"]), ["den"], ["rden"])
            dve(lambda e: e.tensor_tensor(out=S["t1"], in0=S["nr"], in1=LR, op=ALU.mult), ["nr", "LR"], ["t1"])
            dve(lambda e: e.tensor_tensor(out=S["t2"], in0=l1i, in1=LI, op=ALU.mult), ["LKI", "LI"], ["t2"])
            dve(lambda e: e.tensor_tensor(out=S["t1"], in0=S["t1"], in1=S["t2"], op=ALU.add), ["t1", "t2"], ["t1"])
            dve(lambda e: e.tensor_tensor(out=S["qre"], in0=S["t1"], in1=S["rden"], op=ALU.mult), ["t1", "rden"], ["qre"])
            dve(lambda e: e.tensor_tensor(out=S["t1"], in0=l1i, in1=LR, op=ALU.mult), ["LKI", "LR"], ["t1"])
            dve(lambda e: e.tensor_tensor(out=S["t2"], in0=S["nr"], in1=LI, op=ALU.mult), ["nr", "LI"], ["t2"])
            dve(lambda e: e.tensor_tensor(out=S["t1"], in0=S["t1"], in1=S["t2"], op=ALU.subtract), ["t1", "t2"], ["t1"])
            dve(lambda e: e.tensor_tensor(out=S["qim"], in0=S["t1"], in1=S["rden"], op=ALU.mult), ["t1", "rden"], ["qim"])
            dve(lambda e: e.tensor_tensor(out=W["ire"], in0=T["MAGN"][:, :, 0:8], in1=T["COSK"][:, :, 0:8], op=ALU.mult), ["MAGN", "COSK"], ["ire"])
            dve(lambda e: e.scalar_tensor_tensor(out=W["iim"], in0=T["MAGN"][:, :, 0:8], scalar=-1.0, in1=T["SINK"][:, :, 0:8], op0=ALU.mult, op1=ALU.mult), ["MAGN", "SINK"], ["iim"])
            qreb = S["qre"].unsqueeze(2).to_broadcast([128, 32, 8]); qimb = S["qim"].unsqueeze(2).to_broadcast([128, 32, 8])

            def cmul(ore, oim, ar, ai, br, bi, keys_a, keys_b, ko):
                dve(lambda e: e.tensor_tensor(out=W["t1"], in0=ar, in1=br, op=ALU.mult), keys_a + keys_b, ["Wt1"])
                dve(lambda e: e.tensor_tensor(out=W["t2"], in0=ai, in1=bi, op=ALU.mult), keys_a + keys_b, ["Wt2"])
                dve(lambda e: e.tensor_tensor(out=ore, in0=W["t1"], in1=W["t2"], op=ALU.subtract), ["Wt1", "Wt2"], [ko + "re"])
                dve(lambda e: e.tensor_tensor(out=W["t1"], in0=ar, in1=bi, op=ALU.mult), keys_a + keys_b + [ko + "re"], ["Wt1"])
                dve(lambda e: e.tensor_tensor(out=W["t2"], in0=ai, in1=br, op=ALU.mult), keys_a + keys_b + [ko + "re"], ["Wt2"])
                dve(lambda e: e.tensor_tensor(out=oim, in0=W["t1"], in1=W["t2"], op=ALU.add), ["Wt1", "Wt2"], [ko + "im"])
            cmul(W["wre"], W["wim"], W["ire"], W["iim"], qreb, qimb, ["ire", "iim"], ["qre", "qim"], "w")
            cmul(W["w2re"], W["w2im"], T["LKR"][:, :, 8:16], T["LKI"][:, :, 8:16], qreb, qimb, ["LKR", "LKI"], ["qre", "qim"], "w2")

            def outer(dst_re, dst_im, wr, wi, wkeys, Xre, Xim, xkeys, okey, neg_im=False):
                for gh in range(2):
                    gs = slice(gh * 16, gh * 16 + 16)
                    wrb = wr[:, gs, :].unsqueeze(3).to_broadcast([128, 16, 8, 16]); wib = wi[:, gs, :].unsqueeze(3).to_broadcast([128, 16, 8, 16])
                    xrb = Xre[:, gs, :].unsqueeze(2).to_broadcast([128, 16, 8, 16]); xib = Xim[:, gs, :].unsqueeze(2).to_broadcast([128, 16, 8, 16])
                    dr = dst_re[:, gs, :].rearrange("p g (s h) -> p g s h", s=8); di = dst_im[:, gs, :].rearrange("p g (s h) -> p g s h", s=8)
                    dve(lambda e, wrb=wrb, xrb=xrb: e.tensor_tensor(out=PH1, in0=wrb, in1=xrb, op=ALU.mult), wkeys + xkeys, ["PH1"])
                    dve(lambda e, wib=wib, xib=xib: e.tensor_tensor(out=PH2, in0=wib, in1=xib, op=ALU.mult), wkeys + xkeys, ["PH2"])
                    dve(lambda e, dr=dr: e.tensor_tensor(out=dr, in0=PH1, in1=PH2, op=ALU.subtract), ["PH1", "PH2"], [okey + "re"])
                    dve(lambda e, wrb=wrb, xib=xib: e.tensor_tensor(out=PH1, in0=wrb, in1=xib, op=ALU.mult), wkeys + xkeys + [okey + "re"], ["PH1"])
                    dve(lambda e, wib=wib, xrb=xrb: e.tensor_tensor(out=PH2, in0=wib, in1=xrb, op=ALU.mult), wkeys + xkeys + [okey + "re"], ["PH2"])
                    if neg_im:
                        dve(lambda e, di=di: e.scalar_tensor_tensor(out=di, in0=PH1, scalar=-1.0, in1=PH2, op0=ALU.mult, op1=ALU.subtract), ["PH1", "PH2"], [okey + "im"])
                    else:
                        dve(lambda e, di=di: e.tensor_tensor(out=di, in0=PH1, in1=PH2, op=ALU.add), ["PH1", "PH2"], [okey + "im"])
            chk(3.2)
            outer(PhRe, PhIm, W["w2re"], W["w2im"], ["w2re", "w2im"], Bre, Bim, ["Bre", "Bim"], "Ph")
            for g in range(32):
                pb = 2 + (g // 4) % 2
                for ri, src, sk in ((0, PhRe, "Phre"), (1, PhIm, "Phim")):
                    K.op("pe", lambda e, g=g, ri=ri, src=src, pb=pb: e.transpose(
                        PSB(pb)[:, ((g % 4) * 2 + ri) * 128:((g % 4) * 2 + ri + 1) * 128], src[:, g, :], ident_b),
                        reads=[sk, "ident_b"], writes=[pk(pb)])
                if g % 4 == 3:
                    g0 = g - 3
                    cp("act", PhiT[:, g0:g0 + 4].rearrange("p g r m -> p (g r m)"), PSB(pb), [pk(pb)], ["PhiT"])
            chk(3.3)
            outer(PhRe, PhIm, W["wre"], W["wim"], ["wre", "wim"], Bre, Bim, ["Bre", "Bim"], "Ph")
            outer(GamRe, GamIm, T["LKR"][:, :, 0:8], T["LKI"][:, :, 0:8], ["LKR", "LKI"], Cre, Cim, ["Cre", "Cim"], "Gam", neg_im=True)
            l8r = T["LKR"][:, :, 17]; l8i = T["LKI"][:, :, 17]
            dve(lambda e: e.tensor_copy(out=L4[:, 0:2, :], in_=l8r.unsqueeze(1).to_broadcast([128, 2, 32])), ["LKR"], ["L4"])
            dve(lambda e: e.tensor_copy(out=L4[:, 2, :], in_=l8i), ["LKI"], ["L4"])
            dve(lambda e: e.tensor_scalar(out=L4[:, 3, :], in0=l8i, scalar1=-1.0, scalar2=None, op0=ALU.mult), ["LKI"], ["L4"])
            mfb = masks[:, 0:128].unsqueeze(1).to_broadcast([128, 4, 128]); mrb = masks[:, 128:256].unsqueeze(1).to_broadcast([128, 4, 128])
            for g4 in range(8):
                pf_, pr_ = 4 + 2 * (g4 % 2), 5 + 2 * (g4 % 2)
                for gi in range(4):
                    g = g4 * 4 + gi
                    for d, pb in ((0, pf_), (1, pr_)):
                        K.op("pe", lambda e, g=g, gi=gi, d=d, pb=pb: e.matmul(
                            PS(pb, 128, gi * 128), lhsT=PhRe[64 * d:64 * d + 64, g, :], rhs=GamRe[64 * d:64 * d + 64, g, :], start=True, stop=False),
                            reads=["Phre", "Gamre"], writes=[pk(pb)])
                        K.op("pe", lambda e, g=g, gi=gi, d=d, pb=pb: e.matmul(
                            PS(pb, 128, gi * 128), lhsT=PhIm[64 * d:64 * d + 64, g, :], rhs=GamIm[64 * d:64 * d + 64, g, :], start=False, stop=True),
                            reads=["Phim", "Gamim"], writes=[pk(pb)])
                dve(lambda e, pf_=pf_: e.tensor_tensor(out=MT1, in0=PS(pf_).rearrange("p (g m) -> p g m", g=4), in1=mfb, op=ALU.mult), [pk(pf_), "masks"], ["PH1"])
                dve(lambda e, pr_=pr_: e.tensor_tensor(out=MT2, in0=PS(pr_).rearrange("p (g m) -> p g m", g=4), in1=mrb, op=ALU.mult), [pk(pr_), "masks"], ["PH2"])
                dve(lambda e: e.tensor_tensor(out=MT1, in0=MT1, in1=MT2, op=ALU.add), ["PH1", "PH2"], ["PH1"])
                for gi in range(4):
                    g = g4 * 4 + gi
                    dve(lambda e, g=g, gi=gi: e.scalar_tensor_tensor(out=Mi[:, g, :], in0=ident_f, scalar=Dcol[:, g:g + 1], in1=MT1[:, gi, :], op0=ALU.mult, op1=ALU.add),
                        ["PH1", "ident_f", "Dcol"], [("Mi", g)])
            if "d_Mi" in dbg_out:
                dM = A.alloc([128], F32)
                dve(lambda e: e.tensor_copy(out=dM, in_=Mi[:, 0, :]), [("Mi", 0)], ["dM"])
                K.dma("sp", dbg_out["d_Mi"], dM, reads=["dM"], writes=[("dbg", 20)])
            K.barrier()
            chk(3.4)
            A.top = p4mark
            XV = A.alloc([2, 32, NE], BF16)
            for g in range(32):
                for ri in range(2):
                    idx = g * 2 + ri
                    pa = idx % 2
                    pbk = 2 + (idx // 16) % 2
                    for d in range(2):
                        rA = U[:, g, 0:512] if d == 0 else U[:, g, 575:63:-1]
                        rB = U[:, g, 512:543] if d == 0 else U[:, g, 63:32:-1]
                        K.op("pe", lambda e, g=g, ri=ri, d=d, pa=pa, rA=rA: e.matmul(
                            PS(pa)[64 * d:64 * d + 64, :], lhsT=PhiT[:, g, ri, 64 * d:64 * d + 64], rhs=rA, start=True, stop=True),
                            reads=["PhiT", ("U", g // 8)], writes=[pk(pa)])
                        K.op("pe", lambda e, g=g, ri=ri, d=d, pbk=pbk, rB=rB, idx=idx: e.matmul(
                            PS(pbk, 31, (idx % 16) * 32)[64 * d:64 * d + 64, :], lhsT=PhiT[:, g, ri, 64 * d:64 * d + 64], rhs=rB, start=True, stop=True),
                            reads=["PhiT", ("U", g // 8)], writes=[pk(pbk)])
                    cp("act" if idx % 2 == 0 else "dve", XV[:, ri, g, 0:512], PS(pa), [pk(pa)], ["XVin"])
                    if idx % 16 == 15:
                        g0 = (idx - 15) // 2
                        cp("dve", XV[:, :, g0:g0 + 8, 512:543].rearrange("p r g c -> p g r c"),
                           PS(pbk).rearrange("p (g r c) -> p g r c", g=8, r=2)[:, :, :, 0:31], [pk(pbk)], ["XVin"])
            if "d_V" in dbg_out:
                dV = A.alloc([NE], F32)
                dve(lambda e: e.tensor_copy(out=dV, in_=XV[:, 0, 0, :]), ["XVin"], ["dV"])
                K.dma("sp", dbg_out["d_V"], dV, reads=["dV"], writes=[("dbg", 21)])
            chk(3.5)
            XVOUT = []
            dve(lambda e: e.memset(ring[:, 0, :, :], 0.0), [], [("ring", 0)])
            NSTEP = 543
            CV = 16
            rk = lambda slot: ("ring", (slot % RS) // CV)
            for e_ in range(NSTEP):
                cur = ring[:, e_ % RS, :, :]
                nxt = ring[:, (e_ + 1) % RS, :, :].rearrange("p (c r) g -> p c r g", c=2)
                dve(lambda e, cur=cur: e.tensor_tensor(out=rtmp, in0=cur, in1=L4, op=ALU.mult), [rk(e_), "L4"], ["rtmp"])
                dve(lambda e: e.tensor_tensor(out=rC, in0=rtmp[:, 0:2, :], in1=rtmp[:, 3:1:-1, :], op=ALU.add), ["rtmp"], ["rC"])
                dve(lambda e, nxt=nxt, e_=e_: e.tensor_tensor(out=nxt, in0=rC.unsqueeze(1).to_broadcast([128, 2, 2, 32]),
                                                              in1=XV[:, :, :, e_].unsqueeze(1).to_broadcast([128, 2, 2, 32]), op=ALU.add), ["rC", "XVin"], [rk(e_ + 1)])
                if (e_ + 1) % CV == 0 or e_ == NSTEP - 1:
                    n = CV if (e_ + 1) % CV == 0 else (NSTEP % CV)
                    e0 = e_ + 1 - n
                    k = 0
                    while k < n:
                        rs = (e0 + 1 + k) % RS
                        run = min(n - k, RS - rs)
                        cp("act", XV[:, :, :, e0 + k:e0 + k + run], ring[:, rs:rs + run, 0:2, :].rearrange("p c r g -> p r g c"),
                           sorted(set([rk(rs + q) for q in range(run)])), [("XVout", e0 // CV, k)])
                        XVOUT.append(("XVout", e0 // CV, k))
                        k += run
            if "d_X" in dbg_out:
                dX = A.alloc([NE], F32)
                dve(lambda e: e.tensor_copy(out=dX, in_=XV[:, 0, 0, :]), XVOUT + ["XVin"], ["dX"])
                K.dma("sp", dbg_out["d_X"], dX, reads=["dX"], writes=[("dbg", 22)])
            K.barrier()
            chk(3.6)
            A2 = Arena(arena_t); A2.top = ovl
            Ylj = A2.alloc([32, 128], F32)
            Ytj = A2.alloc([8, 512], F32)
            assert A2.top <= p4mark
            y5_r = y5_scr.rearrange("(c s) f -> c s f", s=8)
            for j in range(4):
                for g in range(32):
                    pA = (g % 2) * 3
                    K.op("pe", lambda e, g=g, pA=pA, j=j: e.matmul(PS(pA, 128), lhsT=Mi[:, g, :], rhs=U[:, g, 32 + 128 * j:32 + 128 * (j + 1)], start=True, stop=True),
                         reads=[("Mi", g), ("U", g // 8)], writes=[pk(pA)])
                    for d in range(2):
                        pD = pA + 1 + d
                        for ri, Gm in ((0, GamRe), (1, GamIm)):
                            if d == 0:
                                rhs = XV[0:64, ri, g, 31 + 128 * j:31 + 128 * (j + 1)]
                            else:
                                hi = 542 - 128 * j
                                lo = hi - 128
                                rhs = XV[64:128, ri, g, hi:lo:-1]
                            K.op("pe", lambda e, g=g, pD=pD, d=d, Gm=Gm, rhs=rhs, ri=ri: e.matmul(
                                PS(pD, 128), lhsT=Gm[64 * d:64 * d + 64, g, :], rhs=rhs, start=(ri == 0), stop=(ri == 1)),
                                reads=["Gamre", "Gamim", "XVin"] + XVOUT, writes=[pk(pD)])
                    cp("act", Ylj[:, g, :], PS(pA, 128), [pk(pA)], [("Ylj", g)])
                    K.op("dve", lambda e, g=g, pA=pA: e.tensor_tensor(out=Ylj[:, g, :], in0=Ylj[:, g, :], in1=PS(pA + 1, 128), op=ALU.add),
                         reads=[("Ylj", g), pk(pA + 1)], writes=[("Ylj", g)])
                    K.op("dve", lambda e, g=g, pA=pA: e.tensor_tensor(out=Ylj[:, g, :], in0=Ylj[:, g, :], in1=PS(pA + 2, 128), op=ALU.add),
                         reads=[("Ylj", g), pk(pA + 2)], writes=[("Ylj", g)])
                if j == 0 and "d_Y" in dbg_out:
                    K.dma("sp", dbg_out["d_Y"], Ylj[:, 0, :], reads=[("Ylj", 0)], writes=[("dbg", 7)])
                for g4 in range(8):
                    pb = 6 + g4 % 2
                    for gi in range(4):
                        g = g4 * 4 + gi
                        K.op("pe", lambda e, g=g, gi=gi, pb=pb: e.transpose(PS(pb, 128, gi * 128), Ylj[:, g, :], ident_f),
                             reads=[("Ylj", g), "ident_f"], writes=[pk(pb)])
                    cp("act" if g4 % 2 == 0 else "dve", Ytj[:, :, g4 * 64:(g4 + 1) * 64].rearrange("p s (g h) -> p g s h", g=4),
                       PS(pb).rearrange("p (g s h) -> p g s h", g=4, s=8), [pk(pb)], ["Ytj"])
                K.dma("sp", y5_r[128 * j:128 * (j + 1), :, :], Ytj, reads=["Ytj"], writes=[("y5", j)])
            K.barrier()

            chk(4)
            A.top = p2start
            G = A.alloc([32, 32], F32)
            pjunk = A.alloc([1024], BF16)
            p5keep = A.top
            pjunk2 = A.alloc([1024], BF16)
            w_out_bf = A.alloc([8, 1024], BF16)
            w_glu_bf = A.alloc([4, 512], BF16)
            lgall = A.alloc([32, 36], F32)
            p5tmp = A.top
            D2 = lambda shape, dt: [A.alloc(shape, dt) for _ in range(4)]
            D4 = lambda shape, dt: [A.alloc(shape, dt) for _ in range(8)]
            Yt = D4([512], F32); fnTt5 = D4([4, 128], BF16)
            ygf = D2([512], F32); ygb = D2([512], BF16); ygT = D2([4, 128], BF16)
            sg = D2([512], F32); y2 = D2([512], F32); y2n = D2([512], BF16); y2nT = D2([4, 128], BF16)
            xr = D4([1024], F32); x1 = D2([1024], F32)
            xs2 = D2([1024], BF16); tT = D2([8, 128], BF16); tTf = D2([8, 128], F32)
            pstat = A.alloc([32, 8], F32)
            w_out_r = w_out.rearrange("(kc p) n -> p kc n", p=128)
            for kc in range(8):
                K.dma("pool", w_out_bf[:, kc, :], w_out_r[:, kc, :], writes=["w_out_bf"])
            K.dma("pool", w_glu_bf, w_glu.rearrange("(kc p) n -> p kc n", p=128), writes=["w_glu_bf"])
            for kc in range(8):
                K.op("dve", lambda e, kc=kc: e.tensor_scalar(out=w_out_bf[:, kc, :], in0=w_out_bf[:, kc, :], scalar1=cols[:, 80 + kc:81 + kc], scalar2=None, op0=ALU.mult),
                     reads=["w_out_bf", "cols"], writes=["w_out_bf"])
            y_r = y.rearrange("(c s) d -> s c d", s=8)
            x1_r = x1_scr.rearrange("(c s) d -> s c d", s=8)
            y5_sr = y5_scr.rearrange("(c s) f -> s c f", s=8)
            pj = [pjunk, pjunk2, A.alloc([1024], BF16), A.alloc([1024], BF16)]

            def p5_loads(t_):
                j_, s_, b_ = t_ // 8, t_ % 8, t_ % 8
                K.dma("sp", Yt[b_], y5_sr[s_, j_ * 128:(j_ + 1) * 128, :], reads=[("y5", j_)], writes=[("Yt", b_)])
                K.dma("sp", fnTt5[b_], fT_scr[t_], reads=[("fTscr", t_)], writes=[("fnTt5", b_)])
                K.dma("sp", xr[b_], x_r[s_, j_ * 128:(j_ + 1) * 128, :], writes=[("xr", b_)])
            def p5_tile(tl):
                j, s = tl // 8, tl % 8
                tl = j * 8 + s
                b2 = tl % 4
                bT, bG, bO = 2 * b2, 2 * b2 + 1, 2 * b2 + 1
                b4 = tl % 8
                b3 = tl % 4
                K.op("act", lambda e, b2=b2, b4=b4: e.activation(out=ygf[b2], in_=Yt[b4], func=AF.Gelu_apprx_tanh), reads=[("Yt", b4)], writes=[("ygf", b2)])
                K.op("pool", lambda e, b2=b2: e.tensor_copy(out=ygb[b2], in_=ygf[b2]), reads=[("ygf", b2)], writes=[("ygb", b2)])
                yield
                for fb in range(4):
                    K.op("pe", lambda e, fb=fb, b2=b2, bT=bT: e.transpose(PSB(bT)[:, fb * 128:(fb + 1) * 128], ygb[b2][:, fb * 128:(fb + 1) * 128], ident_b),
                         reads=[("ygb", b2), "ident_b"], writes=[pk(bT)])
                cp("dve", ygT[b2].rearrange("p f t -> p (f t)"), PSB(bT)[:, 0:512], [pk(bT)], [("ygT", b2)])
                yield
                for fb in range(4):
                    K.op("pe", lambda e, fb=fb, b2=b2, bG=bG: e.matmul(PS(bG), lhsT=ygT[b2][:, fb, :], rhs=w_glu_bf[:, fb, :], start=(fb == 0), stop=(fb == 3)),
                         reads=[("ygT", b2), "w_glu_bf"], writes=[pk(bG)])
                K.op("act", lambda e, b2=b2, bG=bG: e.activation(out=sg[b2], in_=PS(bG), func=AF.Sigmoid), reads=[pk(bG)], writes=[("sg", b2)])
                K.op("dve", lambda e, b2=b2: e.tensor_tensor(out=y2[b2], in0=ygf[b2], in1=sg[b2], op=ALU.mult), reads=[("ygf", b2), ("sg", b2)], writes=[("y2", b2)])
                yield
                rms_prep(100 + tl, ("y2", b2), y2[b2], 128, 512.0, pstat[:, tl, 0:4], pj[b2][:, 0:512], y2n[b2], ("y2n", b2), junk_key=("pj", b2))
                yield
                for fb in range(4):
                    K.op("pe", lambda e, fb=fb, b2=b2, bT=bT: e.transpose(PSB(bT)[:, 512 + fb * 128:512 + (fb + 1) * 128], y2n[b2][:, fb * 128:(fb + 1) * 128], ident_b),
                         reads=[("y2n", b2), "ident_b"], writes=[pk(bT)])
                cp("dve", y2nT[b2].rearrange("p f t -> p (f t)"), PSB(bT)[:, 512:1024], [pk(bT)], [("y2nT", b2)])
                yield
                for hf in range(2):
                    pb = bO
                    for kb in range(8):
                        lhsT = y2nT[b2][:, kb, :] if kb < 4 else fnTt5[b4][:, kb - 4, :]
                        K.op("pe", lambda e, hf=hf, kb=kb, pb=pb, lhsT=lhsT: e.matmul(
                            PS(pb), lhsT=lhsT, rhs=w_out_bf[:, kb, hf * 512:(hf + 1) * 512], start=(kb == 0), stop=(kb == 7)),
                            reads=[("y2nT", b2), ("fnTt5", b4), "w_out_bf"], writes=[pk(pb)])
                    K.op("dve", lambda e, hf=hf, pb=pb, b2=b2: e.tensor_tensor(out=x1[b2][:, hf * 512:(hf + 1) * 512], in0=PS(pb), in1=g1_bc[:, hf * 512:(hf + 1) * 512], op=ALU.mult),
                         reads=[pk(pb), "g1_bc"], writes=[("x1", b2)])
                yield
                K.op("pool", lambda e, b2=b2, b4=b4: e.tensor_tensor(out=x1[b2], in0=x1[b2], in1=xr[b4], op=ALU.add), reads=[("x1", b2), ("xr", b4)], writes=[("x1", b2)])
                K.dma("sp", x1_r[s, j * 128:(j + 1) * 128, :], x1[b2], reads=[("x1", b2)], writes=[("x1scr", tl)])
                if tl == 0 and "d_x1" in dbg_out:
                    K.dma("sp", dbg_out["d_x1"], x1[0], reads=[("x1", 0)], writes=[("dbg", 8)])
                yield
                rms_prep(200 + tl, ("x1", b2), x1[b2], 128, 1024.0, pstat[:, tl, 4:8], pj[b2], xs2[b2], ("xs2", b2), junk_key=("pj", b2))
                yield
                for kc in range(8):
                    K.op("pe", lambda e, kc=kc, b2=b2, bT=bT: e.transpose(PSB(bT)[:, kc * 128:(kc + 1) * 128], xs2[b2][:, kc * 128:(kc + 1) * 128], ident_b),
                         reads=[("xs2", b2), "ident_b"], writes=[pk(bT)])
                psv = PSB(bT).rearrange("p (k t) -> p k t", k=8)
                K.op("dve", lambda e, psv=psv, b3=b3: e.tensor_tensor(out=tTf[b3], in0=psv, in1=vecs[:, 4, :].unsqueeze(2).to_broadcast([128, 8, 128]), op=ALU.mult),
                     reads=[pk(bT), "vecs"], writes=[("tTf", b3)])
                yield
                K.op("pool", lambda e, b3=b3: e.tensor_tensor(out=tTf[b3], in0=tTf[b3], in1=vecs[:, 5, :].unsqueeze(2).to_broadcast([128, 8, 128]), op=ALU.add),
                     reads=[("tTf", b3), "vecs"], writes=[("tTf", b3)])
                cp("act", tT[b2], tTf[b3], [("tTf", b3)], [("tT", b2)])
                K.dma("sp", tT_scr[:, :, tl * 128:(tl + 1) * 128].rearrange("k p t -> p k t"), tT[b2], reads=[("tT", b2)], writes=[("tTscr", tl)])
                yield
                for kc in range(8):
                    K.op("pe", lambda e, kc=kc, b3=b3, bG=bG: e.matmul(PS(bG, 36), lhsT=tTf[b3][:, kc, :], rhs=wrt[:, kc, :], start=(kc == 0), stop=(kc == 7)),
                         reads=[("tTf", b3), "wrt"], writes=[pk(bG)])
                K.op("dve", lambda e, tl=tl, bG=bG: e.tensor_tensor(out=lgall[:, tl, :], in0=PS(bG, 36), in1=brt_bc, op=ALU.add), reads=[pk(bG), "brt_bc"], writes=["lgall"])

            for t_ in range(4):
                p5_loads(t_)
            for pr in range(8):
                if pr + 1 < 8:
                    for t_ in range(4):
                        p5_loads(4 * pr + 4 + t_)
                gens = [p5_tile(4 * pr + q) for q in range(4)]
                alive = [True] * 4
                while any(alive):
                    for gi_, g_ in enumerate(gens):
                        if alive[gi_]:
                            try:
                                next(g_)
                            except StopIteration:
                                alive[gi_] = False
            K.barrier()
            A.top = p5tmp
            NTL = 32
            ra = lambda n: A.alloc([NTL, n], F32)
            gmax = ra(1); sel = ra(4); exg = ra(4); gsum = ra(1); gw = ra(1); t8 = ra(32); es_ = ra(8); m1 = ra(1); mk1 = ra(8)
            es2 = ra(8); m2 = ra(1); mk2 = ra(8); dd = ra(1); exd = ra(1); den = ra(1); w1 = ra(1); w2 = ra(1); comb = ra(8); cb2 = ra(8)
            gl3 = lgall[:, :, 0:4]; el4 = lgall[:, :, 4:36].rearrange("p t (g e) -> p t g e", g=4)

            def R(fn, eng="dve"):
                K.op(eng, fn, reads=["rt", "lgall"], writes=["rt"])
            bc4 = lambda a: a[:, :, 0:1].to_broadcast([128, NTL, 4])
            bc8 = lambda a: a[:, :, 0:1].to_broadcast([128, NTL, 8])
            R(lambda e: e.tensor_reduce(out=gmax[:, :, 0], in_=gl3, axis=AX.X, op=ALU.max))
            R(lambda e: e.tensor_tensor(out=sel, in0=gl3, in1=bc4(gmax), op=ALU.is_equal))
            R(lambda e: e.tensor_tensor(out=exg, in0=gl3, in1=bc4(gmax), op=ALU.subtract))
            R(lambda e: e.activation(out=exg, in_=exg, func=AF.Exp), eng="act")
            R(lambda e: e.tensor_reduce(out=gsum[:, :, 0], in_=exg, axis=AX.X, op=ALU.add))
            R(lambda e: e.reciprocal(out=gw, in_=gsum))
            R(lambda e: e.tensor_tensor(out=t8.rearrange("p t (g e) -> p t g e", g=4), in0=el4, in1=sel.unsqueeze(3).to_broadcast([128, NTL, 4, 8]), op=ALU.mult))
            R(lambda e: e.tensor_reduce(out=es_, in_=t8.rearrange("p t (g e) -> p t e g", g=4), axis=AX.X, op=ALU.add))
            R(lambda e: e.tensor_reduce(out=m1[:, :, 0], in_=es_, axis=AX.X, op=ALU.max))
            R(lambda e: e.tensor_tensor(out=mk1, in0=es_, in1=bc8(m1), op=ALU.is_equal))
            R(lambda e: e.scalar_tensor_tensor(out=es2, in0=mk1, scalar=-1e30, in1=es_, op0=ALU.mult, op1=ALU.add))
            R(lambda e: e.tensor_reduce(out=m2[:, :, 0], in_=es2, axis=AX.X, op=ALU.max))
            R(lambda e: e.tensor_tensor(out=mk2, in0=es2, in1=bc8(m2), op=ALU.is_equal))
            R(lambda e: e.tensor_tensor(out=dd, in0=m2, in1=m1, op=ALU.subtract))
            R(lambda e: e.activation(out=exd, in_=dd, func=AF.Exp), eng="act")
            R(lambda e: e.tensor_scalar(out=den, in0=exd, scalar1=1.0, scalar2=None, op0=ALU.add))
            R(lambda e: e.reciprocal(out=w1, in_=den))
            R(lambda e: e.tensor_tensor(out=w1, in0=w1, in1=gw, op=ALU.mult))
            R(lambda e: e.tensor_tensor(out=w2, in0=w1, in1=exd, op=ALU.mult))
            R(lambda e: e.tensor_tensor(out=comb, in0=mk1, in1=bc8(w1), op=ALU.mult))
            R(lambda e: e.tensor_tensor(out=cb2, in0=mk2, in1=bc8(w2), op=ALU.mult))
            R(lambda e: e.tensor_tensor(out=comb, in0=comb, in1=cb2, op=ALU.add))
            K.op("dve", lambda e: e.tensor_tensor(out=G.rearrange("p t (g e) -> p t g e", g=4),
                                                  in0=sel.unsqueeze(3).to_broadcast([128, NTL, 4, 8]),
                                                  in1=comb.unsqueeze(2).to_broadcast([128, NTL, 4, 8]), op=ALU.mult),
                 reads=["rt"], writes=[("G", tl_) for tl_ in range(32)])
            if "d_G" in dbg_out:
                K.dma("sp", dbg_out["d_G"], G[:, 0, :], reads=[("G", 0)], writes=[("dbg", 9)])
            K.barrier()

            chk(5)
            A.top = p5keep
            tTh2 = [A.alloc([8, 2048], BF16) for _ in range(2)]
            acc = A.alloc([16, 1024], F32)
            wg = [A.alloc([8, 256], BF16) for _ in range(2)]
            wu = [A.alloc([8, 256], BF16) for _ in range(2)]
            wd = [A.alloc([2, 1024], BF16) for _ in range(2)]
            sl = [A.alloc([512], F32) for _ in range(2)]
            aT = [A.alloc([2, 512], BF16) for _ in range(2)]
            ob = [A.alloc([1024], F32) for _ in range(2)]
            ostat = A.alloc([32, 4], F32)
            ojunk = pjunk
            g2b = A.alloc([1024], BF16)
            K.op("dve", lambda e: e.tensor_copy(out=g2b, in_=g2_bc), reads=["g2_bc"], writes=["g2b"])
            def acc_load(hh, tt):
                tl = hh * 16 + tt
                j, s = tl // 8, tl % 8
                K.dma("act", acc[:, tt, :], x1_r[s, j * 128:(j + 1) * 128, :], reads=[("x1scr", tl)], writes=[("acc", tt)])

            for hh in range(2):
                K.dma("sp", tTh2[hh], tT_scr[:, :, hh * 2048:(hh + 1) * 2048].rearrange("k p t -> p k t"),
                      reads=[("tTscr", tl) for tl in range(hh * 16, hh * 16 + 16)], writes=[("tTh", hh)])
            for tt in range(16):
                acc_load(0, tt)

            def wload(ex_, wi):
                b2 = wi % 2
                K.dma("pool", wg[b2], w_gate[ex_].rearrange("(kc p) f -> p kc f", p=128), writes=[("wg", b2)])
                K.dma("pool", wu[b2], w_up[ex_].rearrange("(kc p) f -> p kc f", p=128), writes=[("wu", b2)])
                K.dma("pool", wd[b2], w_down[ex_].rearrange("(fb p) n -> p fb n", p=128), writes=[("wd", b2)])
                K.op("pool", lambda e, b2=b2: e.tensor_tensor(out=wd[b2], in0=wd[b2], in1=g2b.unsqueeze(1).to_broadcast([128, 2, 1024]), op=ALU.mult),
                     reads=[("wd", b2), "g2b"], writes=[("wd", b2)])

            def hgu(hh, ex_, tg, it_):
                b2 = (hh * 32 + ex_) % 2
                ab = it_ % 2
                tTh = tTh2[hh]
                for fb in range(2):
                    pg, pu_ = 0 + fb, 2 + fb
                    for kc in range(8):
                        K.op("pe", lambda e, kc=kc, fb=fb, b2=b2, tg=tg, pg=pg, tTh=tTh: e.matmul(
                            PS(pg), lhsT=wg[b2][:, kc, fb * 128:(fb + 1) * 128], rhs=tTh[:, kc, tg * 512:(tg + 1) * 512], start=(kc == 0), stop=(kc == 7)),
                            reads=[("wg", b2), ("tTh", hh)], writes=[pk(pg)])
                    for kc in range(8):
                        K.op("pe", lambda e, kc=kc, fb=fb, b2=b2, tg=tg, pu_=pu_, tTh=tTh: e.matmul(
                            PS(pu_), lhsT=wu[b2][:, kc, fb * 128:(fb + 1) * 128], rhs=tTh[:, kc, tg * 512:(tg + 1) * 512], start=(kc == 0), stop=(kc == 7)),
                            reads=[("wu", b2), ("tTh", hh)], writes=[pk(pu_)])
                    K.op("act", lambda e, fb=fb, pg=pg: e.activation(out=sl[fb], in_=PS(pg), func=AF.Silu), reads=[pk(pg)], writes=[("sl", fb)])
                    K.op("dve", lambda e, fb=fb, pu_=pu_, ab=ab: e.tensor_tensor(out=aT[ab][:, fb, :], in0=sl[fb], in1=PS(pu_), op=ALU.mult),
                         reads=[("sl", fb), pk(pu_)], writes=[("aT", ab, fb)])

            def down(hh, ex_, tg, it_):
                b2 = (hh * 32 + ex_) % 2
                ab = it_ % 2
                for t4 in range(4):
                    tt = tg * 4 + t4
                    tl = hh * 16 + tt
                    for hf in range(2):
                        po = 4 + (t4 * 2 + hf) % 4
                        for fb in range(2):
                            K.op("pe", lambda e, fb=fb, hf=hf, po=po, ab=ab, t4=t4, b2=b2: e.matmul(
                                PS(po), lhsT=aT[ab][:, fb, t4 * 128:(t4 + 1) * 128], rhs=wd[b2][:, fb, hf * 512:(hf + 1) * 512], start=(fb == 0), stop=(fb == 1)),
                                reads=[("aT", ab, 0), ("aT", ab, 1), ("wd", b2)], writes=[pk(po)])
                        K.op("dve", lambda e, po=po, tt=tt, hf=hf, tl=tl, ex_=ex_: e.scalar_tensor_tensor(
                            out=acc[:, tt, hf * 512:(hf + 1) * 512], in0=PS(po), scalar=G[:, tl, ex_:ex_ + 1], in1=acc[:, tt, hf * 512:(hf + 1) * 512],
                            op0=ALU.mult, op1=ALU.add), reads=[pk(po), ("G", tl), ("acc", tt)], writes=[("acc", tt)])

            def final_norm(hh, tt):
                tl = hh * 16 + tt
                j, s = tl // 8, tl % 8
                b2 = tt % 2
                K.op("act", lambda e, tt=tt, tl=tl: e.activation(out=ojunk, in_=acc[:, tt, :], func=AF.Square, accum_out=ostat[:, tl, 0:1]),
                     reads=[("acc", tt)], writes=["ojunk", ("ostat", tl)])
                K.op("act", lambda e, tl=tl: e.activation(out=ostat[:, tl, 1:2], in_=ostat[:, tl, 0:1], func=AF.Sqrt, scale=1.0 / 1024.0, bias=small[:, 0:1]),
                     reads=[("ostat", tl), "small"], writes=[("ostat", tl)])
                K.op("dve", lambda e, tl=tl: e.reciprocal(out=ostat[:, tl, 2:3], in_=ostat[:, tl, 1:2]), reads=[("ostat", tl)], writes=[("ostat", tl)])
                K.op("dve", lambda e, tt=tt, tl=tl, b2=b2: e.scalar_tensor_tensor(out=ob[b2], in0=acc[:, tt, :], scalar=ostat[:, tl, 2:3], in1=fin_bc, op0=ALU.mult, op1=ALU.mult),
                     reads=[("acc", tt), ("ostat", tl), "fin_bc"], writes=[("ob", b2)])
                K.dma("sp", y_r[s, j * 128:(j + 1) * 128, :], ob[b2], reads=[("ob", b2)], writes=[("y", tl)])

            items = [(hh, ex_, tg) for hh in range(2) for ex_ in range(32) for tg in range(4)]
            wload(0, 0)
            hgu(0, 0, 0, 0)
            for it_ in range(len(items)):
                hh, ex_, tg = items[it_]
                if it_ + 1 < len(items):
                    nh, ne, ntg = items[it_ + 1]
                    if ntg == 0:
                        wload(ne, nh * 32 + ne)
                    hgu(nh, ne, ntg, it_ + 1)
                down(hh, ex_, tg, it_)
                if ex_ == 31:
                    for t4 in range(4):
                        final_norm(hh, tg * 4 + t4)
                        if hh == 0:
                            acc_load(1, tg * 4 + t4)
        except _Stop:
            pass
        K.finish()

        @block.tensor
        def _(e):
            for f in K.eng["pe"]["prog"]:
                f(e)

        @block.scalar
        def _(e):
            for f in K.eng["act"]["prog"]:
                f(e)

        @block.vector
        def _(e):
            for f in K.eng["dve"]["prog"]:
                f(e)

        @block.gpsimd
        def _(e):
            for f in K.eng["pool"]["prog"]:
                f(e)

        @block.sync
        def _(e):
            for f in K.eng["sp"]["prog"]:
                f(e)
    return nc


_CONST = None


def _consts():
    global _CONST
    if _CONST is not None:
        return _CONST
    c = {}
    c["c_ident"] = np.eye(128, dtype=np.float32)
    s_in = np.arange(128) // 16
    mf = (s_in[:, None] <= s_in[None, :]).astype(np.float32)
    mr = (s_in[:, None] >= s_in[None, :]).astype(np.float32)
    c["c_masks"] = np.concatenate([mf, mr], axis=1)
    kid = np.zeros((128, 18), np.float32)
    s = np.arange(8)
    kid[0:64, 0:8] = s + 1
    kid[64:128, 0:8] = 8 - s
    kid[:, 8:16] = 8 - kid[:, 0:8]
    kid[:, 16] = 1
    kid[:, 17] = 8
    c["c_kidx"] = kid
    rep = np.zeros((16, 128), np.float32)
    for p in range(128):
        rep[p % 16, p] = 1.0
    c["c_rep16"] = rep
    jj = np.arange(128)
    ang = 2 * np.pi * np.outer(jj, jj) / 128.0
    sc = 1.0 / math.sqrt(L * 128.0)
    c["c_dftc"] = np.concatenate([np.cos(ang) * sc, np.sin(ang) * sc], axis=1).astype(np.float32)
    ii = np.arange(128)
    tab = np.zeros((128, 2, 64, 128), dtype=ml_dtypes.bfloat16)
    for s_ in range(8):
        for s2 in range(8):
            mm = ((8 * ii[:, None] + s_) * (8 * ii[None, :] + s2)) % L
            a = 2 * np.pi * mm / L
            tab[:, 0, s_ * 8 + s2, :] = np.cos(a).astype(ml_dtypes.bfloat16)
            tab[:, 1, s_ * 8 + s2, :] = np.sin(a).astype(ml_dtypes.bfloat16)
    c["c_tab"] = tab
    _CONST = c
    return c


_NC_CACHE = {}


def _get_nc(dbg=()):
    key = tuple(dbg)
    if key not in _NC_CACHE:
        _NC_CACHE[key] = build_nc(dbg)
    return _NC_CACHE[key]


def make_in_maps(inputs, cores):
    c = _consts()
    f = lambda a: np.ascontiguousarray(np.asarray(a, dtype=np.float32))
    shared = {
        "w_ada": f(inputs["w_ada"][0]), "b_ada": f(inputs["b_ada"][0]), "w_in": f(inputs["w_in"][0]),
        "lam_re": f(inputs["s5_lam_re"][0]), "lam_im": f(inputs["s5_lam_im"][0]),
        "b_re": f(inputs["s5_b_re"][0]), "b_im": f(inputs["s5_b_im"][0]),
        "c_re": f(inputs["s5_c_re"][0]), "c_im": f(inputs["s5_c_im"][0]),
        "log_step": f(inputs["s5_log_step"][0]), "s5_d": f(inputs["s5_d"][0]),
        "w_glu": f(inputs["s5_w_glu"][0]), "four_w": f(inputs["fourier_w"][0]), "w_out": f(inputs["w_out"][0]),
        "w_rt": f(np.concatenate([np.asarray(inputs["moe_w_group"][0]), np.asarray(inputs["moe_w_router"][0]).reshape(D, 32)], axis=1)),
        "b_rt": f(np.concatenate([np.asarray(inputs["moe_b_group"][0]), np.asarray(inputs["moe_b_router"][0]).reshape(32)])),
        "w_gate": f(inputs["moe_w_gate"][0]), "w_up": f(inputs["moe_w_up"][0]), "w_down": f(inputs["moe_w_down"][0]),
        "fin_g": f(inputs["final_norm_g"]),
    }
    shared.update(c)
    maps = []
    for b in cores:
        rows = np.zeros((128, 128), np.float32)
        rows[0:8] = np.asarray(inputs["c"][b]).reshape(8, 128)
        rows[8:16] = np.asarray(inputs["c_ctx"]).reshape(8, 128)
        rows[16:24] = np.asarray(inputs["norm1_g"][0]).reshape(8, 128)
        rows[24:32] = np.asarray(inputs["norm2_g"][0]).reshape(8, 128)
        rows[32:80] = np.asarray(inputs["b_ada"][0]).reshape(48, 128)
        rows[80:84] = np.asarray(inputs["mix_norm_s5_g"][0]).reshape(4, 128)
        rows[84:88] = np.asarray(inputs["mix_norm_f_g"][0]).reshape(4, 128)
        m = dict(shared)
        m["x"] = f(inputs["x"][b])
        m["ctx"] = f(inputs["ctx"][b])
        m["rows_in"] = rows
        maps.append(m)
    return maps


def kernel(**inputs):
    nc = _get_nc()
    maps = make_in_maps(inputs, list(range(8)))
    res = run_bass_kernel_spmd(nc, maps, core_ids=list(range(8)))
    return np.stack([np.asarray(r["y"], dtype=np.float32) for r in res.results], axis=0)
```

```python
import math
import numpy as np
import ml_dtypes
import concourse.bass as bass
import concourse.mybir as mybir
from concourse.bass_utils import run_bass_kernel_spmd

F32 = mybir.dt.float32
BF16 = mybir.dt.bfloat16
I32 = mybir.dt.int32
AF = mybir.ActivationFunctionType
ALU = mybir.AluOpType
AX = mybir.AxisListType

L = 4096
D = 1024
NCH = 512
NCX = 32
NE = 544
EPS = 1e-6
TWO_PI = 2.0 * math.pi
ARENA = 106000

DEBUG = False
SELF_ORDERED = ("pe",)


class Sched:
    def __init__(self, nc, sems, dma_sems):
        self.nc = nc
        self.eng = {n: dict(prog=[], sem=sems[n], count=0, known={}) for n in ("pe", "act", "dve", "pool", "sp")}
        self.dma_sems = dma_sems
        self.dma_counts = [0] * len(dma_sems)
        self.dma_rr = 0
        self.dma_rr_sw = 0
        self.last_w = {}
        self.readers = {}

    def _deps(self, reads, writes):
        toks = []
        for k in reads:
            t = self.last_w.get(k)
            if t:
                toks.append(t)
        for k in writes:
            t = self.last_w.get(k)
            if t:
                toks.append(t)
            toks += list(self.readers.get(k, {}).values())
        return toks

    def _waits(self, ename, toks):
        E = self.eng[ename]
        need = {}
        for (sid, sem, val, src) in toks:
            if src == ename and ename in SELF_ORDERED:
                continue
            if E["known"].get(sid, 0) >= val:
                continue
            if need.get(sid, (None, 0))[1] < val:
                need[sid] = (sem, val)
        out = []
        for sid, (sem, val) in need.items():
            E["known"][sid] = val
            out.append((sem, val))
        return out

    def _commit(self, tok, reads, writes):
        for k in writes:
            self.last_w[k] = tok
            self.readers[k] = {}
        for k in reads:
            d = self.readers.setdefault(k, {})
            o = d.get(tok[0])
            if o is None or o[2] < tok[2]:
                d[tok[0]] = tok

    def op(self, ename, fn, reads=(), writes=()):
        E = self.eng[ename]
        waits = self._waits(ename, self._deps(reads, writes))
        E["count"] += 1
        val = E["count"]
        sem = E["sem"]

        def run(e, fn=fn, waits=waits, sem=sem):
            for (s, v) in waits:
                e.wait_ge(s, v)
            fn(e).then_inc(sem, 1)
        E["prog"].append(run)
        tok = (id(sem), sem, val, ename)
        self._commit(tok, reads, writes)
        return tok

    def dma(self, q, out, in_, reads=(), writes=(), **kw):
        E = self.eng[q]
        nsw = 12
        nhw = len(self.dma_sems) - nsw
        if q == "pool":
            i = nhw + self.dma_rr_sw
            self.dma_rr_sw = (self.dma_rr_sw + 1) % nsw
        else:
            i = self.dma_rr
            self.dma_rr = (i + 1) % nhw
        sem = self.dma_sems[i]
        prev = self.dma_counts[i]
        toks = self._deps(reads, writes)
        if prev > 0:
            toks.append((id(sem), sem, 16 * prev, "dma"))
        waits = self._waits(q, toks)
        self.dma_counts[i] += 1
        val = 16 * self.dma_counts[i]

        def run(e, waits=waits, sem=sem, out=out, in_=in_, kw=kw):
            for (s, v) in waits:
                e.wait_ge(s, v)
            e.dma_start(out=out, in_=in_, **kw).then_inc(sem, 16)
        E["prog"].append(run)
        tok = (id(sem), sem, val, "dma")
        self._commit(tok, reads, writes)
        return tok

    def barrier(self):
        toks = []
        for n, E in self.eng.items():
            if E["count"] > 0:
                toks.append((id(E["sem"]), E["sem"], E["count"], "bar"))
        for i, s in enumerate(self.dma_sems):
            if self.dma_counts[i] > 0:
                toks.append((id(s), s, 16 * self.dma_counts[i], "bar"))
        for n, E in self.eng.items():
            waits = self._waits(n, toks)
            if waits:
                def run(e, waits=waits):
                    for (s, v) in waits:
                        e.wait_ge(s, v)
                E["prog"].append(run)

    def finish(self):
        toks = []
        for i, s in enumerate(self.dma_sems):
            if self.dma_counts[i] > 0:
                toks.append((id(s), s, 16 * self.dma_counts[i], "bar"))
        waits = self._waits("sp", toks)

        def run(e, waits=waits):
            for (s, v) in waits:
                e.wait_ge(s, v)
        self.eng["sp"]["prog"].append(run)


class Arena:
    def __init__(self, ap):
        self.ap = ap
        self.top = 0

    def alloc(self, shape, dtype, parts=128):
        n = 1
        for d in shape:
            n *= d
        nb = n * (2 if dtype == BF16 else 4)
        nb = (nb + 63) // 64 * 64
        off = self.top
        self.top += nb // 2
        assert self.top <= ARENA, f"arena overflow {self.top}"
        v = self.ap[0:parts, off:off + nb // 2]
        if dtype != BF16:
            v = v.bitcast(dtype)
        v = v[:, 0:n]
        if len(shape) == 2:
            return v.rearrange("p (a b) -> p a b", a=shape[0])
        if len(shape) == 3:
            return v.rearrange("p (a b c) -> p a b c", a=shape[0], b=shape[1])
        if len(shape) == 4:
            return v.rearrange("p (a b c d) -> p a b c d", a=shape[0], b=shape[1], c=shape[2])
        return v


def build_nc(dbg=(), stop_after=99):
    nc = bass.Bass("TRN2", target_bir_lowering=False)

    def din(name, shape, dt=F32):
        return nc.dram_tensor(name, list(shape), dt, kind="ExternalInput").ap()

    x = din("x", [L, D])
    ctx = din("ctx", [256, D])
    rows_in = din("rows_in", [128, 128])
    w_ada = din("w_ada", [D, 6 * D])
    b_ada = din("b_ada", [6 * D])
    w_in = din("w_in", [D, D])
    lam_re = din("lam_re", [2, 32, 64])
    lam_im = din("lam_im", [2, 32, 64])
    b_re = din("b_re", [2, 32, 64, 16])
    b_im = din("b_im", [2, 32, 64, 16])
    c_re = din("c_re", [2, 32, 16, 64])
    c_im = din("c_im", [2, 32, 16, 64])
    log_step = din("log_step", [2, 32])
    s5_d = din("s5_d", [512])
    w_glu = din("w_glu", [512, 512])
    four_w = din("four_w", [4, 128, 128])
    w_out = din("w_out", [D, D])
    w_rt = din("w_rt", [D, 36])
    b_rt = din("b_rt", [36])
    w_gate = din("w_gate", [32, D, 256])
    w_up = din("w_up", [32, D, 256])
    w_down = din("w_down", [32, 256, D])
    fin_g = din("fin_g", [D])
    c_ident = din("c_ident", [128, 128])
    c_masks = din("c_masks", [128, 256])
    c_kidx = din("c_kidx", [128, 18])
    c_rep16 = din("c_rep16", [16, 128])
    c_dftc = din("c_dftc", [128, 256])
    c_tab = din("c_tab", [128, 2, 64, 128], BF16)
    y = nc.dram_tensor("y", [L, D], F32, kind="ExternalOutput").ap()
    x1_scr = nc.dram_tensor("x1_scr", [L, D], F32, kind="Internal").ap()
    tT_scr = nc.dram_tensor("tT_scr", [8, 128, L], BF16, kind="Internal").ap()
    fT_scr = nc.dram_tensor("fT_scr", [32, 128, 4, 128], BF16, kind="Internal").ap()
    y5_scr = nc.dram_tensor("y5_scr", [L, 512], F32, kind="Internal").ap()
    dbg_out = {}
    for name, shape in dbg:
        dbg_out[name] = nc.dram_tensor(name, list(shape), F32, kind="ExternalOutput").ap()

    from contextlib import ExitStack
    with ExitStack() as es:
        arena_t = es.enter_context(nc.sbuf_tensor("arena", [128, ARENA], BF16))
        ps_t = es.enter_context(nc.psum_tensor("ps", [128, 4096], F32))
        sems = {n: es.enter_context(nc.semaphore("sem_" + n)) for n in ("pe", "act", "dve", "pool", "sp")}
        dma_sems = [es.enter_context(nc.semaphore(f"dsem{i}")) for i in range(40)]
        block = es.enter_context(nc.Block())
        K = Sched(nc, sems, dma_sems)
        A = Arena(arena_t)

        def PS(b, n=512, off=0):
            return ps_t[:, b * 512 + off: b * 512 + off + n]

        def PSB(b):
            return ps_t[:, b * 512:(b + 1) * 512].bitcast(BF16)

        def pk(b):
            return ("ps", b)

        def cp(eng, out, in_, reads, writes):
            if eng == "act":
                K.op("act", lambda e: e.activation(out=out, in_=in_, func=AF.Identity), reads=reads, writes=writes)
            else:
                K.op(eng, lambda e: e.tensor_copy(out=out, in_=in_), reads=reads, writes=writes)

        def dump(name, src_ap, key):
            if name in dbg_out:
                K.dma("sp", dbg_out[name], src_ap, reads=[key], writes=[("dbg", name)])

        ident_f = A.alloc([128], F32)
        ident_b = A.alloc([128], BF16)
        masks = A.alloc([256], F32)
        kidx = A.alloc([18], F32)
        rep16 = A.alloc([128], F32)
        dftc = A.alloc([256], F32)
        cols = A.alloc([128], F32)
        scol = A.alloc([16], F32)
        modc = A.alloc([48, 2], F32)
        vecs = A.alloc([8, 8], F32)
        g1_bc = A.alloc([1024], F32)
        g2_bc = A.alloc([1024], F32)
        fin_bc = A.alloc([1024], F32)
        brt_bc = A.alloc([36], F32)
        Dcol = A.alloc([32], F32)
        AcAs = A.alloc([4, 256], BF16)
        wrt = A.alloc([8, 36], F32)
        small = A.alloc([64], F32)
        persist_top = A.top

        class _Stop(Exception):
            pass

        def chk(k):
            if stop_after == k:
                raise _Stop()

        try:
            K.dma("sp", ident_f, c_ident, writes=["ident_f"])
            K.dma("sp", masks, c_masks, writes=["masks"])
            K.dma("sp", kidx, c_kidx, writes=["kidx"])
            K.dma("sp", rep16[0:16, :], c_rep16, writes=["rep16"])
            K.dma("sp", dftc, c_dftc, writes=["dftc"])
            K.dma("sp", g1_bc, b_ada[2048:3072].partition_broadcast(128), writes=["g1_bc"])
            K.dma("sp", g2_bc, b_ada[5120:6144].partition_broadcast(128), writes=["g2_bc"])
            K.dma("sp", fin_bc, fin_g.partition_broadcast(128), writes=["fin_bc"])
            K.dma("sp", brt_bc, b_rt.partition_broadcast(128), writes=["brt_bc"])
            K.dma("sp", wrt, w_rt.rearrange("(kc p) n -> p kc n", p=128), writes=["wrt"])
            K.op("dve", lambda e: e.tensor_copy(out=ident_b, in_=ident_f), reads=["ident_f"], writes=["ident_b"])

            p2start = A.top
            U = A.alloc([32, 576], BF16)
            PQ = A.alloc([32, 1024], BF16)
            p2mark = A.top
            w_in_bf = A.alloc([8, 1024], BF16)
            w_in_r = w_in.rearrange("(kc p) n -> p kc n", p=128)
            for kc in range(8):
                K.dma("pool", w_in_bf[:, kc, :], w_in_r[:, kc, :], writes=["w_in_bf"])
            mark = A.top
            rows_sb = A.alloc([128], F32)
            srep = A.alloc([8, 128], F32)
            wa = [A.alloc([8, 512], F32) for _ in range(2)]
            K.dma("sp", rows_sb, rows_in, writes=["rows_sb"])
            K.op("pe", lambda e: e.transpose(PS(0, 128), rows_sb, ident_f), reads=["rows_sb", "ident_f"], writes=[pk(0)])
            K.op("dve", lambda e: e.tensor_copy(out=cols, in_=PS(0, 128)), reads=[pk(0)], writes=["cols"])
            K.op("act", lambda e: e.activation(out=scol, in_=cols[:, 0:16], func=AF.Silu), reads=["cols"], writes=["scol"])
            K.op("dve", lambda e: e.tensor_copy(out=srep, in_=scol[:, 0:8].unsqueeze(2).to_broadcast([128, 8, 128])),
                 reads=["scol"], writes=["srep"])
            wa_rr = [w_ada.rearrange("(kc p) n -> p kc n", p=128)[:, :, cc * 512:(cc + 1) * 512] for cc in range(12)]
            for cc in range(12):
                buf = wa[cc % 2]
                K.dma("sp" if cc % 2 == 0 else "act", buf, wa_rr[cc], writes=[("wa", cc % 2)])
                for fb in range(4):
                    j = cc * 4 + fb
                    for kc in range(8):
                        K.op("pe", lambda e, buf=buf, fb=fb, kc=kc, j=j: e.matmul(
                            PS(1, 2, 2 * j), lhsT=buf[:, kc, fb * 128:(fb + 1) * 128], rhs=scol[:, kc:16:8],
                            start=(kc == 0), stop=(kc == 7)),
                            reads=[("wa", cc % 2), "scol"], writes=[pk(1)])
                if cc in (4, 5, 10, 11):
                    tgt = g1_bc if cc in (4, 5) else g2_bc
                    tk = "g1_bc" if cc in (4, 5) else "g2_bc"
                    half = cc % 2 if cc in (4, 5) else (cc - 10)
                    for kc in range(8):
                        K.op("pe", lambda e, buf=buf, kc=kc: e.matmul(
                            PS(2), lhsT=srep[:, kc, :], rhs=buf[:, kc, :], start=(kc == 0), stop=(kc == 7)),
                            reads=[("wa", cc % 2), "srep"], writes=[pk(2)])
                    K.op("dve", lambda e, tgt=tgt, half=half: e.tensor_tensor(
                        out=tgt[:, half * 512:(half + 1) * 512], in0=PS(2), in1=tgt[:, half * 512:(half + 1) * 512], op=ALU.add),
                        reads=[pk(2), tk], writes=[tk])
            K.op("dve", lambda e: e.tensor_tensor(
                out=modc, in0=PS(1, 96).rearrange("p (j t) -> p j t", t=2),
                in1=cols[:, 32:80].unsqueeze(2).to_broadcast([128, 48, 2]), op=ALU.add),
                reads=[pk(1), "cols"], writes=["modc"])
            def vec_ops(e_name, dst, g_lo, sc_j, which):
                K.op("dve", lambda e: e.scalar_tensor_tensor(
                    out=vecs[:, dst, :], in0=modc[:, sc_j:sc_j + 8, which], scalar=1.0, in1=cols[:, g_lo:g_lo + 8],
                    op0=ALU.add, op1=ALU.mult), reads=["modc", "cols"], writes=["vecs"])
            vec_ops("dve", 0, 16, 8, 0)
            vec_ops("dve", 2, 16, 8, 1)
            vec_ops("dve", 4, 24, 32, 0)
            K.op("dve", lambda e: e.tensor_copy(out=vecs[:, 1, :], in_=modc[:, 0:8, 0]), reads=["modc"], writes=["vecs"])
            K.op("dve", lambda e: e.tensor_copy(out=vecs[:, 3, :], in_=modc[:, 0:8, 1]), reads=["modc"], writes=["vecs"])
            K.op("dve", lambda e: e.tensor_copy(out=vecs[:, 5, :], in_=modc[:, 24:32, 0]), reads=["modc"], writes=["vecs"])
            if "d_mod" in dbg_out:
                K.dma("sp", dbg_out["d_mod"], modc.rearrange("p j t -> p (j t)"), reads=["modc"], writes=[("dbg", 0)])
                K.dma("sp", dbg_out["d_g1"], g1_bc, reads=["g1_bc"], writes=[("dbg", 1)])
            drow = A.alloc([32], F32)
            with nc.allow_non_contiguous_dma(reason="tiny s5_d gather"):
                K.dma("sp", drow[0:16, :], s5_d.rearrange("(g h) -> h g", h=16), writes=["drow"], allow_slow_non_contiguous=True)
            K.op("pe", lambda e: e.matmul(PS(3, 32), lhsT=rep16[0:16, :], rhs=drow[0:16, :], start=True, stop=True),
                 reads=["rep16", "drow"], writes=[pk(3)])
            K.op("dve", lambda e: e.tensor_copy(out=Dcol, in_=PS(3, 32)), reads=[pk(3)], writes=["Dcol"])
            wf = A.alloc([4, 128], F32)
            K.dma("sp", wf, four_w.rearrange("g m d -> m g d"), writes=["wf"])
            for g in range(4):
                for t in range(2):
                    K.op("pe", lambda e, g=g, t=t: e.matmul(
                        PS(4, 128, (g * 2 + t) * 128 % 512) if False else ps_t[:, 4 * 512 + (g * 2 + t) * 128: 4 * 512 + (g * 2 + t) * 128 + 128],
                        lhsT=dftc[:, t * 128:(t + 1) * 128], rhs=wf[:, g, :], start=True, stop=True),
                        reads=["dftc", "wf"], writes=[pk(4 + (g * 2 + t) // 4)])
            K.op("dve", lambda e: e.tensor_copy(out=AcAs.rearrange("p g n -> p (g n)"), in_=ps_t[:, 4 * 512:6 * 512]),
                 reads=[pk(4), pk(5)], writes=["AcAs"])
            K.barrier()
            A.top = mark

            chk(1)
            xt = [A.alloc([1024], F32) for _ in range(4)]
            junk2 = [A.alloc([1024], BF16) for _ in range(2)]
            junk = A.alloc([1024], BF16)
            xs = [A.alloc([1024], BF16) for _ in range(2)]
            hT = [A.alloc([8, 128], BF16) for _ in range(2)]
            Zs = [A.alloc([32, 128], BF16) for _ in range(2)]
            vT = [A.alloc([4, 128], BF16) for _ in range(2)]
            stat = A.alloc([40, 4], F32)
            x_r = x.rearrange("(c s) d -> s c d", s=8)
            ctx_r = ctx.rearrange("(c s) d -> s c d", s=8)

            def rms_prep(ti, src_key, src_ap, rows, nfeat, st, junk_ap, out_bf, out_key, eng_scale="dve", junk_key="junk"):
                K.op("act", lambda e: e.activation(out=junk_ap, in_=src_ap, func=AF.Square, accum_out=st[0:rows, 0:1]),
                     reads=[src_key], writes=[junk_key, ("stat", ti)])
                K.op("act", lambda e: e.activation(out=st[0:rows, 1:2], in_=st[0:rows, 0:1], func=AF.Sqrt,
                                                   scale=1.0 / nfeat, bias=small[0:rows, 0:1]),
                     reads=[("stat", ti), "small"], writes=[("stat", ti)])
                K.op("dve", lambda e: e.reciprocal(out=st[0:rows, 2:3], in_=st[0:rows, 1:2]),
                     reads=[("stat", ti)], writes=[("stat", ti)])
                K.op(eng_scale, lambda e: e.tensor_scalar(out=out_bf, in0=src_ap, scalar1=st[0:rows, 2:3], scalar2=None, op0=ALU.mult),
                     reads=[src_key, ("stat", ti)], writes=[out_key])

            K.op("dve", lambda e: e.memset(small[:, 0:1], EPS), writes=["small"])

            tiles = [("c", s, 0) for s in range(8)] + [("x", s, j) for j in range(4) for s in range(8)]
            def p2_tile(ti):
                kind, s, j = tiles[ti]
                rows = 32 if kind == "c" else 128
                b2 = ti % 2
                b4 = ti % 4
                rms_prep(ti, ("xt", b4), xt[b4][0:rows, :], rows, 1024.0, stat[:, ti, :], junk2[b2][0:rows, :], xs[b2][0:rows, :], ("xs", b2), junk_key=("junk2", b2))
                yield
                pb = 0 + b2
                for kc in range(8):
                    K.op("pe", lambda e, kc=kc, b2=b2, pb=pb, rows=rows: e.transpose(
                        PSB(pb)[:, kc * 128: kc * 128 + rows], xs[b2][0:rows, kc * 128:(kc + 1) * 128], ident_b[0:rows, 0:rows]),
                        reads=[("xs", b2), "ident_b"], writes=[pk(pb)])
                yield
                av, sv = (2, 3) if kind == "c" else (0, 1)
                psv = PSB(pb).rearrange("p (k t) -> p k t", k=8)[:, :, 0:rows]
                K.op("dve", lambda e, psv=psv, b2=b2, rows=rows, av=av: e.tensor_tensor(
                    out=hT[b2][:, :, 0:rows], in0=psv, in1=vecs[:, av, :].unsqueeze(2).to_broadcast([128, 8, rows]), op=ALU.mult),
                    reads=[pk(pb), "vecs"], writes=[("hT", b2)])
                K.op("pool", lambda e, b2=b2, rows=rows, sv=sv: e.tensor_tensor(
                    out=hT[b2][:, :, 0:rows], in0=hT[b2][:, :, 0:rows], in1=vecs[:, sv, :].unsqueeze(2).to_broadcast([128, 8, rows]), op=ALU.add),
                    reads=[("hT", b2), "vecs"], writes=[("hT", b2)])
                yield
                zb = (ti // 8) % 2
                pz = 2 + b2
                for kc in range(8):
                    K.op("pe", lambda e, kc=kc, b2=b2, pz=pz, rows=rows: e.matmul(
                        PS(pz)[0:rows, :], lhsT=hT[b2][:, kc, 0:rows], rhs=w_in_bf[:, kc, 0:512], start=(kc == 0), stop=(kc == 7)),
                        reads=[("hT", b2), "w_in_bf"], writes=[pk(pz)])
                K.op("act", lambda e, zb=zb, s=s, pz=pz, rows=rows: e.activation(
                    out=Zs[zb][0:rows].rearrange("p g (s h) -> p g s h", s=8)[:, :, s, :],
                    in_=PS(pz)[0:rows, :].rearrange("p (g h) -> p g h", g=32), func=AF.Identity),
                     reads=[pk(pz)], writes=[("Zs", zb, s)])
                yield
                if kind == "x":
                    pv = 4 + b2
                    for g in range(4):
                        for kc in range(8):
                            K.op("pe", lambda e, g=g, kc=kc, b2=b2, pv=pv: e.matmul(
                                PS(pv, 128, g * 128), lhsT=w_in_bf[:, kc, 512 + g * 128: 512 + (g + 1) * 128], rhs=hT[b2][:, kc, :],
                                start=(kc == 0), stop=(kc == 7)), reads=[("hT", b2), "w_in_bf"], writes=[pk(pv)])
                    K.op("act", lambda e, b2=b2, pv=pv: e.activation(out=vT[b2].rearrange("p g t -> p (g t)"), in_=PS(pv), func=AF.Identity),
                         reads=[pk(pv)], writes=[("vT", b2)])
                    tl = j * 8 + s
                    for g in range(4):
                        K.op("pe", lambda e, g=g, b2=b2: e.matmul(
                            PS(6 + g // 2, 256, (g % 2) * 256), lhsT=vT[b2][:, g, :], rhs=AcAs[:, g, :], start=True, stop=True),
                            reads=[("vT", b2), "AcAs"], writes=[pk(6 + g // 2)])
                    K.op("dve", lambda e, tl=tl: e.tensor_copy(
                        out=PQ[:, tl, :].rearrange("p (r g m) -> p g r m", r=2, g=4),
                        in_=ps_t[:, 6 * 512:8 * 512].rearrange("p (g r m) -> p g r m", g=4, r=2)),
                         reads=[pk(6), pk(7)], writes=[("PQ", tl)])
                yield
                if s == 7:
                    nrow = rows
                    if kind == "c":
                        col_sets = [(0, 32), (544, 576)]
                    else:
                        col_sets = [(32 + 128 * j, 32 + 128 * (j + 1))]
                    for g8 in range(4):
                        pu = 6 + (g8 % 2)
                        for gi in range(8):
                            g = g8 * 8 + gi
                            K.op("pe", lambda e, g=g, gi=gi, zb=zb, pu=pu, nrow=nrow: e.transpose(
                                PSB(pu)[:, gi * 128: gi * 128 + nrow], Zs[zb][0:nrow, g, :], ident_b[0:nrow, 0:nrow]),
                                reads=[("Zs", zb, ss_) for ss_ in range(8)] + ["ident_b"], writes=[pk(pu)])
                        for (c0, c1) in col_sets:
                            cp("act" if g8 % 2 == 0 else "dve", U[:, g8 * 8:(g8 + 1) * 8, c0:c1],
                               PSB(pu).rearrange("p (g c) -> p g c", g=8)[:, :, 0:nrow], [pk(pu)], [("U", g8)])

            def p2_load(ti):
                kind, s, j = tiles[ti]
                rows = 32 if kind == "c" else 128
                src = ctx_r[s, :, :] if kind == "c" else x_r[s, j * 128:(j + 1) * 128, :]
                K.dma("sp", xt[ti % 4][0:rows, :], src, writes=[("xt", ti % 4)])
            p2_load(0); p2_load(1)
            for pr in range(len(tiles) // 2):
                if 2 * pr + 3 < len(tiles):
                    p2_load(2 * pr + 2); p2_load(2 * pr + 3)
                gens = [p2_tile(2 * pr), p2_tile(2 * pr + 1)]
                alive = [True, True]
                while any(alive):
                    for gi_, g_ in enumerate(gens):
                        if alive[gi_]:
                            try:
                                next(g_)
                            except StopIteration:
                                alive[gi_] = False
            K.barrier()
            A.top = p2mark

            chk(2)
            p3mark = A.top
            fnTt = [A.alloc([4, 128], BF16) for _ in range(2)]
            tab = A.alloc([2, 64, 128], BF16)
            fsb = [A.alloc([512], F32) for _ in range(2)]
            fmn = [A.alloc([512], F32) for _ in range(2)]
            fnb = [A.alloc([512], BF16) for _ in range(2)]
            fjunk = A.alloc([512], BF16)
            fstat = A.alloc([32, 4], F32)
            PQv = PQ.rearrange("p t (r n) -> p t r n", r=2)
            for r in range(2):
                K.dma("sp" if r == 0 else "act", tab[:, r], c_tab[:, r], writes=["tab"])
            hoist_lo = A.top
            Lrows = A.alloc([2, 128], F32)
            LR = A.alloc([32], F32); LI = A.alloc([32], F32); LS = A.alloc([32], F32)
            T = {n: A.alloc([32, 18], F32) for n in ("AK", "ANG", "ANGC", "TF", "SINK", "COSK", "MAGP", "MAGN", "LKR", "LKI")}
            TI = A.alloc([32, 18], I32)
            S = {n: A.alloc([32], F32) for n in ("dt", "a", "th", "nr", "den", "rden", "qre", "qim", "t1", "t2")}
            W = {n: A.alloc([32, 8], F32) for n in ("ire", "iim", "wre", "wim", "w2re", "w2im", "t1", "t2")}
            K.dma("act", Lrows[0:32, 0, :].rearrange("g (d p) -> g d p", d=2), lam_re.rearrange("d g p -> g d p"), writes=["Lrows"])
            K.dma("act", Lrows[0:32, 1, :].rearrange("g (d p) -> g d p", d=2), lam_im.rearrange("d g p -> g d p"), writes=["Lrows"])
            for d in range(2):
                K.dma("act", LS[64 * d:64 * d + 64, :], log_step[d].partition_broadcast(64), writes=["LS"])
            for r, dst, key in ((0, LR, "LR"), (1, LI, "LI")):
                K.op("pe", lambda e, r=r: e.transpose(PS(0, 32), Lrows[0:32, r, :], ident_f[0:32, 0:32]), reads=["Lrows", "ident_f"], writes=[pk(0)])
                cp("dve", dst, PS(0, 32), [pk(0)], [key])
            def dve(fn, reads, writes, eng="dve"):
                K.op(eng, fn, reads=reads, writes=writes)
            K.op("act", lambda e: e.activation(out=S["dt"], in_=LS, func=AF.Exp), reads=["LS"], writes=["dt"])
            dve(lambda e: e.tensor_tensor(out=S["a"], in0=LR, in1=S["dt"], op=ALU.mult), ["LR", "dt"], ["a"])
            dve(lambda e: e.tensor_tensor(out=S["th"], in0=LI, in1=S["dt"], op=ALU.mult), ["LI", "dt"], ["th"])
            kidx_b = kidx.unsqueeze(1).to_broadcast([128, 32, 18])
            dve(lambda e: e.tensor_tensor(out=T["AK"], in0=S["a"].unsqueeze(2).to_broadcast([128, 32, 18]), in1=kidx_b, op=ALU.mult), ["a", "kidx"], ["AK"])
            dve(lambda e: e.tensor_tensor(out=T["ANG"], in0=S["th"].unsqueeze(2).to_broadcast([128, 32, 18]), in1=kidx_b, op=ALU.mult), ["th", "kidx"], ["ANG"])
            dve(lambda e: e.tensor_scalar(out=T["ANGC"], in0=T["ANG"], scalar1=math.pi / 2, scalar2=None, op0=ALU.add), ["ANG"], ["ANGC"])
            for src, dst in (("ANG", "SINK"), ("ANGC", "COSK")):
                dve(lambda e, src=src: e.tensor_scalar(out=TI, in0=T[src], scalar1=1.0 / TWO_PI, scalar2=None, op0=ALU.mult), [src], ["TI"])
                dve(lambda e: e.tensor_copy(out=T["TF"], in_=TI), ["TI"], ["TF"])
                dve(lambda e, src=src: e.scalar_tensor_tensor(out=T["TF"], in0=T["TF"], scalar=-TWO_PI, in1=T[src], op0=ALU.mult, op1=ALU.add), ["TF", src], ["TF"])
                dve(lambda e: e.tensor_scalar(out=T["TF"], in0=T["TF"], scalar1=-3.14159, scalar2=3.14159, op0=ALU.max, op1=ALU.min), ["TF"], ["TF"])
                K.op("act", lambda e, dst=dst: e.activation(out=T[dst], in_=T["TF"], func=AF.Sin), reads=["TF"], writes=[dst])
            K.op("act", lambda e: e.activation(out=T["MAGP"], in_=T["AK"], func=AF.Exp), reads=["AK"], writes=["MAGP"])
            K.op("act", lambda e: e.activation(out=T["MAGN"], in_=T["AK"], func=AF.Exp, scale=-1.0), reads=["AK"], writes=["MAGN"])
            dve(lambda e: e.tensor_tensor(out=T["LKR"], in0=T["MAGP"], in1=T["COSK"], op=ALU.mult), ["MAGP", "COSK"], ["LKR"])
            dve(lambda e: e.tensor_tensor(out=T["LKI"], in0=T["MAGP"], in1=T["SINK"], op=ALU.mult), ["MAGP", "SINK"], ["LKI"])
            l1r = T["LKR"][:, :, 16]; l1i = T["LKI"][:, :, 16]
            dve(lambda e: e.tensor_scalar(out=S["nr"], in0=l1r, scalar1=-1.0, scalar2=None, op0=ALU.add), ["LKR"], ["nr"])
            dve(lambda e: e.tensor_tensor(out=S["den"], in0=LR, in1=LR, op=ALU.mult), ["LR"], ["den"])
            dve(lambda e: e.tensor_tensor(out=S["t1"], in0=LI, in1=LI, op=ALU.mult), ["LI"], ["t1"])
            dve(lambda e: e.tensor_tensor(out=S["den"], in0=S["den"], in1=S["t1"], op=ALU.add), ["den", "t1"], ["den"])
            dve(lambda e: e.reciprocal(out=S["rden"], in_=S["den"]), ["den"], ["rden"])
            dve(lambda e: e.tensor_tensor(out=S["t1"], in0=S["nr"], in1=LR, op=ALU.mult), ["nr", "LR"], ["t1"])
            dve(lambda e: e.tensor_tensor(out=S["t2"], in0=l1i, in1=LI, op=ALU.mult), ["LKI", "LI"], ["t2"])
            dve(lambda e: e.tensor_tensor(out=S["t1"], in0=S["t1"], in1=S["t2"], op=ALU.add), ["t1", "t2"], ["t1"])
            dve(lambda e: e.tensor_tensor(out=S["qre"], in0=S["t1"], in1=S["rden"], op=ALU.mult), ["t1", "rden"], ["qre"])
            dve(lambda e: e.tensor_tensor(out=S["t1"], in0=l1i, in1=LR, op=ALU.mult), ["LKI", "LR"], ["t1"])
            dve(lambda e: e.tensor_tensor(out=S["t2"], in0=S["nr"], in1=LI, op=ALU.mult), ["nr", "LI"], ["t2"])
            dve(lambda e: e.tensor_tensor(out=S["t1"], in0=S["t1"], in1=S["t2"], op=ALU.subtract), ["t1", "t2"], ["t1"])
            dve(lambda e: e.tensor_tensor(out=S["qim"], in0=S["t1"], in1=S["rden"], op=ALU.mult), ["t1", "rden"], ["qim"])
            dve(lambda e: e.tensor_tensor(out=W["ire"], in0=T["MAGN"][:, :, 0:8], in1=T["COSK"][:, :, 0:8], op=ALU.mult), ["MAGN", "COSK"], ["ire"])
            dve(lambda e: e.scalar_tensor_tensor(out=W["iim"], in0=T["MAGN"][:, :, 0:8], scalar=-1.0, in1=T["SINK"][:, :, 0:8], op0=ALU.mult, op1=ALU.mult), ["MAGN", "SINK"], ["iim"])
            qreb = S["qre"].unsqueeze(2).to_broadcast([128, 32, 8]); qimb = S["qim"].unsqueeze(2).to_broadcast([128, 32, 8])

            def cmul(ore, oim, ar, ai, br, bi, keys_a, keys_b, ko):
                dve(lambda e: e.tensor_tensor(out=W["t1"], in0=ar, in1=br, op=ALU.mult), keys_a + keys_b, ["Wt1"])
                dve(lambda e: e.tensor_tensor(out=W["t2"], in0=ai, in1=bi, op=ALU.mult), keys_a + keys_b, ["Wt2"])
                dve(lambda e: e.tensor_tensor(out=ore, in0=W["t1"], in1=W["t2"], op=ALU.subtract), ["Wt1", "Wt2"], [ko + "re"])
                dve(lambda e: e.tensor_tensor(out=W["t1"], in0=ar, in1=bi, op=ALU.mult), keys_a + keys_b + [ko + "re"], ["Wt1"])
                dve(lambda e: e.tensor_tensor(out=W["t2"], in0=ai, in1=br, op=ALU.mult), keys_a + keys_b + [ko + "re"], ["Wt2"])
                dve(lambda e: e.tensor_tensor(out=oim, in0=W["t1"], in1=W["t2"], op=ALU.add), ["Wt1", "Wt2"], [ko + "im"])
            cmul(W["wre"], W["wim"], W["ire"], W["iim"], qreb, qimb, ["ire", "iim"], ["qre", "qim"], "w")
            cmul(W["w2re"], W["w2im"], T["LKR"][:, :, 8:16], T["LKI"][:, :, 8:16], qreb, qimb, ["LKR", "LKI"], ["qre", "qim"], "w2")
            def dft_a(ot):
                b2 = ot % 2
                jo, so = ot // 8, ot % 8
                pA, pB = 2 * b2, 2 * b2 + 1
                plan = []
                for it in range(32):
                    ji, si = it // 8, it % 8
                    m = (ji * so + jo * si) % 4
                    ss = si * 8 + so
                    if m == 0:
                        plan += [(pA, 0, 0, it, ss), (pB, 1, 1, it, ss)]
                    elif m == 1:
                        plan += [(pB, 1, 0, it, ss), (pB, 0, 1, it, ss)]
                    elif m == 2:
                        plan += [(pB, 0, 0, it, ss), (pA, 1, 1, it, ss)]
                    else:
                        plan += [(pA, 1, 0, it, ss), (pA, 0, 1, it, ss)]
                la = [p for p in plan if p[0] == pA]; lb = [p for p in plan if p[0] == pB]
                plan = []
                for q in range(max(len(la), len(lb))):
                    if q < len(la):
                        plan.append(la[q])
                    if q < len(lb):
                        plan.append(lb[q])
                cnt = {pA: len(la), pB: len(lb)}
                seen = {pA: 0, pB: 0}
                for (bk, tr, qr, it, ss) in plan:
                    st_ = (seen[bk] == 0)
                    seen[bk] += 1
                    sp_ = (seen[bk] == cnt[bk])
                    K.op("pe", lambda e, bk=bk, tr=tr, qr=qr, it=it, ss=ss, st_=st_, sp_=sp_: e.matmul(
                        PS(bk), lhsT=tab[:, tr, ss, :], rhs=PQv[:, it, qr, :], start=st_, stop=sp_),
                        reads=["tab", ("PQ", it)], writes=[pk(bk)])
                cp("act", fmn[b2], PS(pB), [pk(pB)], [("fmn", b2)])
                K.op("dve", lambda e, b2=b2, pA=pA: e.tensor_tensor(out=fsb[b2], in0=PS(pA), in1=fmn[b2], op=ALU.subtract),
                     reads=[pk(pA), ("fmn", b2)], writes=[("fsb", b2)])
                if ot == 0 and "d_f" in dbg_out:
                    K.dma("sp", dbg_out["d_f"], fsb[0], reads=[("fsb", 0)], writes=[("dbg", 5)])
                K.op("act", lambda e, b2=b2, ot=ot: e.activation(out=fjunk, in_=fsb[b2], func=AF.Square, accum_out=fstat[:, ot, 0:1]),
                     reads=[("fsb", b2)], writes=["fjunk", ("fstat", ot)])
                K.op("act", lambda e, ot=ot: e.activation(out=fstat[:, ot, 1:2], in_=fstat[:, ot, 0:1], func=AF.Sqrt, scale=1.0 / 512.0, bias=small[:, 0:1]),
                     reads=[("fstat", ot), "small"], writes=[("fstat", ot)])
                K.op("dve", lambda e, ot=ot: e.reciprocal(out=fstat[:, ot, 2:3], in_=fstat[:, ot, 1:2]), reads=[("fstat", ot)], writes=[("fstat", ot)])
                K.op("dve", lambda e, b2=b2, ot=ot: e.tensor_scalar(out=fnb[b2], in0=fsb[b2], scalar1=fstat[:, ot, 2:3], scalar2=None, op0=ALU.mult),
                     reads=[("fsb", b2), ("fstat", ot)], writes=[("fnb", b2)])

            def dft_b(ot):
                b2 = ot % 2
                pt = 4 + b2
                for fb in range(4):
                    K.op("pe", lambda e, fb=fb, b2=b2, pt=pt: e.transpose(PSB(pt)[:, fb * 128:(fb + 1) * 128], fnb[b2][:, fb * 128:(fb + 1) * 128], ident_b),
                         reads=[("fnb", b2), "ident_b"], writes=[pk(pt)])
                K.op("dve", lambda e, b2=b2, pt=pt: e.tensor_copy(out=fnTt[b2].rearrange("p f t -> p (f t)"), in_=PSB(pt)[:, 0:512]),
                     reads=[pk(pt)], writes=[("fnTt", b2)])
                K.dma("act", fT_scr[ot], fnTt[b2], reads=[("fnTt", b2)], writes=[("fTscr", ot)])
            for ot in range(32):
                dft_a(ot)
                if ot > 0:
                    dft_b(ot - 1)
            dft_b(31)
            K.barrier()
            A.top = p2start + 32 * 576

            chk(3)
            GamRe = A.alloc([32, 128], BF16)
            GamIm = A.alloc([32, 128], BF16)
            Mi = A.alloc([32, 128], BF16)
            ovl = A.top
            PhiT = A.alloc([32, 2, 128], BF16)
            RS = 32
            ring = A.alloc([RS, 4, 32], F32)
            rtmp = A.alloc([4, 32], F32); rC = A.alloc([2, 32], F32)
            L4 = A.alloc([4, 32], F32)
            p4mark = A.top
            Bre = A.alloc([32, 16], F32); Bim = A.alloc([32, 16], F32)
            Crows = A.alloc([2, 4, 128], F32)
            Cre = A.alloc([32, 16], F32); Cim = A.alloc([32, 16], F32)
            PH1 = A.alloc([16, 8, 16], F32); PH2 = A.alloc([16, 8, 16], F32)
            PhRe = A.alloc([32, 128], BF16); PhIm = A.alloc([32, 128], BF16)
            assert A.top <= hoist_lo, (A.top, hoist_lo)
            MT1 = PH1.rearrange("p g s h -> p (g s h)")[:, 0:512].rearrange("p (g m) -> p g m", g=4)
            MT2 = PH2.rearrange("p g s h -> p (g s h)")[:, 0:512].rearrange("p (g m) -> p g m", g=4)

            with nc.allow_non_contiguous_dma(reason="s5 small param layouts"):
                for d in range(2):
                    K.dma("sp", Bre[64 * d:64 * d + 64], b_re[d].rearrange("g p h -> p g h"), writes=["Bre"])
                    K.dma("act", Bim[64 * d:64 * d + 64], b_im[d].rearrange("g p h -> p g h"), writes=["Bim"])
                    for gb in range(4):
                        K.dma("sp", Crows[:, 0, gb, 64 * d:64 * d + 64], c_re[d, gb * 8:(gb + 1) * 8].rearrange("gl h p -> (gl h) p"), writes=["Crows"])
                        K.dma("act", Crows[:, 1, gb, 64 * d:64 * d + 64], c_im[d, gb * 8:(gb + 1) * 8].rearrange("gl h p -> (gl h) p"), writes=["Crows"])
            for r, dst, key in ((0, Cre, "Cre"), (1, Cim, "Cim")):
                for gb in range(4):
                    K.op("pe", lambda e, r=r, gb=gb: e.transpose(PS(1, 128, gb * 128), Crows[:, r, gb, :], ident_f), reads=["Crows", "ident_f"], writes=[pk(1)])
                cp("dve", dst.rearrange("p g h -> p (g h)"), PS(1), [pk(1)], [key])

            chk(3.1)

            def outer(dst_re, dst_im, wr, wi, wkeys, Xre, Xim, xkeys, okey, neg_im=False):
                for gh in range(2):
                    gs = slice(gh * 16, gh * 16 + 16)
                    wrb = wr[:, gs, :].unsqueeze(3).to_broadcast([128, 16, 8, 16]); wib = wi[:, gs, :].unsqueeze(3).to_broadcast([128, 16, 8, 16])
                    xrb = Xre[:, gs, :].unsqueeze(2).to_broadcast([128, 16, 8, 16]); xib = Xim[:, gs, :].unsqueeze(2).to_broadcast([128, 16, 8, 16])
                    dr = dst_re[:, gs, :].rearrange("p g (s h) -> p g s h", s=8); di = dst_im[:, gs, :].rearrange("p g (s h) -> p g s h", s=8)
                    dve(lambda e, wrb=wrb, xrb=xrb: e.tensor_tensor(out=PH1, in0=wrb, in1=xrb, op=ALU.mult), wkeys + xkeys, ["PH1"])
                    dve(lambda e, wib=wib, xib=xib: e.tensor_tensor(out=PH2, in0=wib, in1=xib, op=ALU.mult), wkeys + xkeys, ["PH2"])
                    dve(lambda e, dr=dr: e.tensor_tensor(out=dr, in0=PH1, in1=PH2, op=ALU.subtract), ["PH1", "PH2"], [okey + "re"])
                    dve(lambda e, wrb=wrb, xib=xib: e.tensor_tensor(out=PH1, in0=wrb, in1=xib, op=ALU.mult), wkeys + xkeys + [okey + "re"], ["PH1"])
                    dve(lambda e, wib=wib, xrb=xrb: e.tensor_tensor(out=PH2, in0=wib, in1=xrb, op=ALU.mult), wkeys + xkeys + [okey + "re"], ["PH2"])
                    if neg_im:
                        dve(lambda e, di=di: e.scalar_tensor_tensor(out=di, in0=PH1, scalar=-1.0, in1=PH2, op0=ALU.mult, op1=ALU.subtract), ["PH1", "PH2"], [okey + "im"])
                    else:
                        dve(lambda e, di=di: e.tensor_tensor(out=di, in0=PH1, in1=PH2, op=ALU.add), ["PH1", "PH2"], [okey + "im"])
            chk(3.2)
            outer(PhRe, PhIm, W["w2re"], W["w2im"], ["w2re", "w2im"], Bre, Bim, ["Bre", "Bim"], "Ph")
            for g in range(32):
                pb = 2 + (g // 4) % 2
                for ri, src, sk in ((0, PhRe, "Phre"), (1, PhIm, "Phim")):
                    K.op("pe", lambda e, g=g, ri=ri, src=src, pb=pb: e.transpose(
                        PSB(pb)[:, ((g % 4) * 2 + ri) * 128:((g % 4) * 2 + ri + 1) * 128], src[:, g, :], ident_b),
                        reads=[sk, "ident_b"], writes=[pk(pb)])
                if g % 4 == 3:
                    g0 = g - 3
                    cp("act", PhiT[:, g0:g0 + 4].rearrange("p g r m -> p (g r m)"), PSB(pb), [pk(pb)], ["PhiT"])
            chk(3.3)
            outer(PhRe, PhIm, W["wre"], W["wim"], ["wre", "wim"], Bre, Bim, ["Bre", "Bim"], "Ph")
            outer(GamRe, GamIm, T["LKR"][:, :, 0:8], T["LKI"][:, :, 0:8], ["LKR", "LKI"], Cre, Cim, ["Cre", "Cim"], "Gam", neg_im=True)
            l8r = T["LKR"][:, :, 17]; l8i = T["LKI"][:, :, 17]
            dve(lambda e: e.tensor_copy(out=L4[:, 0:2, :], in_=l8r.unsqueeze(1).to_broadcast([128, 2, 32])), ["LKR"], ["L4"])
            dve(lambda e: e.tensor_copy(out=L4[:, 2, :], in_=l8i), ["LKI"], ["L4"])
            dve(lambda e: e.tensor_scalar(out=L4[:, 3, :], in0=l8i, scalar1=-1.0, scalar2=None, op0=ALU.mult), ["LKI"], ["L4"])
            mfb = masks[:, 0:128].unsqueeze(1).to_broadcast([128, 4, 128]); mrb = masks[:, 128:256].unsqueeze(1).to_broadcast([128, 4, 128])
            for g4 in range(8):
                pf_, pr_ = 4 + 2 * (g4 % 2), 5 + 2 * (g4 % 2)
                for gi in range(4):
                    g = g4 * 4 + gi
                    for d, pb in ((0, pf_), (1, pr_)):
                        K.op("pe", lambda e, g=g, gi=gi, d=d, pb=pb: e.matmul(
                            PS(pb, 128, gi * 128), lhsT=PhRe[64 * d:64 * d + 64, g, :], rhs=GamRe[64 * d:64 * d + 64, g, :], start=True, stop=False),
                            reads=["Phre", "Gamre"], writes=[pk(pb)])
                        K.op("pe", lambda e, g=g, gi=gi, d=d, pb=pb: e.matmul(
                            PS(pb, 128, gi * 128), lhsT=PhIm[64 * d:64 * d + 64, g, :], rhs=GamIm[64 * d:64 * d + 64, g, :], start=False, stop=True),
                            reads=["Phim", "Gamim"], writes=[pk(pb)])
                dve(lambda e, pf_=pf_: e.tensor_tensor(out=MT1, in0=PS(pf_).rearrange("p (g m) -> p g m", g=4), in1=mfb, op=ALU.mult), [pk(pf_), "masks"], ["PH1"])
                dve(lambda e, pr_=pr_: e.tensor_tensor(out=MT2, in0=PS(pr_).rearrange("p (g m) -> p g m", g=4), in1=mrb, op=ALU.mult), [pk(pr_), "masks"], ["PH2"])
                dve(lambda e: e.tensor_tensor(out=MT1, in0=MT1, in1=MT2, op=ALU.add), ["PH1", "PH2"], ["PH1"])
                for gi in range(4):
                    g = g4 * 4 + gi
                    dve(lambda e, g=g, gi=gi: e.scalar_tensor_tensor(out=Mi[:, g, :], in0=ident_f, scalar=Dcol[:, g:g + 1], in1=MT1[:, gi, :], op0=ALU.mult, op1=ALU.add),
                        ["PH1", "ident_f", "Dcol"], [("Mi", g)])
            if "d_Mi" in dbg_out:
                dM = A.alloc([128], F32)
                dve(lambda e: e.tensor_copy(out=dM, in_=Mi[:, 0, :]), [("Mi", 0)], ["dM"])
                K.dma("sp", dbg_out["d_Mi"], dM, reads=["dM"], writes=[("dbg", 20)])
            K.barrier()
            chk(3.4)
            A.top = p4mark
            XV = A.alloc([2, 32, NE], BF16)
            for g in range(32):
                for ri in range(2):
                    idx = g * 2 + ri
                    pa = idx % 2
                    pbk = 2 + (idx // 16) % 2
                    for d in range(2):
                        rA = U[:, g, 0:512] if d == 0 else U[:, g, 575:63:-1]
                        rB = U[:, g, 512:543] if d == 0 else U[:, g, 63:32:-1]
                        K.op("pe", lambda e, g=g, ri=ri, d=d, pa=pa, rA=rA: e.matmul(
                            PS(pa)[64 * d:64 * d + 64, :], lhsT=PhiT[:, g, ri, 64 * d:64 * d + 64], rhs=rA, start=True, stop=True),
                            reads=["PhiT", ("U", g // 8)], writes=[pk(pa)])
                        K.op("pe", lambda e, g=g, ri=ri, d=d, pbk=pbk, rB=rB, idx=idx: e.matmul(
                            PS(pbk, 31, (idx % 16) * 32)[64 * d:64 * d + 64, :], lhsT=PhiT[:, g, ri, 64 * d:64 * d + 64], rhs=rB, start=True, stop=True),
                            reads=["PhiT", ("U", g // 8)], writes=[pk(pbk)])
                    cp("act" if idx % 2 == 0 else "dve", XV[:, ri, g, 0:512], PS(pa), [pk(pa)], ["XVin"])
                    if idx % 16 == 15:
                        g0 = (idx - 15) // 2
                        cp("dve", XV[:, :, g0:g0 + 8, 512:543].rearrange("p r g c -> p g r c"),
                           PS(pbk).rearrange("p (g r c) -> p g r c", g=8, r=2)[:, :, :, 0:31], [pk(pbk)], ["XVin"])
            if "d_V" in dbg_out:
                dV = A.alloc([NE], F32)
                dve(lambda e: e.tensor_copy(out=dV, in_=XV[:, 0, 0, :]), ["XVin"], ["dV"])
                K.dma("sp", dbg_out["d_V"], dV, reads=["dV"], writes=[("dbg", 21)])
            chk(3.5)
            XVOUT = []
            dve(lambda e: e.memset(ring[:, 0, :, :], 0.0), [], [("ring", 0)])
            NSTEP = 543
            CV = 16
            rk = lambda slot: ("ring", (slot % RS) // CV)
            for e_ in range(NSTEP):
                cur = ring[:, e_ % RS, :, :]
                nxt = ring[:, (e_ + 1) % RS, :, :].rearrange("p (c r) g -> p c r g", c=2)
                dve(lambda e, cur=cur: e.tensor_tensor(out=rtmp, in0=cur, in1=L4, op=ALU.mult), [rk(e_), "L4"], ["rtmp"])
                dve(lambda e: e.tensor_tensor(out=rC, in0=rtmp[:, 0:2, :], in1=rtmp[:, 3:1:-1, :], op=ALU.add), ["rtmp"], ["rC"])
                dve(lambda e, nxt=nxt, e_=e_: e.tensor_tensor(out=nxt, in0=rC.unsqueeze(1).to_broadcast([128, 2, 2, 32]),
                                                              in1=XV[:, :, :, e_].unsqueeze(1).to_broadcast([128, 2, 2, 32]), op=ALU.add), ["rC", "XVin"], [rk(e_ + 1)])
                if (e_ + 1) % CV == 0 or e_ == NSTEP - 1:
                    n = CV if (e_ + 1) % CV == 0 else (NSTEP % CV)
                    e0 = e_ + 1 - n
                    k = 0
                    while k < n:
                        rs = (e0 + 1 + k) % RS
                        run = min(n - k, RS - rs)
                        cp("act", XV[:, :, :, e0 + k:e0 + k + run], ring[:, rs:rs + run, 0:2, :].rearrange("p c r g -> p r g c"),
                           sorted(set([rk(rs + q) for q in range(run)])), [("XVout", e0 // CV, k)])
                        XVOUT.append(("XVout", e0 // CV, k))
                        k += run
            if "d_X" in dbg_out:
                dX = A.alloc([NE], F32)
                dve(lambda e: e.tensor_copy(out=dX, in_=XV[:, 0, 0, :]), XVOUT + ["XVin"], ["dX"])
                K.dma("sp", dbg_out["d_X"], dX, reads=["dX"], writes=[("dbg", 22)])
            K.barrier()
            chk(3.6)
            A2 = Arena(arena_t); A2.top = ovl
            Ylj = A2.alloc([32, 128], F32)
            Ytj = A2.alloc([8, 512], F32)
            assert A2.top <= p4mark
            y5_r = y5_scr.rearrange("(c s) f -> c s f", s=8)
            for j in range(4):
                for g in range(32):
                    pA = (g % 2) * 3
                    K.op("pe", lambda e, g=g, pA=pA, j=j: e.matmul(PS(pA, 128), lhsT=Mi[:, g, :], rhs=U[:, g, 32 + 128 * j:32 + 128 * (j + 1)], start=True, stop=True),
                         reads=[("Mi", g), ("U", g // 8)], writes=[pk(pA)])
                    for d in range(2):
                        pD = pA + 1 + d
                        for ri, Gm in ((0, GamRe), (1, GamIm)):
                            if d == 0:
                                rhs = XV[0:64, ri, g, 31 + 128 * j:31 + 128 * (j + 1)]
                            else:
                                hi = 542 - 128 * j
                                lo = hi - 128
                                rhs = XV[64:128, ri, g, hi:lo:-1]
                            K.op("pe", lambda e, g=g, pD=pD, d=d, Gm=Gm, rhs=rhs, ri=ri: e.matmul(
                                PS(pD, 128), lhsT=Gm[64 * d:64 * d + 64, g, :], rhs=rhs, start=(ri == 0), stop=(ri == 1)),
                                reads=["Gamre", "Gamim", "XVin"] + XVOUT, writes=[pk(pD)])
                    cp("act", Ylj[:, g, :], PS(pA, 128), [pk(pA)], [("Ylj", g)])
                    K.op("dve", lambda e, g=g, pA=pA: e.tensor_tensor(out=Ylj[:, g, :], in0=Ylj[:, g, :], in1=PS(pA + 1, 128), op=ALU.add),
                         reads=[("Ylj", g), pk(pA + 1)], writes=[("Ylj", g)])
                    K.op("dve", lambda e, g=g, pA=pA: e.tensor_tensor(out=Ylj[:, g, :], in0=Ylj[:, g, :], in1=PS(pA + 2, 128), op=ALU.add),
                         reads=[("Ylj", g), pk(pA + 2)], writes=[("Ylj", g)])
                if j == 0 and "d_Y" in dbg_out:
                    K.dma("sp", dbg_out["d_Y"], Ylj[:, 0, :], reads=[("Ylj", 0)], writes=[("dbg", 7)])
                for g4 in range(8):
                    pb = 6 + g4 % 2
                    for gi in range(4):
                        g = g4 * 4 + gi
                        K.op("pe", lambda e, g=g, gi=gi, pb=pb: e.transpose(PS(pb, 128, gi * 128), Ylj[:, g, :], ident_f),
                             reads=[("Ylj", g), "ident_f"], writes=[pk(pb)])
                    cp("act" if g4 % 2 == 0 else "dve", Ytj[:, :, g4 * 64:(g4 + 1) * 64].rearrange("p s (g h) -> p g s h", g=4),
                       PS(pb).rearrange("p (g s h) -> p g s h", g=4, s=8), [pk(pb)], ["Ytj"])
                K.dma("sp", y5_r[128 * j:128 * (j + 1), :, :], Ytj, reads=["Ytj"], writes=[("y5", j)])
            K.barrier()

            chk(4)
            A.top = p2start
            G = A.alloc([32, 32], F32)
            pjunk = A.alloc([1024], BF16)
            p5keep = A.top
            pjunk2 = A.alloc([1024], BF16)
            w_out_bf = A.alloc([8, 1024], BF16)
            w_glu_bf = A.alloc([4, 512], BF16)
            lgall = A.alloc([32, 36], F32)
            p5tmp = A.top
            D2 = lambda shape, dt: [A.alloc(shape, dt) for _ in range(4)]
            D4 = lambda shape, dt: [A.alloc(shape, dt) for _ in range(8)]
            Yt = D4([512], F32); fnTt5 = D4([4, 128], BF16)
            ygf = D2([512], F32); ygb = D2([512], BF16); ygT = D2([4, 128], BF16)
            sg = D2([512], F32); y2 = D2([512], F32); y2n = D2([512], BF16); y2nT = D2([4, 128], BF16)
            xr = D4([1024], F32); x1 = D2([1024], F32)
            xs2 = D2([1024], BF16); tT = D2([8, 128], BF16); tTf = D2([8, 128], F32)
            pstat = A.alloc([32, 8], F32)
            w_out_r = w_out.rearrange("(kc p) n -> p kc n", p=128)
            for kc in range(8):
                K.dma("pool", w_out_bf[:, kc, :], w_out_r[:, kc, :], writes=["w_out_bf"])
            K.dma("pool", w_glu_bf, w_glu.rearrange("(kc p) n -> p kc n", p=128), writes=["w_glu_bf"])
            for kc in range(8):
                K.op("dve", lambda e, kc=kc: e.tensor_scalar(out=w_out_bf[:, kc, :], in0=w_out_bf[:, kc, :], scalar1=cols[:, 80 + kc:81 + kc], scalar2=None, op0=ALU.mult),
                     reads=["w_out_bf", "cols"], writes=["w_out_bf"])
            y_r = y.rearrange("(c s) d -> s c d", s=8)
            x1_r = x1_scr.rearrange("(c s) d -> s c d", s=8)
            y5_sr = y5_scr.rearrange("(c s) f -> s c f", s=8)
            pj = [pjunk, pjunk2, A.alloc([1024], BF16), A.alloc([1024], BF16)]

            def p5_loads(t_):
                j_, s_, b_ = t_ // 8, t_ % 8, t_ % 8
                K.dma("sp", Yt[b_], y5_sr[s_, j_ * 128:(j_ + 1) * 128, :], reads=[("y5", j_)], writes=[("Yt", b_)])
                K.dma("sp", fnTt5[b_], fT_scr[t_], reads=[("fTscr", t_)], writes=[("fnTt5", b_)])
                K.dma("sp", xr[b_], x_r[s_, j_ * 128:(j_ + 1) * 128, :], writes=[("xr", b_)])
            def p5_tile(tl):
                j, s = tl // 8, tl % 8
                tl = j * 8 + s
                b2 = tl % 4
                bT, bG, bO = 2 * b2, 2 * b2 + 1, 2 * b2 + 1
                b4 = tl % 8
                b3 = tl % 4
                K.op("act", lambda e, b2=b2, b4=b4: e.activation(out=ygf[b2], in_=Yt[b4], func=AF.Gelu_apprx_tanh), reads=[("Yt", b4)], writes=[("ygf", b2)])
                K.op("pool", lambda e, b2=b2: e.tensor_copy(out=ygb[b2], in_=ygf[b2]), reads=[("ygf", b2)], writes=[("ygb", b2)])
                yield
                for fb in range(4):
                    K.op("pe", lambda e, fb=fb, b2=b2, bT=bT: e.transpose(PSB(bT)[:, fb * 128:(fb + 1) * 128], ygb[b2][:, fb * 128:(fb + 1) * 128], ident_b),
                         reads=[("ygb", b2), "ident_b"], writes=[pk(bT)])
                cp("dve", ygT[b2].rearrange("p f t -> p (f t)"), PSB(bT)[:, 0:512], [pk(bT)], [("ygT", b2)])
                yield
                for fb in range(4):
                    K.op("pe", lambda e, fb=fb, b2=b2, bG=bG: e.matmul(PS(bG), lhsT=ygT[b2][:, fb, :], rhs=w_glu_bf[:, fb, :], start=(fb == 0), stop=(fb == 3)),
                         reads=[("ygT", b2), "w_glu_bf"], writes=[pk(bG)])
                K.op("act", lambda e, b2=b2, bG=bG: e.activation(out=sg[b2], in_=PS(bG), func=AF.Sigmoid), reads=[pk(bG)], writes=[("sg", b2)])
                K.op("dve", lambda e, b2=b2: e.tensor_tensor(out=y2[b2], in0=ygf[b2], in1=sg[b2], op=ALU.mult), reads=[("ygf", b2), ("sg", b2)], writes=[("y2", b2)])
                yield
                rms_prep(100 + tl, ("y2", b2), y2[b2], 128, 512.0, pstat[:, tl, 0:4], pj[b2][:, 0:512], y2n[b2], ("y2n", b2), junk_key=("pj", b2))
                yield
                for fb in range(4):
                    K.op("pe", lambda e, fb=fb, b2=b2, bT=bT: e.transpose(PSB(bT)[:, 512 + fb * 128:512 + (fb + 1) * 128], y2n[b2][:, fb * 128:(fb + 1) * 128], ident_b),
                         reads=[("y2n", b2), "ident_b"], writes=[pk(bT)])
                cp("dve", y2nT[b2].rearrange("p f t -> p (f t)"), PSB(bT)[:, 512:1024], [pk(bT)], [("y2nT", b2)])
                yield
                for hf in range(2):
                    pb = bO
                    for kb in range(8):
                        lhsT = y2nT[b2][:, kb, :] if kb < 4 else fnTt5[b4][:, kb - 4, :]
                        K.op("pe", lambda e, hf=hf, kb=kb, pb=pb, lhsT=lhsT: e.matmul(
                            PS(pb), lhsT=lhsT, rhs=w_out_bf[:, kb, hf * 512:(hf + 1) * 512], start=(kb == 0), stop=(kb == 7)),
                            reads=[("y2nT", b2), ("fnTt5", b4), "w_out_bf"], writes=[pk(pb)])
                    K.op("dve", lambda e, hf=hf, pb=pb, b2=b2: e.tensor_tensor(out=x1[b2][:, hf * 512:(hf + 1) * 512], in0=PS(pb), in1=g1_bc[:, hf * 512:(hf + 1) * 512], op=ALU.mult),
                         reads=[pk(pb), "g1_bc"], writes=[("x1", b2)])
                yield
                K.op("pool", lambda e, b2=b2, b4=b4: e.tensor_tensor(out=x1[b2], in0=x1[b2], in1=xr[b4], op=ALU.add), reads=[("x1", b2), ("xr", b4)], writes=[("x1", b2)])
                K.dma("sp", x1_r[s, j * 128:(j + 1) * 128, :], x1[b2], reads=[("x1", b2)], writes=[("x1scr", tl)])
                if tl == 0 and "d_x1" in dbg_out:
                    K.dma("sp", dbg_out["d_x1"], x1[0], reads=[("x1", 0)], writes=[("dbg", 8)])
                yield
                rms_prep(200 + tl, ("x1", b2), x1[b2], 128, 1024.0, pstat[:, tl, 4:8], pj[b2], xs2[b2], ("xs2", b2), junk_key=("pj", b2))
                yield
                for kc in range(8):
                    K.op("pe", lambda e, kc=kc, b2=b2, bT=bT: e.transpose(PSB(bT)[:, kc * 128:(kc + 1) * 128], xs2[b2][:, kc * 128:(kc + 1) * 128], ident_b),
                         reads=[("xs2", b2), "ident_b"], writes=[pk(bT)])
                psv = PSB(bT).rearrange("p (k t) -> p k t", k=8)
                K.op("dve", lambda e, psv=psv, b3=b3: e.tensor_tensor(out=tTf[b3], in0=psv, in1=vecs[:, 4, :].unsqueeze(2).to_broadcast([128, 8, 128]), op=ALU.mult),
                     reads=[pk(bT), "vecs"], writes=[("tTf", b3)])
                yield
                K.op("pool", lambda e, b3=b3: e.tensor_tensor(out=tTf[b3], in0=tTf[b3], in1=vecs[:, 5, :].unsqueeze(2).to_broadcast([128, 8, 128]), op=ALU.add),
                     reads=[("tTf", b3), "vecs"], writes=[("tTf", b3)])
                cp("act", tT[b2], tTf[b3], [("tTf", b3)], [("tT", b2)])
                K.dma("sp", tT_scr[:, :, tl * 128:(tl + 1) * 128].rearrange("k p t -> p k t"), tT[b2], reads=[("tT", b2)], writes=[("tTscr", tl)])
                yield
                for kc in range(8):
                    K.op("pe", lambda e, kc=kc, b3=b3, bG=bG: e.matmul(PS(bG, 36), lhsT=tTf[b3][:, kc, :], rhs=wrt[:, kc, :], start=(kc == 0), stop=(kc == 7)),
                         reads=[("tTf", b3), "wrt"], writes=[pk(bG)])
                K.op("dve", lambda e, tl=tl, bG=bG: e.tensor_tensor(out=lgall[:, tl, :], in0=PS(bG, 36), in1=brt_bc, op=ALU.add), reads=[pk(bG), "brt_bc"], writes=["lgall"])

            for t_ in range(4):
                p5_loads(t_)
            for pr in range(8):
                if pr + 1 < 8:
                    for t_ in range(4):
                        p5_loads(4 * pr + 4 + t_)
                gens = [p5_tile(4 * pr + q) for q in range(4)]
                alive = [True] * 4
                while any(alive):
                    for gi_, g_ in enumerate(gens):
                        if alive[gi_]:
                            try:
                                next(g_)
                            except StopIteration:
                                alive[gi_] = False
            K.barrier()
            A.top = p5tmp
            NTL = 32
            ra = lambda n: A.alloc([NTL, n], F32)
            gmax = ra(1); sel = ra(4); exg = ra(4); gsum = ra(1); gw = ra(1); t8 = ra(32); es_ = ra(8); m1 = ra(1); mk1 = ra(8)
            es2 = ra(8); m2 = ra(1); mk2 = ra(8); dd = ra(1); exd = ra(1); den = ra(1); w1 = ra(1); w2 = ra(1); comb = ra(8); cb2 = ra(8)
            gl3 = lgall[:, :, 0:4]; el4 = lgall[:, :, 4:36].rearrange("p t (g e) -> p t g e", g=4)

            def R(fn, eng="dve"):
                K.op(eng, fn, reads=["rt", "lgall"], writes=["rt"])
            bc4 = lambda a: a[:, :, 0:1].to_broadcast([128, NTL, 4])
            bc8 = lambda a: a[:, :, 0:1].to_broadcast([128, NTL, 8])
            R(lambda e: e.tensor_reduce(out=gmax[:, :, 0], in_=gl3, axis=AX.X, op=ALU.max))
            R(lambda e: e.tensor_tensor(out=sel, in0=gl3, in1=bc4(gmax), op=ALU.is_equal))
            R(lambda e: e.tensor_tensor(out=exg, in0=gl3, in1=bc4(gmax), op=ALU.subtract))
            R(lambda e: e.activation(out=exg, in_=exg, func=AF.Exp), eng="act")
            R(lambda e: e.tensor_reduce(out=gsum[:, :, 0], in_=exg, axis=AX.X, op=ALU.add))
            R(lambda e: e.reciprocal(out=gw, in_=gsum))
            R(lambda e: e.tensor_tensor(out=t8.rearrange("p t (g e) -> p t g e", g=4), in0=el4, in1=sel.unsqueeze(3).to_broadcast([128, NTL, 4, 8]), op=ALU.mult))
            R(lambda e: e.tensor_reduce(out=es_, in_=t8.rearrange("p t (g e) -> p t e g", g=4), axis=AX.X, op=ALU.add))
            R(lambda e: e.tensor_reduce(out=m1[:, :, 0], in_=es_, axis=AX.X, op=ALU.max))
            R(lambda e: e.tensor_tensor(out=mk1, in0=es_, in1=bc8(m1), op=ALU.is_equal))
            R(lambda e: e.scalar_tensor_tensor(out=es2, in0=mk1, scalar=-1e30, in1=es_, op0=ALU.mult, op1=ALU.add))
            R(lambda e: e.tensor_reduce(out=m2[:, :, 0], in_=es2, axis=AX.X, op=ALU.max))
            R(lambda e: e.tensor_tensor(out=mk2, in0=es2, in1=bc8(m2), op=ALU.is_equal))
            R(lambda e: e.tensor_tensor(out=dd, in0=m2, in1=m1, op=ALU.subtract))
            R(lambda e: e.activation(out=exd, in_=dd, func=AF.Exp), eng="act")
            R(lambda e: e.tensor_scalar(out=den, in0=exd, scalar1=1.0, scalar2=None, op0=ALU.add))
            R(lambda e: e.reciprocal(out=w1, in_=den))
            R(lambda e: e.tensor_tensor(out=w1, in0=w1, in1=gw, op=ALU.mult))
            R(lambda e: e.tensor_tensor(out=w2, in0=w1, in1=exd, op=ALU.mult))
            R(lambda e: e.tensor_tensor(out=comb, in0=mk1, in1=bc8(w1), op=ALU.mult))
            R(lambda e: e.tensor_tensor(out=cb2, in0=mk2, in1=bc8(w2), op=ALU.mult))
            R(lambda e: e.tensor_tensor(out=comb, in0=comb, in1=cb2, op=ALU.add))
            K.op("dve", lambda e: e.tensor_tensor(out=G.rearrange("p t (g e) -> p t g e", g=4),
                                                  in0=sel.unsqueeze(3).to_broadcast([128, NTL, 4, 8]),
                                                  in1=comb.unsqueeze(2).to_broadcast([128, NTL, 4, 8]), op=ALU.mult),
                 reads=["rt"], writes=[("G", tl_) for tl_ in range(32)])
            if "d_G" in dbg_out:
                K.dma("sp", dbg_out["d_G"], G[:, 0, :], reads=[("G", 0)], writes=[("dbg", 9)])
            K.barrier()

            chk(5)
            A.top = p5keep
            tTh2 = [A.alloc([8, 2048], BF16) for _ in range(2)]
            acc = A.alloc([16, 1024], F32)
            wg = [A.alloc([8, 256], BF16) for _ in range(2)]
            wu = [A.alloc([8, 256], BF16) for _ in range(2)]
            wd = [A.alloc([2, 1024], BF16) for _ in range(2)]
            sl = [A.alloc([512], F32) for _ in range(2)]
            aT = [A.alloc([2, 512], BF16) for _ in range(2)]
            ob = [A.alloc([1024], F32) for _ in range(2)]
            ostat = A.alloc([32, 4], F32)
            ojunk = pjunk
            g2b = A.alloc([1024], BF16)
            K.op("dve", lambda e: e.tensor_copy(out=g2b, in_=g2_bc), reads=["g2_bc"], writes=["g2b"])
            def acc_load(hh, tt):
                tl = hh * 16 + tt
                j, s = tl // 8, tl % 8
                K.dma("act", acc[:, tt, :], x1_r[s, j * 128:(j + 1) * 128, :], reads=[("x1scr", tl)], writes=[("acc", tt)])

            for hh in range(2):
                K.dma("sp", tTh2[hh], tT_scr[:, :, hh * 2048:(hh + 1) * 2048].rearrange("k p t -> p k t"),
                      reads=[("tTscr", tl) for tl in range(hh * 16, hh * 16 + 16)], writes=[("tTh", hh)])
            for tt in range(16):
                acc_load(0, tt)

            def wload(ex_, wi):
                b2 = wi % 2
                K.dma("pool", wg[b2], w_gate[ex_].rearrange("(kc p) f -> p kc f", p=128), writes=[("wg", b2)])
                K.dma("pool", wu[b2], w_up[ex_].rearrange("(kc p) f -> p kc f", p=128), writes=[("wu", b2)])
                K.dma("pool", wd[b2], w_down[ex_].rearrange("(fb p) n -> p fb n", p=128), writes=[("wd", b2)])
                K.op("pool", lambda e, b2=b2: e.tensor_tensor(out=wd[b2], in0=wd[b2], in1=g2b.unsqueeze(1).to_broadcast([128, 2, 1024]), op=ALU.mult),
                     reads=[("wd", b2), "g2b"], writes=[("wd", b2)])

            def hgu(hh, ex_, tg, it_):
                b2 = (hh * 32 + ex_) % 2
                ab = it_ % 2
                tTh = tTh2[hh]
                for fb in range(2):
                    pg, pu_ = 0 + fb, 2 + fb
                    for kc in range(8):
                        K.op("pe", lambda e, kc=kc, fb=fb, b2=b2, tg=tg, pg=pg, tTh=tTh: e.matmul(
                            PS(pg), lhsT=wg[b2][:, kc, fb * 128:(fb + 1) * 128], rhs=tTh[:, kc, tg * 512:(tg + 1) * 512], start=(kc == 0), stop=(kc == 7)),
                            reads=[("wg", b2), ("tTh", hh)], writes=[pk(pg)])
                    for kc in range(8):
                        K.op("pe", lambda e, kc=kc, fb=fb, b2=b2, tg=tg, pu_=pu_, tTh=tTh: e.matmul(
                            PS(pu_), lhsT=wu[b2][:, kc, fb * 128:(fb + 1) * 128], rhs=tTh[:, kc, tg * 512:(tg + 1) * 512], start=(kc == 0), stop=(kc == 7)),
                            reads=[("wu", b2), ("tTh", hh)], writes=[pk(pu_)])
                    K.op("act", lambda e, fb=fb, pg=pg: e.activation(out=sl[fb], in_=PS(pg), func=AF.Silu), reads=[pk(pg)], writes=[("sl", fb)])
                    K.op("dve", lambda e, fb=fb, pu_=pu_, ab=ab: e.tensor_tensor(out=aT[ab][:, fb, :], in0=sl[fb], in1=PS(pu_), op=ALU.mult),
                         reads=[("sl", fb), pk(pu_)], writes=[("aT", ab, fb)])

            def down(hh, ex_, tg, it_):
                b2 = (hh * 32 + ex_) % 2
                ab = it_ % 2
                for t4 in range(4):
                    tt = tg * 4 + t4
                    tl = hh * 16 + tt
                    for hf in range(2):
                        po = 4 + (t4 * 2 + hf) % 4
                        for fb in range(2):
                            K.op("pe", lambda e, fb=fb, hf=hf, po=po, ab=ab, t4=t4, b2=b2: e.matmul(
                                PS(po), lhsT=aT[ab][:, fb, t4 * 128:(t4 + 1) * 128], rhs=wd[b2][:, fb, hf * 512:(hf + 1) * 512], start=(fb == 0), stop=(fb == 1)),
                                reads=[("aT", ab, 0), ("aT", ab, 1), ("wd", b2)], writes=[pk(po)])
                        K.op("dve", lambda e, po=po, tt=tt, hf=hf, tl=tl, ex_=ex_: e.scalar_tensor_tensor(
                            out=acc[:, tt, hf * 512:(hf + 1) * 512], in0=PS(po), scalar=G[:, tl, ex_:ex_ + 1], in1=acc[:, tt, hf * 512:(hf + 1) * 512],
                            op0=ALU.mult, op1=ALU.add), reads=[pk(po), ("G", tl), ("acc", tt)], writes=[("acc", tt)])

            def final_norm(hh, tt):
                tl = hh * 16 + tt
                j, s = tl // 8, tl % 8
                b2 = tt % 2
                K.op("act", lambda e, tt=tt, tl=tl: e.activation(out=ojunk, in_=acc[:, tt, :], func=AF.Square, accum_out=ostat[:, tl, 0:1]),
                     reads=[("acc", tt)], writes=["ojunk", ("ostat", tl)])
                K.op("act", lambda e, tl=tl: e.activation(out=ostat[:, tl, 1:2], in_=ostat[:, tl, 0:1], func=AF.Sqrt, scale=1.0 / 1024.0, bias=small[:, 0:1]),
                     reads=[("ostat", tl), "small"], writes=[("ostat", tl)])
                K.op("dve", lambda e, tl=tl: e.reciprocal(out=ostat[:, tl, 2:3], in_=ostat[:, tl, 1:2]), reads=[("ostat", tl)], writes=[("ostat", tl)])
                K.op("dve", lambda e, tt=tt, tl=tl, b2=b2: e.scalar_tensor_tensor(out=ob[b2], in0=acc[:, tt, :], scalar=ostat[:, tl, 2:3], in1=fin_bc, op0=ALU.mult, op1=ALU.mult),
                     reads=[("acc", tt), ("ostat", tl), "fin_bc"], writes=[("ob", b2)])
                K.dma("sp", y_r[s, j * 128:(j + 1) * 128, :], ob[b2], reads=[("ob", b2)], writes=[("y", tl)])

            items = [(hh, ex_, tg) for hh in range(2) for ex_ in range(32) for tg in range(4)]
            wload(0, 0)
            hgu(0, 0, 0, 0)
            for it_ in range(len(items)):
                hh, ex_, tg = items[it_]
                if it_ + 1 < len(items):
                    nh, ne, ntg = items[it_ + 1]
                    if ntg == 0:
                        wload(ne, nh * 32 + ne)
                    hgu(nh, ne, ntg, it_ + 1)
                down(hh, ex_, tg, it_)
                if ex_ == 31:
                    for t4 in range(4):
                        final_norm(hh, tg * 4 + t4)
                        if hh == 0:
                            acc_load(1, tg * 4 + t4)
        except _Stop:
            pass
        K.finish()

        @block.tensor
        def _(e):
            for f in K.eng["pe"]["prog"]:
                f(e)

        @block.scalar
        def _(e):
            for f in K.eng["act"]["prog"]:
                f(e)

        @block.vector
        def _(e):
            for f in K.eng["dve"]["prog"]:
                f(e)

        @block.gpsimd
        def _(e):
            for f in K.eng["pool"]["prog"]:
                f(e)

        @block.sync
        def _(e):
            for f in K.eng["sp"]["prog"]:
                f(e)
    return nc


_CONST = None


def _consts():
    global _CONST
    if _CONST is not None:
        return _CONST
    c = {}
    c["c_ident"] = np.eye(128, dtype=np.float32)
    s_in = np.arange(128) // 16
    mf = (s_in[:, None] <= s_in[None, :]).astype(np.float32)
    mr = (s_in[:, None] >= s_in[None, :]).astype(np.float32)
    c["c_masks"] = np.concatenate([mf, mr], axis=1)
    kid = np.zeros((128, 18), np.float32)
    s = np.arange(8)
    kid[0:64, 0:8] = s + 1
    kid[64:128, 0:8] = 8 - s
    kid[:, 8:16] = 8 - kid[:, 0:8]
    kid[:, 16] = 1
    kid[:, 17] = 8
    c["c_kidx"] = kid
    rep = np.zeros((16, 128), np.float32)
    for p in range(128):
        rep[p % 16, p] = 1.0
    c["c_rep16"] = rep
    jj = np.arange(128)
    ang = 2 * np.pi * np.outer(jj, jj) / 128.0
    sc = 1.0 / math.sqrt(L * 128.0)
    c["c_dftc"] = np.concatenate([np.cos(ang) * sc, np.sin(ang) * sc], axis=1).astype(np.float32)
    ii = np.arange(128)
    tab = np.zeros((128, 2, 64, 128), dtype=ml_dtypes.bfloat16)
    for s_ in range(8):
        for s2 in range(8):
            mm = ((8 * ii[:, None] + s_) * (8 * ii[None, :] + s2)) % L
            a = 2 * np.pi * mm / L
            tab[:, 0, s_ * 8 + s2, :] = np.cos(a).astype(ml_dtypes.bfloat16)
            tab[:, 1, s_ * 8 + s2, :] = np.sin(a).astype(ml_dtypes.bfloat16)
    c["c_tab"] = tab
    _CONST = c
    return c


_NC_CACHE = {}


def _get_nc(dbg=()):
    key = tuple(dbg)
    if key not in _NC_CACHE:
        _NC_CACHE[key] = build_nc(dbg)
    return _NC_CACHE[key]


def make_in_maps(inputs, cores):
    c = _consts()
    f = lambda a: np.ascontiguousarray(np.asarray(a, dtype=np.float32))
    shared = {
        "w_ada": f(inputs["w_ada"][0]), "b_ada": f(inputs["b_ada"][0]), "w_in": f(inputs["w_in"][0]),
        "lam_re": f(inputs["s5_lam_re"][0]), "lam_im": f(inputs["s5_lam_im"][0]),
        "b_re": f(inputs["s5_b_re"][0]), "b_im": f(inputs["s5_b_im"][0]),
        "c_re": f(inputs["s5_c_re"][0]), "c_im": f(inputs["s5_c_im"][0]),
        "log_step": f(inputs["s5_log_step"][0]), "s5_d": f(inputs["s5_d"][0]),
        "w_glu": f(inputs["s5_w_glu"][0]), "four_w": f(inputs["fourier_w"][0]), "w_out": f(inputs["w_out"][0]),
        "w_rt": f(np.concatenate([np.asarray(inputs["moe_w_group"][0]), np.asarray(inputs["moe_w_router"][0]).reshape(D, 32)], axis=1)),
        "b_rt": f(np.concatenate([np.asarray(inputs["moe_b_group"][0]), np.asarray(inputs["moe_b_router"][0]).reshape(32)])),
        "w_gate": f(inputs["moe_w_gate"][0]), "w_up": f(inputs["moe_w_up"][0]), "w_down": f(inputs["moe_w_down"][0]),
        "fin_g": f(inputs["final_norm_g"]),
    }
    shared.update(c)
    maps = []
    for b in cores:
        rows = np.zeros((128, 128), np.float32)
        rows[0:8] = np.asarray(inputs["c"][b]).reshape(8, 128)
        rows[8:16] = np.asarray(inputs["c_ctx"]).reshape(8, 128)
        rows[16:24] = np.asarray(inputs["norm1_g"][0]).reshape(8, 128)
        rows[24:32] = np.asarray(inputs["norm2_g"][0]).reshape(8, 128)
        rows[32:80] = np.asarray(inputs["b_ada"][0]).reshape(48, 128)
        rows[80:84] = np.asarray(inputs["mix_norm_s5_g"][0]).reshape(4, 128)
        rows[84:88] = np.asarray(inputs["mix_norm_f_g"][0]).reshape(4, 128)
        m = dict(shared)
        m["x"] = f(inputs["x"][b])
        m["ctx"] = f(inputs["ctx"][b])
        m["rows_in"] = rows
        maps.append(m)
    return maps


def kernel(**inputs):
    nc = _get_nc()
    maps = make_in_maps(inputs, list(range(8)))
    res = run_bass_kernel_spmd(nc, maps, core_ids=list(range(8)))
    return np.stack([np.asarray(r["y"], dtype=np.float32) for r in res.results], axis=0)
```

```python
import math
import numpy as np
import ml_dtypes
import concourse.bass as bass
import concourse.mybir as mybir
from concourse.bass_utils import run_bass_kernel_spmd

F32 = mybir.dt.float32
BF16 = mybir.dt.bfloat16
I32 = mybir.dt.int32
AF = mybir.ActivationFunctionType
ALU = mybir.AluOpType
AX = mybir.AxisListType

L = 4096
D = 1024
NCH = 512
NCX = 32
NE = 544
EPS = 1e-6
TWO_PI = 2.0 * math.pi
ARENA = 106000

DEBUG = False
SELF_ORDERED = ("pe",)


class Sched:
    def __init__(self, nc, sems, dma_sems):
        self.nc = nc
        self.eng = {n: dict(prog=[], sem=sems[n], count=0, known={}) for n in ("pe", "act", "dve", "pool", "sp")}
        self.dma_sems = dma_sems
        self.dma_counts = [0] * len(dma_sems)
        self.dma_rr = 0
        self.dma_rr_sw = 0
        self.last_w = {}
        self.readers = {}

    def _deps(self, reads, writes):
        toks = []
        for k in reads:
            t = self.last_w.get(k)
            if t:
                toks.append(t)
        for k in writes:
            t = self.last_w.get(k)
            if t:
                toks.append(t)
            toks += list(self.readers.get(k, {}).values())
        return toks

    def _waits(self, ename, toks):
        E = self.eng[ename]
        need = {}
        for (sid, sem, val, src) in toks:
            if src == ename and ename in SELF_ORDERED:
                continue
            if E["known"].get(sid, 0) >= val:
                continue
            if need.get(sid, (None, 0))[1] < val:
                need[sid] = (sem, val)
        out = []
        for sid, (sem, val) in need.items():
            E["known"][sid] = val
            out.append((sem, val))
        return out

    def _commit(self, tok, reads, writes):
        for k in writes:
            self.last_w[k] = tok
            self.readers[k] = {}
        for k in reads:
            d = self.readers.setdefault(k, {})
            o = d.get(tok[0])
            if o is None or o[2] < tok[2]:
                d[tok[0]] = tok

    def op(self, ename, fn, reads=(), writes=()):
        E = self.eng[ename]
        waits = self._waits(ename, self._deps(reads, writes))
        E["count"] += 1
        val = E["count"]
        sem = E["sem"]

        def run(e, fn=fn, waits=waits, sem=sem):
            for (s, v) in waits:
                e.wait_ge(s, v)
            fn(e).then_inc(sem, 1)
        E["prog"].append(run)
        tok = (id(sem), sem, val, ename)
        self._commit(tok, reads, writes)
        return tok

    def dma(self, q, out, in_, reads=(), writes=(), **kw):
        E = self.eng[q]
        nsw = 12
        nhw = len(self.dma_sems) - nsw
        if q == "pool":
            i = nhw + self.dma_rr_sw
            self.dma_rr_sw = (self.dma_rr_sw + 1) % nsw
        else:
            i = self.dma_rr
            self.dma_rr = (i + 1) % nhw
        sem = self.dma_sems[i]
        prev = self.dma_counts[i]
        toks = self._deps(reads, writes)
        if prev > 0:
            toks.append((id(sem), sem, 16 * prev, "dma"))
        waits = self._waits(q, toks)
        self.dma_counts[i] += 1
        val = 16 * self.dma_counts[i]

        def run(e, waits=waits, sem=sem, out=out, in_=in_, kw=kw):
            for (s, v) in waits:
                e.wait_ge(s, v)
            e.dma_start(out=out, in_=in_, **kw).then_inc(sem, 16)
        E["prog"].append(run)
        tok = (id(sem), sem, val, "dma")
        self._commit(tok, reads, writes)
        return tok

    def barrier(self):
        toks = []
        for n, E in self.eng.items():
            if E["count"] > 0:
                toks.append((id(E["sem"]), E["sem"], E["count"], "bar"))
        for i, s in enumerate(self.dma_sems):
            if self.dma_counts[i] > 0:
                toks.append((id(s), s, 16 * self.dma_counts[i], "bar"))
        for n, E in self.eng.items():
            waits = self._waits(n, toks)
            if waits:
                def run(e, waits=waits):
                    for (s, v) in waits:
                        e.wait_ge(s, v)
                E["prog"].append(run)

    def finish(self):
        toks = []
        for i, s in enumerate(self.dma_sems):
            if self.dma_counts[i] > 0:
                toks.append((id(s), s, 16 * self.dma_counts[i], "bar"))
        waits = self._waits("sp", toks)

        def run(e, waits=waits):
            for (s, v) in waits:
                e.wait_ge(s, v)
        self.eng["sp"]["prog"].append(run)


class Arena:
    def __init__(self, ap):
        self.ap = ap
        self.top = 0

    def alloc(self, shape, dtype, parts=128):
        n = 1
        for d in shape:
            n *= d
        nb = n * (2 if dtype == BF16 else 4)
        nb = (nb + 63) // 64 * 64
        off = self.top
        self.top += nb // 2
        assert self.top <= ARENA, f"arena overflow {self.top}"
        v = self.ap[0:parts, off:off + nb // 2]
        if dtype != BF16:
            v = v.bitcast(dtype)
        v = v[:, 0:n]
        if len(shape) == 2:
            return v.rearrange("p (a b) -> p a b", a=shape[0])
        if len(shape) == 3:
            return v.rearrange("p (a b c) -> p a b c", a=shape[0], b=shape[1])
        if len(shape) == 4:
            return v.rearrange("p (a b c d) -> p a b c d", a=shape[0], b=shape[1], c=shape[2])
        return v


def build_nc(dbg=(), stop_after=99):
    nc = bass.Bass("TRN2", target_bir_lowering=False)

    def din(name, shape, dt=F32):
        return nc.dram_tensor(name, list(shape), dt, kind="ExternalInput").ap()

    x = din("x", [L, D])
    ctx = din("ctx", [256, D])
    rows_in = din("rows_in", [128, 128])
    w_ada = din("w_ada", [D, 6 * D])
    b_ada = din("b_ada", [6 * D])
    w_in = din("w_in", [D, D])
    lam_re = din("lam_re", [2, 32, 64])
    lam_im = din("lam_im", [2, 32, 64])
    b_re = din("b_re", [2, 32, 64, 16])
    b_im = din("b_im", [2, 32, 64, 16])
    c_re = din("c_re", [2, 32, 16, 64])
    c_im = din("c_im", [2, 32, 16, 64])
    log_step = din("log_step", [2, 32])
    s5_d = din("s5_d", [512])
    w_glu = din("w_glu", [512, 512])
    four_w = din("four_w", [4, 128, 128])
    w_out = din("w_out", [D, D])
    w_rt = din("w_rt", [D, 36])
    b_rt = din("b_rt", [36])
    w_gate = din("w_gate", [32, D, 256])
    w_up = din("w_up", [32, D, 256])
    w_down = din("w_down", [32, 256, D])
    fin_g = din("fin_g", [D])
    c_ident = din("c_ident", [128, 128])
    c_masks = din("c_masks", [128, 256])
    c_kidx = din("c_kidx", [128, 18])
    c_rep16 = din("c_rep16", [16, 128])
    c_dftc = din("c_dftc", [128, 256])
    c_tab = din("c_tab", [128, 2, 64, 128], BF16)
    y = nc.dram_tensor("y", [L, D], F32, kind="ExternalOutput").ap()
    x1_scr = nc.dram_tensor("x1_scr", [L, D], F32, kind="Internal").ap()
    tT_scr = nc.dram_tensor("tT_scr", [8, 128, L], BF16, kind="Internal").ap()
    fT_scr = nc.dram_tensor("fT_scr", [32, 128, 4, 128], BF16, kind="Internal").ap()
    y5_scr = nc.dram_tensor("y5_scr", [L, 512], F32, kind="Internal").ap()
    dbg_out = {}
    for name, shape in dbg:
        dbg_out[name] = nc.dram_tensor(name, list(shape), F32, kind="ExternalOutput").ap()

    from contextlib import ExitStack
    with ExitStack() as es:
        arena_t = es.enter_context(nc.sbuf_tensor("arena", [128, ARENA], BF16))
        ps_t = es.enter_context(nc.psum_tensor("ps", [128, 4096], F32))
        sems = {n: es.enter_context(nc.semaphore("sem_" + n)) for n in ("pe", "act", "dve", "pool", "sp")}
        dma_sems = [es.enter_context(nc.semaphore(f"dsem{i}")) for i in range(40)]
        block = es.enter_context(nc.Block())
        K = Sched(nc, sems, dma_sems)
        A = Arena(arena_t)

        def PS(b, n=512, off=0):
            return ps_t[:, b * 512 + off: b * 512 + off + n]

        def PSB(b):
            return ps_t[:, b * 512:(b + 1) * 512].bitcast(BF16)

        def pk(b):
            return ("ps", b)

        def cp(eng, out, in_, reads, writes):
            if eng == "act":
                K.op("act", lambda e: e.activation(out=out, in_=in_, func=AF.Identity), reads=reads, writes=writes)
            else:
                K.op(eng, lambda e: e.tensor_copy(out=out, in_=in_), reads=reads, writes=writes)

        def dump(name, src_ap, key):
            if name in dbg_out:
                K.dma("sp", dbg_out[name], src_ap, reads=[key], writes=[("dbg", name)])

        ident_f = A.alloc([128], F32)
        ident_b = A.alloc([128], BF16)
        masks = A.alloc([256], F32)
        kidx = A.alloc([18], F32)
        rep16 = A.alloc([128], F32)
        dftc = A.alloc([256], F32)
        cols = A.alloc([128], F32)
        scol = A.alloc([16], F32)
        modc = A.alloc([48, 2], F32)
        vecs = A.alloc([8, 8], F32)
        g1_bc = A.alloc([1024], F32)
        g2_bc = A.alloc([1024], F32)
        fin_bc = A.alloc([1024], F32)
        brt_bc = A.alloc([36], F32)
        Dcol = A.alloc([32], F32)
        AcAs = A.alloc([4, 256], BF16)
        wrt = A.alloc([8, 36], F32)
        small = A.alloc([64], F32)
        persist_top = A.top

        class _Stop(Exception):
            pass

        def chk(k):
            if stop_after == k:
                raise _Stop()

        try:
            K.dma("sp", ident_f, c_ident, writes=["ident_f"])
            K.dma("sp", masks, c_masks, writes=["masks"])
            K.dma("sp", kidx, c_kidx, writes=["kidx"])
            K.dma("sp", rep16[0:16, :], c_rep16, writes=["rep16"])
            K.dma("sp", dftc, c_dftc, writes=["dftc"])
            K.dma("sp", g1_bc, b_ada[2048:3072].partition_broadcast(128), writes=["g1_bc"])
            K.dma("sp", g2_bc, b_ada[5120:6144].partition_broadcast(128), writes=["g2_bc"])
            K.dma("sp", fin_bc, fin_g.partition_broadcast(128), writes=["fin_bc"])
            K.dma("sp", brt_bc, b_rt.partition_broadcast(128), writes=["brt_bc"])
            K.dma("sp", wrt, w_rt.rearrange("(kc p) n -> p kc n", p=128), writes=["wrt"])
            K.op("dve", lambda e: e.tensor_copy(out=ident_b, in_=ident_f), reads=["ident_f"], writes=["ident_b"])

            p2start = A.top
            U = A.alloc([32, 576], BF16)
            PQ = A.alloc([32, 1024], BF16)
            p2mark = A.top
            w_in_bf = A.alloc([8, 1024], BF16)
            w_in_r = w_in.rearrange("(kc p) n -> p kc n", p=128)
            for kc in range(8):
                K.dma("pool", w_in_bf[:, kc, :], w_in_r[:, kc, :], writes=["w_in_bf"])
            mark = A.top
            rows_sb = A.alloc([128], F32)
            srep = A.alloc([8, 128], F32)
            wa = [A.alloc([8, 512], F32) for _ in range(2)]
            K.dma("sp", rows_sb, rows_in, writes=["rows_sb"])
            K.op("pe", lambda e: e.transpose(PS(0, 128), rows_sb, ident_f), reads=["rows_sb", "ident_f"], writes=[pk(0)])
            K.op("dve", lambda e: e.tensor_copy(out=cols, in_=PS(0, 128)), reads=[pk(0)], writes=["cols"])
            K.op("act", lambda e: e.activation(out=scol, in_=cols[:, 0:16], func=AF.Silu), reads=["cols"], writes=["scol"])
            K.op("dve", lambda e: e.tensor_copy(out=srep, in_=scol[:, 0:8].unsqueeze(2).to_broadcast([128, 8, 128])),
                 reads=["scol"], writes=["srep"])
            wa_rr = [w_ada.rearrange("(kc p) n -> p kc n", p=128)[:, :, cc * 512:(cc + 1) * 512] for cc in range(12)]
            for cc in range(12):
                buf = wa[cc % 2]
                K.dma("sp" if cc % 2 == 0 else "act", buf, wa_rr[cc], writes=[("wa", cc % 2)])
                for fb in range(4):
                    j = cc * 4 + fb
                    for kc in range(8):
                        K.op("pe", lambda e, buf=buf, fb=fb, kc=kc, j=j: e.matmul(
                            PS(1, 2, 2 * j), lhsT=buf[:, kc, fb * 128:(fb + 1) * 128], rhs=scol[:, kc:16:8],
                            start=(kc == 0), stop=(kc == 7)),
                            reads=[("wa", cc % 2), "scol"], writes=[pk(1)])
                if cc in (4, 5, 10, 11):
                    tgt = g1_bc if cc in (4, 5) else g2_bc
                    tk = "g1_bc" if cc in (4, 5) else "g2_bc"
                    half = cc % 2 if cc in (4, 5) else (cc - 10)
                    for kc in range(8):
                        K.op("pe", lambda e, buf=buf, kc=kc: e.matmul(
                            PS(2), lhsT=srep[:, kc, :], rhs=buf[:, kc, :], start=(kc == 0), stop=(kc == 7)),
                            reads=[("wa", cc % 2), "srep"], writes=[pk(2)])
                    K.op("dve", lambda e, tgt=tgt, half=half: e.tensor_tensor(
                        out=tgt[:, half * 512:(half + 1) * 512], in0=PS(2), in1=tgt[:, half * 512:(half + 1) * 512], op=ALU.add),
                        reads=[pk(2), tk], writes=[tk])
            K.op("dve", lambda e: e.tensor_tensor(
                out=modc, in0=PS(1, 96).rearrange("p (j t) -> p j t", t=2),
                in1=cols[:, 32:80].unsqueeze(2).to_broadcast([128, 48, 2]), op=ALU.add),
                reads=[pk(1), "cols"], writes=["modc"])
            def vec_ops(e_name, dst, g_lo, sc_j, which):
                K.op("dve", lambda e: e.scalar_tensor_tensor(
                    out=vecs[:, dst, :], in0=modc[:, sc_j:sc_j + 8, which], scalar=1.0, in1=cols[:, g_lo:g_lo + 8],
                    op0=ALU.add, op1=ALU.mult), reads=["modc", "cols"], writes=["vecs"])
            vec_ops("dve", 0, 16, 8, 0)
            vec_ops("dve", 2, 16, 8, 1)
            vec_ops("dve", 4, 24, 32, 0)
            K.op("dve", lambda e: e.tensor_copy(out=vecs[:, 1, :], in_=modc[:, 0:8, 0]), reads=["modc"], writes=["vecs"])
            K.op("dve", lambda e: e.tensor_copy(out=vecs[:, 3, :], in_=modc[:, 0:8, 1]), reads=["modc"], writes=["vecs"])
            K.op("dve", lambda e: e.tensor_copy(out=vecs[:, 5, :], in_=modc[:, 24:32, 0]), reads=["modc"], writes=["vecs"])
            if "d_mod" in dbg_out:
                K.dma("sp", dbg_out["d_mod"], modc.rearrange("p j t -> p (j t)"), reads=["modc"], writes=[("dbg", 0)])
                K.dma("sp", dbg_out["d_g1"], g1_bc, reads=["g1_bc"], writes=[("dbg", 1)])
            drow = A.alloc([32], F32)
            with nc.allow_non_contiguous_dma(reason="tiny s5_d gather"):
                K.dma("sp", drow[0:16, :], s5_d.rearrange("(g h) -> h g", h=16), writes=["drow"], allow_slow_non_contiguous=True)
            K.op("pe", lambda e: e.matmul(PS(3, 32), lhsT=rep16[0:16, :], rhs=drow[0:16, :], start=True, stop=True),
                 reads=["rep16", "drow"], writes=[pk(3)])
            K.op("dve", lambda e: e.tensor_copy(out=Dcol, in_=PS(3, 32)), reads=[pk(3)], writes=["Dcol"])
            wf = A.alloc([4, 128], F32)
            K.dma("sp", wf, four_w.rearrange("g m d -> m g d"), writes=["wf"])
            for g in range(4):
                for t in range(2):
                    K.op("pe", lambda e, g=g, t=t: e.matmul(
                        PS(4, 128, (g * 2 + t) * 128 % 512) if False else ps_t[:, 4 * 512 + (g * 2 + t) * 128: 4 * 512 + (g * 2 + t) * 128 + 128],
                        lhsT=dftc[:, t * 128:(t + 1) * 128], rhs=wf[:, g, :], start=True, stop=True),
                        reads=["dftc", "wf"], writes=[pk(4 + (g * 2 + t) // 4)])
            K.op("dve", lambda e: e.tensor_copy(out=AcAs.rearrange("p g n -> p (g n)"), in_=ps_t[:, 4 * 512:6 * 512]),
                 reads=[pk(4), pk(5)], writes=["AcAs"])
            K.barrier()
            A.top = mark

            chk(1)
            xt = [A.alloc([1024], F32) for _ in range(4)]
            junk2 = [A.alloc([1024], BF16) for _ in range(2)]
            junk = A.alloc([1024], BF16)
            xs = [A.alloc([1024], BF16) for _ in range(2)]
            hT = [A.alloc([8, 128], BF16) for _ in range(2)]
            Zs = [A.alloc([32, 128], BF16) for _ in range(2)]
            vT = [A.alloc([4, 128], BF16) for _ in range(2)]
            stat = A.alloc([40, 4], F32)
            x_r = x.rearrange("(c s) d -> s c d", s=8)
            ctx_r = ctx.rearrange("(c s) d -> s c d", s=8)

            def rms_prep(ti, src_key, src_ap, rows, nfeat, st, junk_ap, out_bf, out_key, eng_scale="dve", junk_key="junk"):
                K.op("act", lambda e: e.activation(out=junk_ap, in_=src_ap, func=AF.Square, accum_out=st[0:rows, 0:1]),
                     reads=[src_key], writes=[junk_key, ("stat", ti)])
                K.op("act", lambda e: e.activation(out=st[0:rows, 1:2], in_=st[0:rows, 0:1], func=AF.Sqrt,
                                                   scale=1.0 / nfeat, bias=small[0:rows, 0:1]),
                     reads=[("stat", ti), "small"], writes=[("stat", ti)])
                K.op("dve", lambda e: e.reciprocal(out=st[0:rows, 2:3], in_=st[0:rows, 1:2]),
                     reads=[("stat", ti)], writes=[("stat", ti)])
                K.op(eng_scale, lambda e: e.tensor_scalar(out=out_bf, in0=src_ap, scalar1=st[0:rows, 2:3], scalar2=None, op0=ALU.mult),
                     reads=[src_key, ("stat", ti)], writes=[out_key])

            K.op("dve", lambda e: e.memset(small[:, 0:1], EPS), writes=["small"])

            tiles = [("c", s, 0) for s in range(8)] + [("x", s, j) for j in range(4) for s in range(8)]
            def p2_tile(ti):
                kind, s, j = tiles[ti]
                rows = 32 if kind == "c" else 128
                b2 = ti % 2
                b4 = ti % 4
                rms_prep(ti, ("xt", b4), xt[b4][0:rows, :], rows, 1024.0, stat[:, ti, :], junk2[b2][0:rows, :], xs[b2][0:rows, :], ("xs", b2), junk_key=("junk2", b2))
                yield
                pb = 0 + b2
                for kc in range(8):
                    K.op("pe", lambda e, kc=kc, b2=b2, pb=pb, rows=rows: e.transpose(
                        PSB(pb)[:, kc * 128: kc * 128 + rows], xs[b2][0:rows, kc * 128:(kc + 1) * 128], ident_b[0:rows, 0:rows]),
                        reads=[("xs", b2), "ident_b"], writes=[pk(pb)])
                yield
                av, sv = (2, 3) if kind == "c" else (0, 1)
                psv = PSB(pb).rearrange("p (k t) -> p k t", k=8)[:, :, 0:rows]
                K.op("dve", lambda e, psv=psv, b2=b2, rows=rows, av=av: e.tensor_tensor(
                    out=hT[b2][:, :, 0:rows], in0=psv, in1=vecs[:, av, :].unsqueeze(2).to_broadcast([128, 8, rows]), op=ALU.mult),
                    reads=[pk(pb), "vecs"], writes=[("hT", b2)])
                K.op("pool", lambda e, b2=b2, rows=rows, sv=sv: e.tensor_tensor(
                    out=hT[b2][:, :, 0:rows], in0=hT[b2][:, :, 0:rows], in1=vecs[:, sv, :].unsqueeze(2).to_broadcast([128, 8, rows]), op=ALU.add),
                    reads=[("hT", b2), "vecs"], writes=[("hT", b2)])
                yield
                zb = (ti // 8) % 2
                pz = 2 + b2
                for kc in range(8):
                    K.op("pe", lambda e, kc=kc, b2=b2, pz=pz, rows=rows: e.matmul(
                        PS(pz)[0:rows, :], lhsT=hT[b2][:, kc, 0:rows], rhs=w_in_bf[:, kc, 0:512], start=(kc == 0), stop=(kc == 7)),
                        reads=[("hT", b2), "w_in_bf"], writes=[pk(pz)])
                K.op("act", lambda e, zb=zb, s=s, pz=pz, rows=rows: e.activation(
                    out=Zs[zb][0:rows].rearrange("p g (s h) -> p g s h", s=8)[:, :, s, :],
                    in_=PS(pz)[0:rows, :].rearrange("p (g h) -> p g h", g=32), func=AF.Identity),
                     reads=[pk(pz)], writes=[("Zs", zb, s)])
                yield
                if kind == "x":
                    pv = 4 + b2
                    for g in range(4):
                        for kc in range(8):
                            K.op("pe", lambda e, g=g, kc=kc, b2=b2, pv=pv: e.matmul(
                                PS(pv, 128, g * 128), lhsT=w_in_bf[:, kc, 512 + g * 128: 512 + (g + 1) * 128], rhs=hT[b2][:, kc, :],
                                start=(kc == 0), stop=(kc == 7)), reads=[("hT", b2), "w_in_bf"], writes=[pk(pv)])
                    K.op("act", lambda e, b2=b2, pv=pv: e.activation(out=vT[b2].rearrange("p g t -> p (g t)"), in_=PS(pv), func=AF.Identity),
                         reads=[pk(pv)], writes=[("vT", b2)])
                    tl = j * 8 + s
                    for g in range(4):
                        K.op("pe", lambda e, g=g, b2=b2: e.matmul(
                            PS(6 + g // 2, 256, (g % 2) * 256), lhsT=vT[b2][:, g, :], rhs=AcAs[:, g, :], start=True, stop=True),
                            reads=[("vT", b2), "AcAs"], writes=[pk(6 + g // 2)])
                    K.op("dve", lambda e, tl=tl: e.tensor_copy(
                        out=PQ[:, tl, :].rearrange("p (r g m) -> p g r m", r=2, g=4),
                        in_=ps_t[:, 6 * 512:8 * 512].rearrange("p (g r m) -> p g r m", g=4, r=2)),
                         reads=[pk(6), pk(7)], writes=[("PQ", tl)])
                yield
                if s == 7:
                    nrow = rows
                    if kind == "c":
                        col_sets = [(0, 32), (544, 576)]
                    else:
                        col_sets = [(32 + 128 * j, 32 + 128 * (j + 1))]
                    for g8 in range(4):
                        pu = 6 + (g8 % 2)
                        for gi in range(8):
                            g = g8 * 8 + gi
                            K.op("pe", lambda e, g=g, gi=gi, zb=zb, pu=pu, nrow=nrow: e.transpose(
                                PSB(pu)[:, gi * 128: gi * 128 + nrow], Zs[zb][0:nrow, g, :], ident_b[0:nrow, 0:nrow]),
                                reads=[("Zs", zb, ss_) for ss_ in range(8)] + ["ident_b"], writes=[pk(pu)])
                        for (c0, c1) in col_sets:
                            cp("act" if g8 % 2 == 0 else "dve", U[:, g8 * 8:(g8 + 1) * 8, c0:c1],
                               PSB(pu).rearrange("p (g c) -> p g c", g=8)[:, :, 0:nrow], [pk(pu)], [("U", g8)])

            def p2_load(ti):
                kind, s, j = tiles[ti]
                rows = 32 if kind == "c" else 128
                src = ctx_r[s, :, :] if kind == "c" else x_r[s, j * 128:(j + 1) * 128, :]
                K.dma("sp", xt[ti % 4][0:rows, :], src, writes=[("xt", ti % 4)])
            p2_load(0); p2_load(1)
            for pr in range(len(tiles) // 2):
                if 2 * pr + 3 < len(tiles):
                    p2_load(2 * pr + 2); p2_load(2 * pr + 3)
                gens = [p2_tile(2 * pr), p2_tile(2 * pr + 1)]
                alive = [True, True]
                while any(alive):
                    for gi_, g_ in enumerate(gens):
                        if alive[gi_]:
                            try:
                                next(g_)
                            except StopIteration:
                                alive[gi_] = False
            K.barrier()
            A.top = p2mark

            chk(2)
            p3mark = A.top
            fnTt = [A.alloc([4, 128], BF16) for _ in range(2)]
            tab = A.alloc([2, 64, 128], BF16)
            fsb = [A.alloc([512], F32) for _ in range(2)]
            fmn = [A.alloc([512], F32) for _ in range(2)]
            fnb = [A.alloc([512], BF16) for _ in range(2)]
            fjunk = A.alloc([512], BF16)
            fstat = A.alloc([32, 4], F32)
            PQv = PQ.rearrange("p t (r n) -> p t r n", r=2)
            for r in range(2):
                for so_ in range(8):
                    K.dma("sp" if r == 0 else "act", tab[:, r, so_ * 8:(so_ + 1) * 8, :], c_tab[:, r, so_ * 8:(so_ + 1) * 8, :], writes=[("tab", r, so_)])
            def dft_a(ot):
                b2 = ot % 2
                jo, so = ot // 8, ot % 8
                pA, pB = 2 * b2, 2 * b2 + 1
                plan = []
                for it in range(32):
                    ji, si = it // 8, it % 8
                    m = (ji * so + jo * si) % 4
                    ss = so * 8 + si
                    if m == 0:
                        plan += [(pA, 0, 0, it, ss), (pB, 1, 1, it, ss)]
                    elif m == 1:
                        plan += [(pB, 1, 0, it, ss), (pB, 0, 1, it, ss)]
                    elif m == 2:
                        plan += [(pB, 0, 0, it, ss), (pA, 1, 1, it, ss)]
                    else:
                        plan += [(pA, 1, 0, it, ss), (pA, 0, 1, it, ss)]
                la = [p for p in plan if p[0] == pA]; lb = [p for p in plan if p[0] == pB]
                plan = []
                for q in range(max(len(la), len(lb))):
                    if q < len(la):
                        plan.append(la[q])
                    if q < len(lb):
                        plan.append(lb[q])
                cnt = {pA: len(la), pB: len(lb)}
                seen = {pA: 0, pB: 0}
                for (bk, tr, qr, it, ss) in plan:
                    st_ = (seen[bk] == 0)
                    seen[bk] += 1
                    sp_ = (seen[bk] == cnt[bk])
                    K.op("pe", lambda e, bk=bk, tr=tr, qr=qr, it=it, ss=ss, st_=st_, sp_=sp_: e.matmul(
                        PS(bk), lhsT=tab[:, tr, ss, :], rhs=PQv[:, it, qr, :], start=st_, stop=sp_),
                        reads=[("tab", tr, ss // 8), ("PQ", it)], writes=[pk(bk)])
                cp("act", fmn[b2], PS(pB), [pk(pB)], [("fmn", b2)])
                K.op("dve", lambda e, b2=b2, pA=pA: e.tensor_tensor(out=fsb[b2], in0=PS(pA), in1=fmn[b2], op=ALU.subtract),
                     reads=[pk(pA), ("fmn", b2)], writes=[("fsb", b2)])
                if ot == 0 and "d_f" in dbg_out:
                    K.dma("sp", dbg_out["d_f"], fsb[0], reads=[("fsb", 0)], writes=[("dbg", 5)])
                K.op("act", lambda e, b2=b2, ot=ot: e.activation(out=fjunk, in_=fsb[b2], func=AF.Square, accum_out=fstat[:, ot, 0:1]),
                     reads=[("fsb", b2)], writes=["fjunk", ("fstat", ot)])
                K.op("act", lambda e, ot=ot: e.activation(out=fstat[:, ot, 1:2], in_=fstat[:, ot, 0:1], func=AF.Sqrt, scale=1.0 / 512.0, bias=small[:, 0:1]),
                     reads=[("fstat", ot), "small"], writes=[("fstat", ot)])
                K.op("dve", lambda e, ot=ot: e.reciprocal(out=fstat[:, ot, 2:3], in_=fstat[:, ot, 1:2]), reads=[("fstat", ot)], writes=[("fstat", ot)])
                K.op("dve", lambda e, b2=b2, ot=ot: e.tensor_scalar(out=fnb[b2], in0=fsb[b2], scalar1=fstat[:, ot, 2:3], scalar2=None, op0=ALU.mult),
                     reads=[("fsb", b2), ("fstat", ot)], writes=[("fnb", b2)])

            def dft_b(ot):
                b2 = ot % 2
                pt = 4 + b2
                for fb in range(4):
                    K.op("pe", lambda e, fb=fb, b2=b2, pt=pt: e.transpose(PSB(pt)[:, fb * 128:(fb + 1) * 128], fnb[b2][:, fb * 128:(fb + 1) * 128], ident_b),
                         reads=[("fnb", b2), "ident_b"], writes=[pk(pt)])
                K.op("dve", lambda e, b2=b2, pt=pt: e.tensor_copy(out=fnTt[b2].rearrange("p f t -> p (f t)"), in_=PSB(pt)[:, 0:512]),
                     reads=[pk(pt)], writes=[("fnTt", b2)])
                K.dma("act", fT_scr[ot], fnTt[b2], reads=[("fnTt", b2)], writes=[("fTscr", ot)])
            for ot in range(32):
                dft_a(ot)
                if ot > 0:
                    dft_b(ot - 1)
            dft_b(31)
            K.barrier()
            A.top = p2start + 32 * 576

            chk(3)
            GamRe = A.alloc([32, 128], BF16)
            GamIm = A.alloc([32, 128], BF16)
            Mi = A.alloc([32, 128], BF16)
            ovl = A.top
            PhiT = A.alloc([32, 2, 128], BF16)
            RS = 32
            ring = A.alloc([RS, 4, 32], F32)
            rtmp = A.alloc([4, 32], F32); rC = A.alloc([2, 32], F32)
            L4 = A.alloc([4, 32], F32)
            p4mark = A.top
            Lrows = A.alloc([2, 128], F32)
            LR = A.alloc([32], F32); LI = A.alloc([32], F32); LS = A.alloc([32], F32)
            Bre = A.alloc([32, 16], F32); Bim = A.alloc([32, 16], F32)
            Crows = A.alloc([2, 4, 128], F32)
            Cre = A.alloc([32, 16], F32); Cim = A.alloc([32, 16], F32)
            T = {n: A.alloc([32, 18], F32) for n in ("AK", "ANG", "ANGC", "TF", "SINK", "COSK", "MAGP", "MAGN", "LKR", "LKI")}
            TI = A.alloc([32, 18], I32)
            S = {n: A.alloc([32], F32) for n in ("dt", "a", "th", "nr", "den", "rden", "qre", "qim", "t1", "t2")}
            W = {n: A.alloc([32, 8], F32) for n in ("ire", "iim", "wre", "wim", "w2re", "w2im", "t1", "t2")}
            PH1 = A.alloc([16, 8, 16], F32); PH2 = A.alloc([16, 8, 16], F32)
            PhRe = A.alloc([32, 128], BF16); PhIm = A.alloc([32, 128], BF16)
            MT1 = PH1.rearrange("p g s h -> p (g s h)")[:, 0:512].rearrange("p (g m) -> p g m", g=4)
            MT2 = PH2.rearrange("p g s h -> p (g s h)")[:, 0:512].rearrange("p (g m) -> p g m", g=4)

            with nc.allow_non_contiguous_dma(reason="s5 small param layouts"):
                K.dma("sp", Lrows[0:32, 0, :].rearrange("g (d p) -> g d p", d=2), lam_re.rearrange("d g p -> g d p"), writes=["Lrows"])
                K.dma("sp", Lrows[0:32, 1, :].rearrange("g (d p) -> g d p", d=2), lam_im.rearrange("d g p -> g d p"), writes=["Lrows"])
                for d in range(2):
                    K.dma("sp", LS[64 * d:64 * d + 64, :], log_step[d].partition_broadcast(64), writes=["LS"])
                    K.dma("sp", Bre[64 * d:64 * d + 64], b_re[d].rearrange("g p h -> p g h"), writes=["Bre"])
                    K.dma("act", Bim[64 * d:64 * d + 64], b_im[d].rearrange("g p h -> p g h"), writes=["Bim"])
                    for gb in range(4):
                        K.dma("sp", Crows[:, 0, gb, 64 * d:64 * d + 64], c_re[d, gb * 8:(gb + 1) * 8].rearrange("gl h p -> (gl h) p"), writes=["Crows"])
                        K.dma("act", Crows[:, 1, gb, 64 * d:64 * d + 64], c_im[d, gb * 8:(gb + 1) * 8].rearrange("gl h p -> (gl h) p"), writes=["Crows"])
            for r, dst, key in ((0, LR, "LR"), (1, LI, "LI")):
                K.op("pe", lambda e, r=r: e.transpose(PS(0, 32), Lrows[0:32, r, :], ident_f[0:32, 0:32]), reads=["Lrows", "ident_f"], writes=[pk(0)])
                cp("dve", dst, PS(0, 32), [pk(0)], [key])
            for r, dst, key in ((0, Cre, "Cre"), (1, Cim, "Cim")):
                for gb in range(4):
                    K.op("pe", lambda e, r=r, gb=gb: e.transpose(PS(1, 128, gb * 128), Crows[:, r, gb, :], ident_f), reads=["Crows", "ident_f"], writes=[pk(1)])
                cp("dve", dst.rearrange("p g h -> p (g h)"), PS(1), [pk(1)], [key])

            def dve(fn, reads, writes, eng="dve"):
                K.op(eng, fn, reads=reads, writes=writes)
            chk(3.1)
            K.op("act", lambda e: e.activation(out=S["dt"], in_=LS, func=AF.Exp), reads=["LS"], writes=["dt"])
            dve(lambda e: e.tensor_tensor(out=S["a"], in0=LR, in1=S["dt"], op=ALU.mult), ["LR", "dt"], ["a"])
            dve(lambda e: e.tensor_tensor(out=S["th"], in0=LI, in1=S["dt"], op=ALU.mult), ["LI", "dt"], ["th"])
            kidx_b = kidx.unsqueeze(1).to_broadcast([128, 32, 18])
            dve(lambda e: e.tensor_tensor(out=T["AK"], in0=S["a"].unsqueeze(2).to_broadcast([128, 32, 18]), in1=kidx_b, op=ALU.mult), ["a", "kidx"], ["AK"])
            dve(lambda e: e.tensor_tensor(out=T["ANG"], in0=S["th"].unsqueeze(2).to_broadcast([128, 32, 18]), in1=kidx_b, op=ALU.mult), ["th", "kidx"], ["ANG"])
            dve(lambda e: e.tensor_scalar(out=T["ANGC"], in0=T["ANG"], scalar1=math.pi / 2, scalar2=None, op0=ALU.add), ["ANG"], ["ANGC"])
            for src, dst in (("ANG", "SINK"), ("ANGC", "COSK")):
                dve(lambda e, src=src: e.tensor_scalar(out=TI, in0=T[src], scalar1=1.0 / TWO_PI, scalar2=None, op0=ALU.mult), [src], ["TI"])
                dve(lambda e: e.tensor_copy(out=T["TF"], in_=TI), ["TI"], ["TF"])
                dve(lambda e, src=src: e.scalar_tensor_tensor(out=T["TF"], in0=T["TF"], scalar=-TWO_PI, in1=T[src], op0=ALU.mult, op1=ALU.add), ["TF", src], ["TF"])
                dve(lambda e: e.tensor_scalar(out=T["TF"], in0=T["TF"], scalar1=-3.14159, scalar2=3.14159, op0=ALU.max, op1=ALU.min), ["TF"], ["TF"])
                K.op("act", lambda e, dst=dst: e.activation(out=T[dst], in_=T["TF"], func=AF.Sin), reads=["TF"], writes=[dst])
            K.op("act", lambda e: e.activation(out=T["MAGP"], in_=T["AK"], func=AF.Exp), reads=["AK"], writes=["MAGP"])
            K.op("act", lambda e: e.activation(out=T["MAGN"], in_=T["AK"], func=AF.Exp, scale=-1.0), reads=["AK"], writes=["MAGN"])
            dve(lambda e: e.tensor_tensor(out=T["LKR"], in0=T["MAGP"], in1=T["COSK"], op=ALU.mult), ["MAGP", "COSK"], ["LKR"])
            dve(lambda e: e.tensor_tensor(out=T["LKI"], in0=T["MAGP"], in1=T["SINK"], op=ALU.mult), ["MAGP", "SINK"], ["LKI"])
            l1r = T["LKR"][:, :, 16]; l1i = T["LKI"][:, :, 16]
            dve(lambda e: e.tensor_scalar(out=S["nr"], in0=l1r, scalar1=-1.0, scalar2=None, op0=ALU.add), ["LKR"], ["nr"])
            dve(lambda e: e.tensor_tensor(out=S["den"], in0=LR, in1=LR, op=ALU.mult), ["LR"], ["den"])
            dve(lambda e: e.tensor_tensor(out=S["t1"], in0=LI, in1=LI, op=ALU.mult), ["LI"], ["t1"])
            dve(lambda e: e.tensor_tensor(out=S["den"], in0=S["den"], in1=S["t1"], op=ALU.add), ["den", "t1"], ["den"])
            dve(lambda e: e.reciprocal(out=S["rden"], in_=S["den"]), ["den"], ["rden"])
            dve(lambda e: e.tensor_tensor(out=S["t1"], in0=S["nr"], in1=LR, op=ALU.mult), ["nr", "LR"], ["t1"])
            dve(lambda e: e.tensor_tensor(out=S["t2"], in0=l1i, in1=LI, op=ALU.mult), ["LKI", "LI"], ["t2"])
            dve(lambda e: e.tensor_tensor(out=S["t1"], in0=S["t1"], in1=S["t2"], op=ALU.add), ["t1", "t2"], ["t1"])
            dve(lambda e: e.tensor_tensor(out=S["qre"], in0=S["t1"], in1=S["rden"], op=ALU.mult), ["t1", "rden"], ["qre"])
            dve(lambda e: e.tensor_tensor(out=S["t1"], in0=l1i, in1=LR, op=ALU.mult), ["LKI", "LR"], ["t1"])
            dve(lambda e: e.tensor_tensor(out=S["t2"], in0=S["nr"], in1=LI, op=ALU.mult), ["nr", "LI"], ["t2"])
            dve(lambda e: e.tensor_tensor(out=S["t1"], in0=S["t1"], in1=S["t2"], op=ALU.subtract), ["t1", "t2"], ["t1"])
            dve(lambda e: e.tensor_tensor(out=S["qim"], in0=S["t1"], in1=S["rden"], op=ALU.mult), ["t1", "rden"], ["qim"])
            dve(lambda e: e.tensor_tensor(out=W["ire"], in0=T["MAGN"][:, :, 0:8], in1=T["COSK"][:, :, 0:8], op=ALU.mult), ["MAGN", "COSK"], ["ire"])
            dve(lambda e: e.scalar_tensor_tensor(out=W["iim"], in0=T["MAGN"][:, :, 0:8], scalar=-1.0, in1=T["SINK"][:, :, 0:8], op0=ALU.mult, op1=ALU.mult), ["MAGN", "SINK"], ["iim"])
            qreb = S["qre"].unsqueeze(2).to_broadcast([128, 32, 8]); qimb = S["qim"].unsqueeze(2).to_broadcast([128, 32, 8])

            def cmul(ore, oim, ar, ai, br, bi, keys_a, keys_b, ko):
                dve(lambda e: e.tensor_tensor(out=W["t1"], in0=ar, in1=br, op=ALU.mult), keys_a + keys_b, ["Wt1"])
                dve(lambda e: e.tensor_tensor(out=W["t2"], in0=ai, in1=bi, op=ALU.mult), keys_a + keys_b, ["Wt2"])
                dve(lambda e: e.tensor_tensor(out=ore, in0=W["t1"], in1=W["t2"], op=ALU.subtract), ["Wt1", "Wt2"], [ko + "re"])
                dve(lambda e: e.tensor_tensor(out=W["t1"], in0=ar, in1=bi, op=ALU.mult), keys_a + keys_b + [ko + "re"], ["Wt1"])
                dve(lambda e: e.tensor_tensor(out=W["t2"], in0=ai, in1=br, op=ALU.mult), keys_a + keys_b + [ko + "re"], ["Wt2"])
                dve(lambda e: e.tensor_tensor(out=oim, in0=W["t1"], in1=W["t2"], op=ALU.add), ["Wt1", "Wt2"], [ko + "im"])
            cmul(W["wre"], W["wim"], W["ire"], W["iim"], qreb, qimb, ["ire", "iim"], ["qre", "qim"], "w")
            cmul(W["w2re"], W["w2im"], T["LKR"][:, :, 8:16], T["LKI"][:, :, 8:16], qreb, qimb, ["LKR", "LKI"], ["qre", "qim"], "w2")

            def outer(dst_re, dst_im, wr, wi, wkeys, Xre, Xim, xkeys, okey, neg_im=False):
                for gh in range(2):
                    gs = slice(gh * 16, gh * 16 + 16)
                    wrb = wr[:, gs, :].unsqueeze(3).to_broadcast([128, 16, 8, 16]); wib = wi[:, gs, :].unsqueeze(3).to_broadcast([128, 16, 8, 16])
                    xrb = Xre[:, gs, :].unsqueeze(2).to_broadcast([128, 16, 8, 16]); xib = Xim[:, gs, :].unsqueeze(2).to_broadcast([128, 16, 8, 16])
                    dr = dst_re[:, gs, :].rearrange("p g (s h) -> p g s h", s=8); di = dst_im[:, gs, :].rearrange("p g (s h) -> p g s h", s=8)
                    dve(lambda e, wrb=wrb, xrb=xrb: e.tensor_tensor(out=PH1, in0=wrb, in1=xrb, op=ALU.mult), wkeys + xkeys, ["PH1"])
                    dve(lambda e, wib=wib, xib=xib: e.tensor_tensor(out=PH2, in0=wib, in1=xib, op=ALU.mult), wkeys + xkeys, ["PH2"])
                    dve(lambda e, dr=dr: e.tensor_tensor(out=dr, in0=PH1, in1=PH2, op=ALU.subtract), ["PH1", "PH2"], [okey + "re"])
                    dve(lambda e, wrb=wrb, xib=xib: e.tensor_tensor(out=PH1, in0=wrb, in1=xib, op=ALU.mult), wkeys + xkeys + [okey + "re"], ["PH1"])
                    dve(lambda e, wib=wib, xrb=xrb: e.tensor_tensor(out=PH2, in0=wib, in1=xrb, op=ALU.mult), wkeys + xkeys + [okey + "re"], ["PH2"])
                    if neg_im:
                        dve(lambda e, di=di: e.scalar_tensor_tensor(out=di, in0=PH1, scalar=-1.0, in1=PH2, op0=ALU.mult, op1=ALU.subtract), ["PH1", "PH2"], [okey + "im"])
                    else:
                        dve(lambda e, di=di: e.tensor_tensor(out=di, in0=PH1, in1=PH2, op=ALU.add), ["PH1", "PH2"], [okey + "im"])
            chk(3.2)
            outer(PhRe, PhIm, W["w2re"], W["w2im"], ["w2re", "w2im"], Bre, Bim, ["Bre", "Bim"], "Ph")
            for g in range(32):
                pb = 2 + (g // 4) % 2
                for ri, src, sk in ((0, PhRe, "Phre"), (1, PhIm, "Phim")):
                    K.op("pe", lambda e, g=g, ri=ri, src=src, pb=pb: e.transpose(
                        PSB(pb)[:, ((g % 4) * 2 + ri) * 128:((g % 4) * 2 + ri + 1) * 128], src[:, g, :], ident_b),
                        reads=[sk, "ident_b"], writes=[pk(pb)])
                if g % 4 == 3:
                    g0 = g - 3
                    cp("act", PhiT[:, g0:g0 + 4].rearrange("p g r m -> p (g r m)"), PSB(pb), [pk(pb)], ["PhiT"])
            chk(3.3)
            outer(PhRe, PhIm, W["wre"], W["wim"], ["wre", "wim"], Bre, Bim, ["Bre", "Bim"], "Ph")
            outer(GamRe, GamIm, T["LKR"][:, :, 0:8], T["LKI"][:, :, 0:8], ["LKR", "LKI"], Cre, Cim, ["Cre", "Cim"], "Gam", neg_im=True)
            l8r = T["LKR"][:, :, 17]; l8i = T["LKI"][:, :, 17]
            dve(lambda e: e.tensor_copy(out=L4[:, 0:2, :], in_=l8r.unsqueeze(1).to_broadcast([128, 2, 32])), ["LKR"], ["L4"])
            dve(lambda e: e.tensor_copy(out=L4[:, 2, :], in_=l8i), ["LKI"], ["L4"])
            dve(lambda e: e.tensor_scalar(out=L4[:, 3, :], in0=l8i, scalar1=-1.0, scalar2=None, op0=ALU.mult), ["LKI"], ["L4"])
            mfb = masks[:, 0:128].unsqueeze(1).to_broadcast([128, 4, 128]); mrb = masks[:, 128:256].unsqueeze(1).to_broadcast([128, 4, 128])
            for g4 in range(8):
                pf_, pr_ = 4 + 2 * (g4 % 2), 5 + 2 * (g4 % 2)
                for gi in range(4):
                    g = g4 * 4 + gi
                    for d, pb in ((0, pf_), (1, pr_)):
                        K.op("pe", lambda e, g=g, gi=gi, d=d, pb=pb: e.matmul(
                            PS(pb, 128, gi * 128), lhsT=PhRe[64 * d:64 * d + 64, g, :], rhs=GamRe[64 * d:64 * d + 64, g, :], start=True, stop=False),
                            reads=["Phre", "Gamre"], writes=[pk(pb)])
                        K.op("pe", lambda e, g=g, gi=gi, d=d, pb=pb: e.matmul(
                            PS(pb, 128, gi * 128), lhsT=PhIm[64 * d:64 * d + 64, g, :], rhs=GamIm[64 * d:64 * d + 64, g, :], start=False, stop=True),
                            reads=["Phim", "Gamim"], writes=[pk(pb)])
                dve(lambda e, pf_=pf_: e.tensor_tensor(out=MT1, in0=PS(pf_).rearrange("p (g m) -> p g m", g=4), in1=mfb, op=ALU.mult), [pk(pf_), "masks"], ["PH1"])
                dve(lambda e, pr_=pr_: e.tensor_tensor(out=MT2, in0=PS(pr_).rearrange("p (g m) -> p g m", g=4), in1=mrb, op=ALU.mult), [pk(pr_), "masks"], ["PH2"])
                dve(lambda e: e.tensor_tensor(out=MT1, in0=MT1, in1=MT2, op=ALU.add), ["PH1", "PH2"], ["PH1"])
                for gi in range(4):
                    g = g4 * 4 + gi
                    dve(lambda e, g=g, gi=gi: e.scalar_tensor_tensor(out=Mi[:, g, :], in0=ident_f, scalar=Dcol[:, g:g + 1], in1=MT1[:, gi, :], op0=ALU.mult, op1=ALU.add),
                        ["PH1", "ident_f", "Dcol"], [("Mi", g)])
            if "d_Mi" in dbg_out:
                dM = A.alloc([128], F32)
                dve(lambda e: e.tensor_copy(out=dM, in_=Mi[:, 0, :]), [("Mi", 0)], ["dM"])
                K.dma("sp", dbg_out["d_Mi"], dM, reads=["dM"], writes=[("dbg", 20)])
            K.barrier()
            chk(3.4)
            A.top = p4mark
            XV = A.alloc([2, 32, NE], BF16)
            A.alloc([512], BF16)
            wob_lo = A.top
            w_out_bf = A.alloc([8, 1024], BF16)
            w_glu_bf = A.alloc([4, 512], BF16)
            w_out_r = w_out.rearrange("(kc p) n -> p kc n", p=128)
            for kc in range(8):
                K.dma("pool", w_out_bf[:, kc, :], w_out_r[:, kc, :], writes=["w_out_bf"])
            K.dma("pool", w_glu_bf, w_glu.rearrange("(kc p) n -> p kc n", p=128), writes=["w_glu_bf"])
            for kc in range(8):
                K.op("dve", lambda e, kc=kc: e.tensor_scalar(out=w_out_bf[:, kc, :], in0=w_out_bf[:, kc, :], scalar1=cols[:, 80 + kc:81 + kc], scalar2=None, op0=ALU.mult),
                     reads=["w_out_bf", "cols"], writes=["w_out_bf"])
            for g in range(32):
                for ri in range(2):
                    idx = g * 2 + ri
                    pa = idx % 2
                    pbk = 2 + (idx // 16) % 2
                    for d in range(2):
                        rA = U[:, g, 0:512] if d == 0 else U[:, g, 575:63:-1]
                        rB = U[:, g, 512:543] if d == 0 else U[:, g, 63:32:-1]
                        K.op("pe", lambda e, g=g, ri=ri, d=d, pa=pa, rA=rA: e.matmul(
                            PS(pa)[64 * d:64 * d + 64, :], lhsT=PhiT[:, g, ri, 64 * d:64 * d + 64], rhs=rA, start=True, stop=True),
                            reads=["PhiT", ("U", g // 8)], writes=[pk(pa)])
                        K.op("pe", lambda e, g=g, ri=ri, d=d, pbk=pbk, rB=rB, idx=idx: e.matmul(
                            PS(pbk, 31, (idx % 16) * 32)[64 * d:64 * d + 64, :], lhsT=PhiT[:, g, ri, 64 * d:64 * d + 64], rhs=rB, start=True, stop=True),
                            reads=["PhiT", ("U", g // 8)], writes=[pk(pbk)])
                    cp("act" if idx % 2 == 0 else "dve", XV[:, ri, g, 0:512], PS(pa), [pk(pa)], ["XVin"])
                    if idx % 16 == 15:
                        g0 = (idx - 15) // 2
                        cp("dve", XV[:, :, g0:g0 + 8, 512:543].rearrange("p r g c -> p g r c"),
                           PS(pbk).rearrange("p (g r c) -> p g r c", g=8, r=2)[:, :, :, 0:31], [pk(pbk)], ["XVin"])
            if "d_V" in dbg_out:
                dV = A.alloc([NE], F32)
                dve(lambda e: e.tensor_copy(out=dV, in_=XV[:, 0, 0, :]), ["XVin"], ["dV"])
                K.dma("sp", dbg_out["d_V"], dV, reads=["dV"], writes=[("dbg", 21)])
            chk(3.5)
            XVOUT = []
            dve(lambda e: e.memset(ring[:, 0, :, :], 0.0), [], [("ring", 0)])
            NSTEP = 543
            CV = 16
            rk = lambda slot: ("ring", (slot % RS) // CV)
            for e_ in range(NSTEP):
                cur = ring[:, e_ % RS, :, :]
                nxt = ring[:, (e_ + 1) % RS, :, :].rearrange("p (c r) g -> p c r g", c=2)
                dve(lambda e, cur=cur: e.tensor_tensor(out=rtmp, in0=cur, in1=L4, op=ALU.mult), [rk(e_), "L4"], ["rtmp"])
                dve(lambda e: e.tensor_tensor(out=rC, in0=rtmp[:, 0:2, :], in1=rtmp[:, 3:1:-1, :], op=ALU.add), ["rtmp"], ["rC"])
                dve(lambda e, nxt=nxt, e_=e_: e.tensor_tensor(out=nxt, in0=rC.unsqueeze(1).to_broadcast([128, 2, 2, 32]),
                                                              in1=XV[:, :, :, e_].unsqueeze(1).to_broadcast([128, 2, 2, 32]), op=ALU.add), ["rC", "XVin"], [rk(e_ + 1)])
                if (e_ + 1) % CV == 0 or e_ == NSTEP - 1:
                    n = CV if (e_ + 1) % CV == 0 else (NSTEP % CV)
                    e0 = e_ + 1 - n
                    k = 0
                    while k < n:
                        rs = (e0 + 1 + k) % RS
                        run = min(n - k, RS - rs)
                        cp("act", XV[:, :, :, e0 + k:e0 + k + run], ring[:, rs:rs + run, 0:2, :].rearrange("p c r g -> p r g c"),
                           sorted(set([rk(rs + q) for q in range(run)])), [("XVout", e0 // CV, k)])
                        XVOUT.append(("XVout", e0 // CV, k))
                        k += run
            if "d_X" in dbg_out:
                dX = A.alloc([NE], F32)
                dve(lambda e: e.tensor_copy(out=dX, in_=XV[:, 0, 0, :]), XVOUT + ["XVin"], ["dX"])
                K.dma("sp", dbg_out["d_X"], dX, reads=["dX"], writes=[("dbg", 22)])
            K.barrier()
            chk(3.6)
            A2 = Arena(arena_t); A2.top = ovl
            Ylj = A2.alloc([32, 128], F32)
            Ytj = A2.alloc([8, 512], F32)
            assert A2.top <= p4mark
            y5_r = y5_scr.rearrange("(c s) f -> c s f", s=8)
            for j in range(4):
                for g in range(32):
                    pA = (g % 2) * 3
                    K.op("pe", lambda e, g=g, pA=pA, j=j: e.matmul(PS(pA, 128), lhsT=Mi[:, g, :], rhs=U[:, g, 32 + 128 * j:32 + 128 * (j + 1)], start=True, stop=True),
                         reads=[("Mi", g), ("U", g // 8)], writes=[pk(pA)])
                    for d in range(2):
                        pD = pA + 1 + d
                        for ri, Gm in ((0, GamRe), (1, GamIm)):
                            if d == 0:
                                rhs = XV[0:64, ri, g, 31 + 128 * j:31 + 128 * (j + 1)]
                            else:
                                hi = 542 - 128 * j
                                lo = hi - 128
                                rhs = XV[64:128, ri, g, hi:lo:-1]
                            K.op("pe", lambda e, g=g, pD=pD, d=d, Gm=Gm, rhs=rhs, ri=ri: e.matmul(
                                PS(pD, 128), lhsT=Gm[64 * d:64 * d + 64, g, :], rhs=rhs, start=(ri == 0), stop=(ri == 1)),
                                reads=["Gamre", "Gamim", "XVin"] + XVOUT, writes=[pk(pD)])
                    cp("act", Ylj[:, g, :], PS(pA, 128), [pk(pA)], [("Ylj", g)])
                    K.op("dve", lambda e, g=g, pA=pA: e.tensor_tensor(out=Ylj[:, g, :], in0=Ylj[:, g, :], in1=PS(pA + 1, 128), op=ALU.add),
                         reads=[("Ylj", g), pk(pA + 1)], writes=[("Ylj", g)])
                    K.op("dve", lambda e, g=g, pA=pA: e.tensor_tensor(out=Ylj[:, g, :], in0=Ylj[:, g, :], in1=PS(pA + 2, 128), op=ALU.add),
                         reads=[("Ylj", g), pk(pA + 2)], writes=[("Ylj", g)])
                if j == 0 and "d_Y" in dbg_out:
                    K.dma("sp", dbg_out["d_Y"], Ylj[:, 0, :], reads=[("Ylj", 0)], writes=[("dbg", 7)])
                for g4 in range(8):
                    pb = 6 + g4 % 2
                    for gi in range(4):
                        g = g4 * 4 + gi
                        K.op("pe", lambda e, g=g, gi=gi, pb=pb: e.transpose(PS(pb, 128, gi * 128), Ylj[:, g, :], ident_f),
                             reads=[("Ylj", g), "ident_f"], writes=[pk(pb)])
                    cp("act" if g4 % 2 == 0 else "dve", Ytj[:, :, g4 * 64:(g4 + 1) * 64].rearrange("p s (g h) -> p g s h", g=4),
                       PS(pb).rearrange("p (g s h) -> p g s h", g=4, s=8), [pk(pb)], ["Ytj"])
                K.dma("sp", y5_r[128 * j:128 * (j + 1), :, :], Ytj, reads=["Ytj"], writes=[("y5", j)])
            K.barrier()

            chk(4)
            A.top = p2start
            G = A.alloc([32, 32], F32)
            pjunk = A.alloc([1024], BF16)
            p5keep = A.top
            pjunk2 = A.alloc([1024], BF16)
            lgall = A.alloc([32, 36], F32)
            p5tmp = A.top
            D2 = lambda shape, dt: [A.alloc(shape, dt) for _ in range(4)]
            D4 = lambda shape, dt: [A.alloc(shape, dt) for _ in range(8)]
            Yt = D4([512], F32); fnTt5 = D4([4, 128], BF16)
            ygf = D2([512], F32); ygb = D2([512], BF16); ygT = D2([4, 128], BF16)
            sg = D2([512], F32); y2 = D2([512], F32); y2n = D2([512], BF16); y2nT = D2([4, 128], BF16)
            xr = D4([1024], F32); x1 = D2([1024], F32)
            xs2 = D2([1024], BF16); tT = D2([8, 128], BF16); tTf = D2([8, 128], F32)
            pstat = A.alloc([32, 8], F32)
            y_r = y.rearrange("(c s) d -> s c d", s=8)
            x1_r = x1_scr.rearrange("(c s) d -> s c d", s=8)
            y5_sr = y5_scr.rearrange("(c s) f -> s c f", s=8)
            pj = [pjunk, pjunk2, A.alloc([1024], BF16), A.alloc([1024], BF16)]
            assert A.top <= wob_lo, (A.top, wob_lo)

            def p5_loads(t_):
                j_, s_, b_ = t_ // 8, t_ % 8, t_ % 8
                K.dma("sp", Yt[b_], y5_sr[s_, j_ * 128:(j_ + 1) * 128, :], reads=[("y5", j_)], writes=[("Yt", b_)])
                K.dma("sp", fnTt5[b_], fT_scr[t_], reads=[("fTscr", t_)], writes=[("fnTt5", b_)])
                K.dma("sp", xr[b_], x_r[s_, j_ * 128:(j_ + 1) * 128, :], writes=[("xr", b_)])
            def p5_tile(tl):
                j, s = tl // 8, tl % 8
                tl = j * 8 + s
                b2 = tl % 4
                bT, bG, bO = 2 * b2, 2 * b2 + 1, 2 * b2 + 1
                b4 = tl % 8
                b3 = tl % 4
                K.op("act", lambda e, b2=b2, b4=b4: e.activation(out=ygf[b2], in_=Yt[b4], func=AF.Gelu_apprx_tanh), reads=[("Yt", b4)], writes=[("ygf", b2)])
                K.op("pool", lambda e, b2=b2: e.tensor_copy(out=ygb[b2], in_=ygf[b2]), reads=[("ygf", b2)], writes=[("ygb", b2)])
                yield
                for fb in range(4):
                    K.op("pe", lambda e, fb=fb, b2=b2, bT=bT: e.transpose(PSB(bT)[:, fb * 128:(fb + 1) * 128], ygb[b2][:, fb * 128:(fb + 1) * 128], ident_b),
                         reads=[("ygb", b2), "ident_b"], writes=[pk(bT)])
                cp("dve", ygT[b2].rearrange("p f t -> p (f t)"), PSB(bT)[:, 0:512], [pk(bT)], [("ygT", b2)])
                yield
                for fb in range(4):
                    K.op("pe", lambda e, fb=fb, b2=b2, bG=bG: e.matmul(PS(bG), lhsT=ygT[b2][:, fb, :], rhs=w_glu_bf[:, fb, :], start=(fb == 0), stop=(fb == 3)),
                         reads=[("ygT", b2), "w_glu_bf"], writes=[pk(bG)])
                K.op("act", lambda e, b2=b2, bG=bG: e.activation(out=sg[b2], in_=PS(bG), func=AF.Sigmoid), reads=[pk(bG)], writes=[("sg", b2)])
                K.op("dve", lambda e, b2=b2: e.tensor_tensor(out=y2[b2], in0=ygf[b2], in1=sg[b2], op=ALU.mult), reads=[("ygf", b2), ("sg", b2)], writes=[("y2", b2)])
                yield
                rms_prep(100 + tl, ("y2", b2), y2[b2], 128, 512.0, pstat[:, tl, 0:4], pj[b2][:, 0:512], y2n[b2], ("y2n", b2), junk_key=("pj", b2))
                yield
                for fb in range(4):
                    K.op("pe", lambda e, fb=fb, b2=b2, bT=bT: e.transpose(PSB(bT)[:, 512 + fb * 128:512 + (fb + 1) * 128], y2n[b2][:, fb * 128:(fb + 1) * 128], ident_b),
                         reads=[("y2n", b2), "ident_b"], writes=[pk(bT)])
                cp("dve", y2nT[b2].rearrange("p f t -> p (f t)"), PSB(bT)[:, 512:1024], [pk(bT)], [("y2nT", b2)])
                yield
                for hf in range(2):
                    pb = bO
                    for kb in range(8):
                        lhsT = y2nT[b2][:, kb, :] if kb < 4 else fnTt5[b4][:, kb - 4, :]
                        K.op("pe", lambda e, hf=hf, kb=kb, pb=pb, lhsT=lhsT: e.matmul(
                            PS(pb), lhsT=lhsT, rhs=w_out_bf[:, kb, hf * 512:(hf + 1) * 512], start=(kb == 0), stop=(kb == 7)),
                            reads=[("y2nT", b2), ("fnTt5", b4), "w_out_bf"], writes=[pk(pb)])
                    K.op("dve", lambda e, hf=hf, pb=pb, b2=b2: e.tensor_tensor(out=x1[b2][:, hf * 512:(hf + 1) * 512], in0=PS(pb), in1=g1_bc[:, hf * 512:(hf + 1) * 512], op=ALU.mult),
                         reads=[pk(pb), "g1_bc"], writes=[("x1", b2)])
                yield
                K.op("pool", lambda e, b2=b2, b4=b4: e.tensor_tensor(out=x1[b2], in0=x1[b2], in1=xr[b4], op=ALU.add), reads=[("x1", b2), ("xr", b4)], writes=[("x1", b2)])
                K.dma("sp", x1_r[s, j * 128:(j + 1) * 128, :], x1[b2], reads=[("x1", b2)], writes=[("x1scr", tl)])
                if tl == 0 and "d_x1" in dbg_out:
                    K.dma("sp", dbg_out["d_x1"], x1[0], reads=[("x1", 0)], writes=[("dbg", 8)])
                yield
                rms_prep(200 + tl, ("x1", b2), x1[b2], 128, 1024.0, pstat[:, tl, 4:8], pj[b2], xs2[b2], ("xs2", b2), junk_key=("pj", b2))
                yield
                for kc in range(8):
                    K.op("pe", lambda e, kc=kc, b2=b2, bT=bT: e.transpose(PSB(bT)[:, kc * 128:(kc + 1) * 128], xs2[b2][:, kc * 128:(kc + 1) * 128], ident_b),
                         reads=[("xs2", b2), "ident_b"], writes=[pk(bT)])
                psv = PSB(bT).rearrange("p (k t) -> p k t", k=8)
                K.op("dve", lambda e, psv=psv, b3=b3: e.tensor_tensor(out=tTf[b3], in0=psv, in1=vecs[:, 4, :].unsqueeze(2).to_broadcast([128, 8, 128]), op=ALU.mult),
                     reads=[pk(bT), "vecs"], writes=[("tTf", b3)])
                yield
                K.op("pool", lambda e, b3=b3: e.tensor_tensor(out=tTf[b3], in0=tTf[b3], in1=vecs[:, 5, :].unsqueeze(2).to_broadcast([128, 8, 128]), op=ALU.add),
                     reads=[("tTf", b3), "vecs"], writes=[("tTf", b3)])
                cp("act", tT[b2], tTf[b3], [("tTf", b3)], [("tT", b2)])
                K.dma("sp", tT_scr[:, :, tl * 128:(tl + 1) * 128].rearrange("k p t -> p k t"), tT[b2], reads=[("tT", b2)], writes=[("tTscr", tl)])
                yield
                for kc in range(8):
                    K.op("pe", lambda e, kc=kc, b3=b3, bG=bG: e.matmul(PS(bG, 36), lhsT=tTf[b3][:, kc, :], rhs=wrt[:, kc, :], start=(kc == 0), stop=(kc == 7)),
                         reads=[("tTf", b3), "wrt"], writes=[pk(bG)])
                K.op("dve", lambda e, tl=tl, bG=bG: e.tensor_tensor(out=lgall[:, tl, :], in0=PS(bG, 36), in1=brt_bc, op=ALU.add), reads=[pk(bG), "brt_bc"], writes=["lgall"])

            for t_ in range(4):
                p5_loads(t_)
            for pr in range(8):
                if pr + 1 < 8:
                    for t_ in range(4):
                        p5_loads(4 * pr + 4 + t_)
                gens = [p5_tile(4 * pr + q) for q in range(4)]
                alive = [True] * 4
                while any(alive):
                    for gi_, g_ in enumerate(gens):
                        if alive[gi_]:
                            try:
                                next(g_)
                            except StopIteration:
                                alive[gi_] = False
            K.barrier()
            A.top = p5tmp
            NTL = 32
            ra = lambda n: A.alloc([NTL, n], F32)
            gmax = ra(1); sel = ra(4); exg = ra(4); gsum = ra(1); gw = ra(1); t8 = ra(32); es_ = ra(8); m1 = ra(1); mk1 = ra(8)
            es2 = ra(8); m2 = ra(1); mk2 = ra(8); dd = ra(1); exd = ra(1); den = ra(1); w1 = ra(1); w2 = ra(1); comb = ra(8); cb2 = ra(8)
            gl3 = lgall[:, :, 0:4]; el4 = lgall[:, :, 4:36].rearrange("p t (g e) -> p t g e", g=4)

            def R(fn, eng="dve"):
                K.op(eng, fn, reads=["rt", "lgall"], writes=["rt"])
            bc4 = lambda a: a[:, :, 0:1].to_broadcast([128, NTL, 4])
            bc8 = lambda a: a[:, :, 0:1].to_broadcast([128, NTL, 8])
            R(lambda e: e.tensor_reduce(out=gmax[:, :, 0], in_=gl3, axis=AX.X, op=ALU.max))
            R(lambda e: e.tensor_tensor(out=sel, in0=gl3, in1=bc4(gmax), op=ALU.is_equal))
            R(lambda e: e.tensor_tensor(out=exg, in0=gl3, in1=bc4(gmax), op=ALU.subtract))
            R(lambda e: e.activation(out=exg, in_=exg, func=AF.Exp), eng="act")
            R(lambda e: e.tensor_reduce(out=gsum[:, :, 0], in_=exg, axis=AX.X, op=ALU.add))
            R(lambda e: e.reciprocal(out=gw, in_=gsum))
            R(lambda e: e.tensor_tensor(out=t8.rearrange("p t (g e) -> p t g e", g=4), in0=el4, in1=sel.unsqueeze(3).to_broadcast([128, NTL, 4, 8]), op=ALU.mult))
            R(lambda e: e.tensor_reduce(out=es_, in_=t8.rearrange("p t (g e) -> p t e g", g=4), axis=AX.X, op=ALU.add))
            R(lambda e: e.tensor_reduce(out=m1[:, :, 0], in_=es_, axis=AX.X, op=ALU.max))
            R(lambda e: e.tensor_tensor(out=mk1, in0=es_, in1=bc8(m1), op=ALU.is_equal))
            R(lambda e: e.scalar_tensor_tensor(out=es2, in0=mk1, scalar=-1e30, in1=es_, op0=ALU.mult, op1=ALU.add))
            R(lambda e: e.tensor_reduce(out=m2[:, :, 0], in_=es2, axis=AX.X, op=ALU.max))
            R(lambda e: e.tensor_tensor(out=mk2, in0=es2, in1=bc8(m2), op=ALU.is_equal))
            R(lambda e: e.tensor_tensor(out=dd, in0=m2, in1=m1, op=ALU.subtract))
            R(lambda e: e.activation(out=exd, in_=dd, func=AF.Exp), eng="act")
            R(lambda e: e.tensor_scalar(out=den, in0=exd, scalar1=1.0, scalar2=None, op0=ALU.add))
            R(lambda e: e.reciprocal(out=w1, in_=den))
            R(lambda e: e.tensor_tensor(out=w1, in0=w1, in1=gw, op=ALU.mult))
            R(lambda e: e.tensor_tensor(out=w2, in0=w1, in1=exd, op=ALU.mult))
            R(lambda e: e.tensor_tensor(out=comb, in0=mk1, in1=bc8(w1), op=ALU.mult))
            R(lambda e: e.tensor_tensor(out=cb2, in0=mk2, in1=bc8(w2), op=ALU.mult))
            R(lambda e: e.tensor_tensor(out=comb, in0=comb, in1=cb2, op=ALU.add))
            K.op("dve", lambda e: e.tensor_tensor(out=G.rearrange("p t (g e) -> p t g e", g=4),
                                                  in0=sel.unsqueeze(3).to_broadcast([128, NTL, 4, 8]),
                                                  in1=comb.unsqueeze(2).to_broadcast([128, NTL, 4, 8]), op=ALU.mult),
                 reads=["rt"], writes=[("G", tl_) for tl_ in range(32)])
            if "d_G" in dbg_out:
                K.dma("sp", dbg_out["d_G"], G[:, 0, :], reads=[("G", 0)], writes=[("dbg", 9)])
            K.barrier()

            chk(5)
            A.top = p5keep
            tTh2 = [A.alloc([8, 2048], BF16) for _ in range(2)]
            acc = A.alloc([16, 1024], F32)
            wg = [A.alloc([8, 256], BF16) for _ in range(2)]
            wu = [A.alloc([8, 256], BF16) for _ in range(2)]
            wd = [A.alloc([2, 1024], BF16) for _ in range(2)]
            sl = [A.alloc([512], F32) for _ in range(2)]
            aT = [A.alloc([2, 512], BF16) for _ in range(2)]
            ob = [A.alloc([1024], F32) for _ in range(2)]
            ostat = A.alloc([32, 4], F32)
            ojunk = pjunk
            g2b = A.alloc([1024], BF16)
            K.op("dve", lambda e: e.tensor_copy(out=g2b, in_=g2_bc), reads=["g2_bc"], writes=["g2b"])
            def acc_load(hh, tt):
                tl = hh * 16 + tt
                j, s = tl // 8, tl % 8
                K.dma("act", acc[:, tt, :], x1_r[s, j * 128:(j + 1) * 128, :], reads=[("x1scr", tl)], writes=[("acc", tt)])

            for hh in range(2):
                for tg_ in range(4):
                    c0_ = hh * 2048 + tg_ * 512
                    K.dma("sp", tTh2[hh][:, :, tg_ * 512:(tg_ + 1) * 512], tT_scr[:, :, c0_:c0_ + 512].rearrange("k p t -> p k t"),
                          reads=[("tTscr", tl) for tl in range(hh * 16 + tg_ * 4, hh * 16 + tg_ * 4 + 4)], writes=[("tTh", hh, tg_)])
            for tt in range(16):
                acc_load(0, tt)

            def wload(ex_, wi):
                b2 = wi % 2
                K.dma("pool", wg[b2], w_gate[ex_].rearrange("(kc p) f -> p kc f", p=128), writes=[("wg", b2)])
                K.dma("pool", wu[b2], w_up[ex_].rearrange("(kc p) f -> p kc f", p=128), writes=[("wu", b2)])
                K.dma("pool", wd[b2], w_down[ex_].rearrange("(fb p) n -> p fb n", p=128), writes=[("wd", b2)])
                K.op("pool", lambda e, b2=b2: e.tensor_tensor(out=wd[b2], in0=wd[b2], in1=g2b.unsqueeze(1).to_broadcast([128, 2, 1024]), op=ALU.mult),
                     reads=[("wd", b2), "g2b"], writes=[("wd", b2)])

            def hgu(hh, ex_, tg, it_):
                b2 = (hh * 32 + ex_) % 2
                ab = it_ % 2
                tTh = tTh2[hh]
                for fb in range(2):
                    pg, pu_ = 0 + fb, 2 + fb
                    for kc in range(8):
                        K.op("pe", lambda e, kc=kc, fb=fb, b2=b2, tg=tg, pg=pg, tTh=tTh: e.matmul(
                            PS(pg), lhsT=wg[b2][:, kc, fb * 128:(fb + 1) * 128], rhs=tTh[:, kc, tg * 512:(tg + 1) * 512], start=(kc == 0), stop=(kc == 7)),
                            reads=[("wg", b2), ("tTh", hh, tg)], writes=[pk(pg)])
                    for kc in range(8):
                        K.op("pe", lambda e, kc=kc, fb=fb, b2=b2, tg=tg, pu_=pu_, tTh=tTh: e.matmul(
                            PS(pu_), lhsT=wu[b2][:, kc, fb * 128:(fb + 1) * 128], rhs=tTh[:, kc, tg * 512:(tg + 1) * 512], start=(kc == 0), stop=(kc == 7)),
                            reads=[("wu", b2), ("tTh", hh, tg)], writes=[pk(pu_)])
                    K.op("act", lambda e, fb=fb, pg=pg: e.activation(out=sl[fb], in_=PS(pg), func=AF.Silu), reads=[pk(pg)], writes=[("sl", fb)])
                    K.op("dve", lambda e, fb=fb, pu_=pu_, ab=ab: e.tensor_tensor(out=aT[ab][:, fb, :], in0=sl[fb], in1=PS(pu_), op=ALU.mult),
                         reads=[("sl", fb), pk(pu_)], writes=[("aT", ab, fb)])

            def down(hh, ex_, tg, it_):
                b2 = (hh * 32 + ex_) % 2
                ab = it_ % 2
                for t4 in range(4):
                    tt = tg * 4 + t4
                    tl = hh * 16 + tt
                    for hf in range(2):
                        po = 4 + (t4 * 2 + hf) % 4
                        for fb in range(2):
                            K.op("pe", lambda e, fb=fb, hf=hf, po=po, ab=ab, t4=t4, b2=b2: e.matmul(
                                PS(po), lhsT=aT[ab][:, fb, t4 * 128:(t4 + 1) * 128], rhs=wd[b2][:, fb, hf * 512:(hf + 1) * 512], start=(fb == 0), stop=(fb == 1)),
                                reads=[("aT", ab, 0), ("aT", ab, 1), ("wd", b2)], writes=[pk(po)])
                        K.op("dve", lambda e, po=po, tt=tt, hf=hf, tl=tl, ex_=ex_: e.scalar_tensor_tensor(
                            out=acc[:, tt, hf * 512:(hf + 1) * 512], in0=PS(po), scalar=G[:, tl, ex_:ex_ + 1], in1=acc[:, tt, hf * 512:(hf + 1) * 512],
                            op0=ALU.mult, op1=ALU.add), reads=[pk(po), ("G", tl), ("acc", tt)], writes=[("acc", tt)])

            def final_norm(hh, tt):
                tl = hh * 16 + tt
                j, s = tl // 8, tl % 8
                b2 = tt % 2
                K.op("act", lambda e, tt=tt, tl=tl: e.activation(out=ojunk, in_=acc[:, tt, :], func=AF.Square, accum_out=ostat[:, tl, 0:1]),
                     reads=[("acc", tt)], writes=["ojunk", ("ostat", tl)])
                K.op("act", lambda e, tl=tl: e.activation(out=ostat[:, tl, 1:2], in_=ostat[:, tl, 0:1], func=AF.Sqrt, scale=1.0 / 1024.0, bias=small[:, 0:1]),
                     reads=[("ostat", tl), "small"], writes=[("ostat", tl)])
                K.op("dve", lambda e, tl=tl: e.reciprocal(out=ostat[:, tl, 2:3], in_=ostat[:, tl, 1:2]), reads=[("ostat", tl)], writes=[("ostat", tl)])
                K.op("dve", lambda e, tt=tt, tl=tl, b2=b2: e.scalar_tensor_tensor(out=ob[b2], in0=acc[:, tt, :], scalar=ostat[:, tl, 2:3], in1=fin_bc, op0=ALU.mult, op1=ALU.mult),
                     reads=[("acc", tt), ("ostat", tl), "fin_bc"], writes=[("ob", b2)])
                K.dma("sp", y_r[s, j * 128:(j + 1) * 128, :], ob[b2], reads=[("ob", b2)], writes=[("y", tl)])

            items = [(hh, ex_, tg) for hh in range(2) for ex_ in range(32) for tg in range(4)]
            wload(0, 0)
            hgu(0, 0, 0, 0)
            for it_ in range(len(items)):
                hh, ex_, tg = items[it_]
                if it_ + 1 < len(items):
                    nh, ne, ntg = items[it_ + 1]
                    if ntg == 0:
                        wload(ne, nh * 32 + ne)
                    hgu(nh, ne, ntg, it_ + 1)
                down(hh, ex_, tg, it_)
                if ex_ == 31:
                    for t4 in range(4):
                        final_norm(hh, tg * 4 + t4)
                        if hh == 0:
                            acc_load(1, tg * 4 + t4)
        except _Stop:
            pass
        K.finish()

        @block.tensor
        def _(e):
            for f in K.eng["pe"]["prog"]:
                f(e)

        @block.scalar
        def _(e):
            for f in K.eng["act"]["prog"]:
                f(e)

        @block.vector
        def _(e):
            for f in K.eng["dve"]["prog"]:
                f(e)

        @block.gpsimd
        def _(e):
            for f in K.eng["pool"]["prog"]:
                f(e)

        @block.sync
        def _(e):
            for f in K.eng["sp"]["prog"]:
                f(e)
    return nc


_CONST = None


def _consts():
    global _CONST
    if _CONST is not None:
        return _CONST
    c = {}
    c["c_ident"] = np.eye(128, dtype=np.float32)
    s_in = np.arange(128) // 16
    mf = (s_in[:, None] <= s_in[None, :]).astype(np.float32)
    mr = (s_in[:, None] >= s_in[None, :]).astype(np.float32)
    c["c_masks"] = np.concatenate([mf, mr], axis=1)
    kid = np.zeros((128, 18), np.float32)
    s = np.arange(8)
    kid[0:64, 0:8] = s + 1
    kid[64:128, 0:8] = 8 - s
    kid[:, 8:16] = 8 - kid[:, 0:8]
    kid[:, 16] = 1
    kid[:, 17] = 8
    c["c_kidx"] = kid
    rep = np.zeros((16, 128), np.float32)
    for p in range(128):
        rep[p % 16, p] = 1.0
    c["c_rep16"] = rep
    jj = np.arange(128)
    ang = 2 * np.pi * np.outer(jj, jj) / 128.0
    sc = 1.0 / math.sqrt(L * 128.0)
    c["c_dftc"] = np.concatenate([np.cos(ang) * sc, np.sin(ang) * sc], axis=1).astype(np.float32)
    ii = np.arange(128)
    tab = np.zeros((128, 2, 64, 128), dtype=ml_dtypes.bfloat16)
    for s_ in range(8):
        for s2 in range(8):
            mm = ((8 * ii[:, None] + s_) * (8 * ii[None, :] + s2)) % L
            a = 2 * np.pi * mm / L
            tab[:, 0, s2 * 8 + s_, :] = np.cos(a).astype(ml_dtypes.bfloat16)
            tab[:, 1, s2 * 8 + s_, :] = np.sin(a).astype(ml_dtypes.bfloat16)
    c["c_tab"] = tab
    _CONST = c
    return c


_NC_CACHE = {}


def _get_nc(dbg=()):
    key = tuple(dbg)
    if key not in _NC_CACHE:
        _NC_CACHE[key] = build_nc(dbg)
    return _NC_CACHE[key]


def make_in_maps(inputs, cores):
    c = _consts()
    f = lambda a: np.ascontiguousarray(np.asarray(a, dtype=np.float32))
    shared = {
        "w_ada": f(inputs["w_ada"][0]), "b_ada": f(inputs["b_ada"][0]), "w_in": f(inputs["w_in"][0]),
        "lam_re": f(inputs["s5_lam_re"][0]), "lam_im": f(inputs["s5_lam_im"][0]),
        "b_re": f(inputs["s5_b_re"][0]), "b_im": f(inputs["s5_b_im"][0]),
        "c_re": f(inputs["s5_c_re"][0]), "c_im": f(inputs["s5_c_im"][0]),
        "log_step": f(inputs["s5_log_step"][0]), "s5_d": f(inputs["s5_d"][0]),
        "w_glu": f(inputs["s5_w_glu"][0]), "four_w": f(inputs["fourier_w"][0]), "w_out": f(inputs["w_out"][0]),
        "w_rt": f(np.concatenate([np.asarray(inputs["moe_w_group"][0]), np.asarray(inputs["moe_w_router"][0]).reshape(D, 32)], axis=1)),
        "b_rt": f(np.concatenate([np.asarray(inputs["moe_b_group"][0]), np.asarray(inputs["moe_b_router"][0]).reshape(32)])),
        "w_gate": f(inputs["moe_w_gate"][0]), "w_up": f(inputs["moe_w_up"][0]), "w_down": f(inputs["moe_w_down"][0]),
        "fin_g": f(inputs["final_norm_g"]),
    }
    shared.update(c)
    maps = []
    for b in cores:
        rows = np.zeros((128, 128), np.float32)
        rows[0:8] = np.asarray(inputs["c"][b]).reshape(8, 128)
        rows[8:16] = np.asarray(inputs["c_ctx"]).reshape(8, 128)
        rows[16:24] = np.asarray(inputs["norm1_g"][0]).reshape(8, 128)
        rows[24:32] = np.asarray(inputs["norm2_g"][0]).reshape(8, 128)
        rows[32:80] = np.asarray(inputs["b_ada"][0]).reshape(48, 128)
        rows[80:84] = np.asarray(inputs["mix_norm_s5_g"][0]).reshape(4, 128)
        rows[84:88] = np.asarray(inputs["mix_norm_f_g"][0]).reshape(4, 128)
        m = dict(shared)
        m["x"] = f(inputs["x"][b])
        m["ctx"] = f(inputs["ctx"][b])
        m["rows_in"] = rows
        maps.append(m)
    return maps


def kernel(**inputs):
    nc = _get_nc()
    maps = make_in_maps(inputs, list(range(8)))
    res = run_bass_kernel_spmd(nc, maps, core_ids=list(range(8)))
    return np.stack([np.asarray(r["y"], dtype=np.float32) for r in res.results], axis=0)
```

```python
import math
import numpy as np
import ml_dtypes
import concourse.bass as bass
import concourse.mybir as mybir
from concourse.bass_utils import run_bass_kernel_spmd

F32 = mybir.dt.float32
BF16 = mybir.dt.bfloat16
I32 = mybir.dt.int32
AF = mybir.ActivationFunctionType
ALU = mybir.AluOpType
AX = mybir.AxisListType

L = 4096
D = 1024
NCH = 512
NCX = 32
NE = 544
EPS = 1e-6
TWO_PI = 2.0 * math.pi
ARENA = 106000

DEBUG = False
SELF_ORDERED = ("pe",)


class Sched:
    def __init__(self, nc, sems, dma_sems):
        self.nc = nc
        self.eng = {n: dict(prog=[], sem=sems[n], count=0, known={}) for n in ("pe", "act", "dve", "pool", "sp")}
        self.dma_sems = dma_sems
        self.dma_counts = [0] * len(dma_sems)
        self.dma_rr = 0
        self.dma_rr_sw = 0
        self.last_w = {}
        self.readers = {}

    def _deps(self, reads, writes):
        toks = []
        for k in reads:
            t = self.last_w.get(k)
            if t:
                toks.append(t)
        for k in writes:
            t = self.last_w.get(k)
            if t:
                toks.append(t)
            toks += list(self.readers.get(k, {}).values())
        return toks

    def _waits(self, ename, toks):
        E = self.eng[ename]
        need = {}
        for (sid, sem, val, src) in toks:
            if src == ename and ename in SELF_ORDERED:
                continue
            if E["known"].get(sid, 0) >= val:
                continue
            if need.get(sid, (None, 0))[1] < val:
                need[sid] = (sem, val)
        out = []
        for sid, (sem, val) in need.items():
            E["known"][sid] = val
            out.append((sem, val))
        return out

    def _commit(self, tok, reads, writes):
        for k in writes:
            self.last_w[k] = tok
            self.readers[k] = {}
        for k in reads:
            d = self.readers.setdefault(k, {})
            o = d.get(tok[0])
            if o is None or o[2] < tok[2]:
                d[tok[0]] = tok

    def op(self, ename, fn, reads=(), writes=()):
        E = self.eng[ename]
        waits = self._waits(ename, self._deps(reads, writes))
        E["count"] += 1
        val = E["count"]
        sem = E["sem"]

        def run(e, fn=fn, waits=waits, sem=sem):
            for (s, v) in waits:
                e.wait_ge(s, v)
            fn(e).then_inc(sem, 1)
        E["prog"].append(run)
        tok = (id(sem), sem, val, ename)
        self._commit(tok, reads, writes)
        return tok

    def dma(self, q, out, in_, reads=(), writes=(), **kw):
        E = self.eng[q]
        nsw = 12
        nhw = len(self.dma_sems) - nsw
        if q == "pool":
            i = nhw + self.dma_rr_sw
            self.dma_rr_sw = (self.dma_rr_sw + 1) % nsw
        else:
            i = self.dma_rr
            self.dma_rr = (i + 1) % nhw
        sem = self.dma_sems[i]
        prev = self.dma_counts[i]
        toks = self._deps(reads, writes)
        if prev > 0:
            toks.append((id(sem), sem, 16 * prev, "dma"))
        waits = self._waits(q, toks)
        self.dma_counts[i] += 1
        val = 16 * self.dma_counts[i]

        def run(e, waits=waits, sem=sem, out=out, in_=in_, kw=kw):
            for (s, v) in waits:
                e.wait_ge(s, v)
            e.dma_start(out=out, in_=in_, **kw).then_inc(sem, 16)
        E["prog"].append(run)
        tok = (id(sem), sem, val, "dma")
        self._commit(tok, reads, writes)
        return tok

    def barrier(self):
        toks = []
        for n, E in self.eng.items():
            if E["count"] > 0:
                toks.append((id(E["sem"]), E["sem"], E["count"], "bar"))
        for i, s in enumerate(self.dma_sems):
            if self.dma_counts[i] > 0:
                toks.append((id(s), s, 16 * self.dma_counts[i], "bar"))
        for n, E in self.eng.items():
            waits = self._waits(n, toks)
            if waits:
                def run(e, waits=waits):
                    for (s, v) in waits:
                        e.wait_ge(s, v)
                E["prog"].append(run)

    def finish(self):
        toks = []
        for i, s in enumerate(self.dma_sems):
            if self.dma_counts[i] > 0:
                toks.append((id(s), s, 16 * self.dma_counts[i], "bar"))
        waits = self._waits("sp", toks)

        def run(e, waits=waits):
            for (s, v) in waits:
                e.wait_ge(s, v)
        self.eng["sp"]["prog"].append(run)


class Arena:
    def __init__(self, ap):
        self.ap = ap
        self.top = 0

    def alloc(self, shape, dtype, parts=128):
        n = 1
        for d in shape:
            n *= d
        nb = n * (2 if dtype == BF16 else 4)
        nb = (nb + 63) // 64 * 64
        off = self.top
        self.top += nb // 2
        assert self.top <= ARENA, f"arena overflow {self.top}"
        v = self.ap[0:parts, off:off + nb // 2]
        if dtype != BF16:
            v = v.bitcast(dtype)
        v = v[:, 0:n]
        if len(shape) == 2:
            return v.rearrange("p (a b) -> p a b", a=shape[0])
        if len(shape) == 3:
            return v.rearrange("p (a b c) -> p a b c", a=shape[0], b=shape[1])
        if len(shape) == 4:
            return v.rearrange("p (a b c d) -> p a b c d", a=shape[0], b=shape[1], c=shape[2])
        return v


def build_nc(dbg=(), stop_after=99):
    nc = bass.Bass("TRN2", target_bir_lowering=False)

    def din(name, shape, dt=F32):
        return nc.dram_tensor(name, list(shape), dt, kind="ExternalInput").ap()

    x = din("x", [L, D])
    ctx = din("ctx", [256, D])
    rows_in = din("rows_in", [128, 128])
    w_ada = din("w_ada", [D, 6 * D])
    b_ada = din("b_ada", [6 * D])
    w_in = din("w_in", [D, D])
    lam_re = din("lam_re", [2, 32, 64])
    lam_im = din("lam_im", [2, 32, 64])
    b_re = din("b_re", [2, 32, 64, 16])
    b_im = din("b_im", [2, 32, 64, 16])
    c_re = din("c_re", [2, 32, 16, 64])
    c_im = din("c_im", [2, 32, 16, 64])
    log_step = din("log_step", [2, 32])
    s5_d = din("s5_d", [512])
    w_glu = din("w_glu", [512, 512])
    four_w = din("four_w", [4, 128, 128])
    w_out = din("w_out", [D, D])
    w_rt = din("w_rt", [D, 36])
    b_rt = din("b_rt", [36])
    w_gate = din("w_gate", [32, D, 256])
    w_up = din("w_up", [32, D, 256])
    w_down = din("w_down", [32, 256, D])
    fin_g = din("fin_g", [D])
    c_ident = din("c_ident", [128, 128])
    c_masks = din("c_masks", [128, 256])
    c_kidx = din("c_kidx", [128, 18])
    c_rep16 = din("c_rep16", [16, 128])
    c_dftc = din("c_dftc", [128, 256])
    c_tab = din("c_tab", [128, 2, 64, 128], BF16)
    y = nc.dram_tensor("y", [L, D], F32, kind="ExternalOutput").ap()
    x1_scr = nc.dram_tensor("x1_scr", [L, D], F32, kind="Internal").ap()
    tT_scr = nc.dram_tensor("tT_scr", [8, 128, L], BF16, kind="Internal").ap()
    fT_scr = nc.dram_tensor("fT_scr", [32, 128, 4, 128], BF16, kind="Internal").ap()
    y5_scr = nc.dram_tensor("y5_scr", [L, 512], F32, kind="Internal").ap()
    dbg_out = {}
    for name, shape in dbg:
        dbg_out[name] = nc.dram_tensor(name, list(shape), F32, kind="ExternalOutput").ap()

    from contextlib import ExitStack
    with ExitStack() as es:
        arena_t = es.enter_context(nc.sbuf_tensor("arena", [128, ARENA], BF16))
        ps_t = es.enter_context(nc.psum_tensor("ps", [128, 4096], F32))
        sems = {n: es.enter_context(nc.semaphore("sem_" + n)) for n in ("pe", "act", "dve", "pool", "sp")}
        dma_sems = [es.enter_context(nc.semaphore(f"dsem{i}")) for i in range(40)]
        block = es.enter_context(nc.Block())
        K = Sched(nc, sems, dma_sems)
        A = Arena(arena_t)

        def PS(b, n=512, off=0):
            return ps_t[:, b * 512 + off: b * 512 + off + n]

        def PSB(b):
            return ps_t[:, b * 512:(b + 1) * 512].bitcast(BF16)

        def pk(b):
            return ("ps", b)

        def cp(eng, out, in_, reads, writes):
            if eng == "act":
                K.op("act", lambda e: e.activation(out=out, in_=in_, func=AF.Identity), reads=reads, writes=writes)
            else:
                K.op(eng, lambda e: e.tensor_copy(out=out, in_=in_), reads=reads, writes=writes)

        def dump(name, src_ap, key):
            if name in dbg_out:
                K.dma("sp", dbg_out[name], src_ap, reads=[key], writes=[("dbg", name)])

        ident_f = A.alloc([128], F32)
        ident_b = A.alloc([128], BF16)
        masks = A.alloc([256], F32)
        kidx = A.alloc([18], F32)
        rep16 = A.alloc([128], F32)
        dftc = A.alloc([256], F32)
        cols = A.alloc([128], F32)
        scol = A.alloc([16], F32)
        modc = A.alloc([48, 2], F32)
        vecs = A.alloc([8, 8], F32)
        g1_bc = A.alloc([1024], F32)
        g2_bc = A.alloc([1024], F32)
        fin_bc = A.alloc([1024], F32)
        brt_bc = A.alloc([36], F32)
        Dcol = A.alloc([32], F32)
        AcAs = A.alloc([4, 256], BF16)
        wrt = A.alloc([8, 36], F32)
        small = A.alloc([64], F32)
        persist_top = A.top

        class _Stop(Exception):
            pass

        def chk(k):
            if stop_after == k:
                raise _Stop()

        try:
            K.dma("sp", ident_f, c_ident, writes=["ident_f"])
            K.dma("sp", masks, c_masks, writes=["masks"])
            K.dma("sp", kidx, c_kidx, writes=["kidx"])
            K.dma("sp", rep16[0:16, :], c_rep16, writes=["rep16"])
            K.dma("sp", dftc, c_dftc, writes=["dftc"])
            K.dma("sp", g1_bc, b_ada[2048:3072].partition_broadcast(128), writes=["g1_bc"])
            K.dma("sp", g2_bc, b_ada[5120:6144].partition_broadcast(128), writes=["g2_bc"])
            K.dma("sp", fin_bc, fin_g.partition_broadcast(128), writes=["fin_bc"])
            K.dma("sp", brt_bc, b_rt.partition_broadcast(128), writes=["brt_bc"])
            K.dma("sp", wrt, w_rt.rearrange("(kc p) n -> p kc n", p=128), writes=["wrt"])
            K.op("dve", lambda e: e.tensor_copy(out=ident_b, in_=ident_f), reads=["ident_f"], writes=["ident_b"])

            p2start = A.top
            U = A.alloc([32, 576], BF16)
            PQ = A.alloc([32, 1024], BF16)
            p2mark = A.top
            w_in_bf = A.alloc([8, 1024], BF16)
            w_in_r = w_in.rearrange("(kc p) n -> p kc n", p=128)
            for kc in range(8):
                K.dma("pool", w_in_bf[:, kc, :], w_in_r[:, kc, :], writes=["w_in_bf"])
            mark = A.top
            rows_sb = A.alloc([128], F32)
            srep = A.alloc([8, 128], F32)
            wa = [A.alloc([8, 512], F32) for _ in range(2)]
            K.dma("sp", rows_sb, rows_in, writes=["rows_sb"])
            K.op("pe", lambda e: e.transpose(PS(0, 128), rows_sb, ident_f), reads=["rows_sb", "ident_f"], writes=[pk(0)])
            K.op("dve", lambda e: e.tensor_copy(out=cols, in_=PS(0, 128)), reads=[pk(0)], writes=["cols"])
            K.op("act", lambda e: e.activation(out=scol, in_=cols[:, 0:16], func=AF.Silu), reads=["cols"], writes=["scol"])
            K.op("dve", lambda e: e.tensor_copy(out=srep, in_=scol[:, 0:8].unsqueeze(2).to_broadcast([128, 8, 128])),
                 reads=["scol"], writes=["srep"])
            wa_rr = [w_ada.rearrange("(kc p) n -> p kc n", p=128)[:, :, cc * 512:(cc + 1) * 512] for cc in range(12)]
            for cc in range(12):
                buf = wa[cc % 2]
                K.dma("sp" if cc % 2 == 0 else "act", buf, wa_rr[cc], writes=[("wa", cc % 2)])
                for fb in range(4):
                    j = cc * 4 + fb
                    for kc in range(8):
                        K.op("pe", lambda e, buf=buf, fb=fb, kc=kc, j=j: e.matmul(
                            PS(1, 2, 2 * j), lhsT=buf[:, kc, fb * 128:(fb + 1) * 128], rhs=scol[:, kc:16:8],
                            start=(kc == 0), stop=(kc == 7)),
                            reads=[("wa", cc % 2), "scol"], writes=[pk(1)])
                if cc in (4, 5, 10, 11):
                    tgt = g1_bc if cc in (4, 5) else g2_bc
                    tk = "g1_bc" if cc in (4, 5) else "g2_bc"
                    half = cc % 2 if cc in (4, 5) else (cc - 10)
                    for kc in range(8):
                        K.op("pe", lambda e, buf=buf, kc=kc: e.matmul(
                            PS(2), lhsT=srep[:, kc, :], rhs=buf[:, kc, :], start=(kc == 0), stop=(kc == 7)),
                            reads=[("wa", cc % 2), "srep"], writes=[pk(2)])
                    K.op("dve", lambda e, tgt=tgt, half=half: e.tensor_tensor(
                        out=tgt[:, half * 512:(half + 1) * 512], in0=PS(2), in1=tgt[:, half * 512:(half + 1) * 512], op=ALU.add),
                        reads=[pk(2), tk], writes=[tk])
            K.op("dve", lambda e: e.tensor_tensor(
                out=modc, in0=PS(1, 96).rearrange("p (j t) -> p j t", t=2),
                in1=cols[:, 32:80].unsqueeze(2).to_broadcast([128, 48, 2]), op=ALU.add),
                reads=[pk(1), "cols"], writes=["modc"])
            def vec_ops(e_name, dst, g_lo, sc_j, which):
                K.op("dve", lambda e: e.scalar_tensor_tensor(
                    out=vecs[:, dst, :], in0=modc[:, sc_j:sc_j + 8, which], scalar=1.0, in1=cols[:, g_lo:g_lo + 8],
                    op0=ALU.add, op1=ALU.mult), reads=["modc", "cols"], writes=["vecs"])
            vec_ops("dve", 0, 16, 8, 0)
            vec_ops("dve", 2, 16, 8, 1)
            vec_ops("dve", 4, 24, 32, 0)
            K.op("dve", lambda e: e.tensor_copy(out=vecs[:, 1, :], in_=modc[:, 0:8, 0]), reads=["modc"], writes=["vecs"])
            K.op("dve", lambda e: e.tensor_copy(out=vecs[:, 3, :], in_=modc[:, 0:8, 1]), reads=["modc"], writes=["vecs"])
            K.op("dve", lambda e: e.tensor_copy(out=vecs[:, 5, :], in_=modc[:, 24:32, 0]), reads=["modc"], writes=["vecs"])
            if "d_mod" in dbg_out:
                K.dma("sp", dbg_out["d_mod"], modc.rearrange("p j t -> p (j t)"), reads=["modc"], writes=[("dbg", 0)])
                K.dma("sp", dbg_out["d_g1"], g1_bc, reads=["g1_bc"], writes=[("dbg", 1)])
            drow = A.alloc([32], F32)
            with nc.allow_non_contiguous_dma(reason="tiny s5_d gather"):
                K.dma("sp", drow[0:16, :], s5_d.rearrange("(g h) -> h g", h=16), writes=["drow"], allow_slow_non_contiguous=True)
            K.op("pe", lambda e: e.matmul(PS(3, 32), lhsT=rep16[0:16, :], rhs=drow[0:16, :], start=True, stop=True),
                 reads=["rep16", "drow"], writes=[pk(3)])
            K.op("dve", lambda e: e.tensor_copy(out=Dcol, in_=PS(3, 32)), reads=[pk(3)], writes=["Dcol"])
            wf = A.alloc([4, 128], F32)
            K.dma("sp", wf, four_w.rearrange("g m d -> m g d"), writes=["wf"])
            for g in range(4):
                for t in range(2):
                    K.op("pe", lambda e, g=g, t=t: e.matmul(
                        PS(4, 128, (g * 2 + t) * 128 % 512) if False else ps_t[:, 4 * 512 + (g * 2 + t) * 128: 4 * 512 + (g * 2 + t) * 128 + 128],
                        lhsT=dftc[:, t * 128:(t + 1) * 128], rhs=wf[:, g, :], start=True, stop=True),
                        reads=["dftc", "wf"], writes=[pk(4 + (g * 2 + t) // 4)])
            K.op("dve", lambda e: e.tensor_copy(out=AcAs.rearrange("p g n -> p (g n)"), in_=ps_t[:, 4 * 512:6 * 512]),
                 reads=[pk(4), pk(5)], writes=["AcAs"])
            K.barrier()
            A.top = mark

            chk(1)
            xt = [A.alloc([1024], F32) for _ in range(4)]
            junk2 = [A.alloc([1024], BF16) for _ in range(2)]
            junk = A.alloc([1024], BF16)
            xs = [A.alloc([1024], BF16) for _ in range(2)]
            hT = [A.alloc([8, 128], BF16) for _ in range(2)]
            Zs = [A.alloc([32, 128], BF16) for _ in range(2)]
            vT = [A.alloc([4, 128], BF16) for _ in range(2)]
            stat = A.alloc([40, 4], F32)
            x_r = x.rearrange("(c s) d -> s c d", s=8)
            ctx_r = ctx.rearrange("(c s) d -> s c d", s=8)

            def rms_prep(ti, src_key, src_ap, rows, nfeat, st, junk_ap, out_bf, out_key, eng_scale="dve", junk_key="junk"):
                K.op("act", lambda e: e.activation(out=junk_ap, in_=src_ap, func=AF.Square, accum_out=st[0:rows, 0:1]),
                     reads=[src_key], writes=[junk_key, ("stat", ti)])
                K.op("act", lambda e: e.activation(out=st[0:rows, 1:2], in_=st[0:rows, 0:1], func=AF.Sqrt,
                                                   scale=1.0 / nfeat, bias=small[0:rows, 0:1]),
                     reads=[("stat", ti), "small"], writes=[("stat", ti)])
                K.op("dve", lambda e: e.reciprocal(out=st[0:rows, 2:3], in_=st[0:rows, 1:2]),
                     reads=[("stat", ti)], writes=[("stat", ti)])
                K.op(eng_scale, lambda e: e.tensor_scalar(out=out_bf, in0=src_ap, scalar1=st[0:rows, 2:3], scalar2=None, op0=ALU.mult),
                     reads=[src_key, ("stat", ti)], writes=[out_key])

            K.op("dve", lambda e: e.memset(small[:, 0:1], EPS), writes=["small"])

            tiles = [("c", s, 0) for s in range(8)] + [("x", s, j) for j in range(4) for s in range(8)]
            def p2_tile(ti):
                kind, s, j = tiles[ti]
                rows = 32 if kind == "c" else 128
                b2 = ti % 2
                b4 = ti % 4
                rms_prep(ti, ("xt", b4), xt[b4][0:rows, :], rows, 1024.0, stat[:, ti, :], junk2[b2][0:rows, :], xs[b2][0:rows, :], ("xs", b2), junk_key=("junk2", b2))
                yield
                pb = 0 + b2
                for kc in range(8):
                    K.op("pe", lambda e, kc=kc, b2=b2, pb=pb, rows=rows: e.transpose(
                        PSB(pb)[:, kc * 128: kc * 128 + rows], xs[b2][0:rows, kc * 128:(kc + 1) * 128], ident_b[0:rows, 0:rows]),
                        reads=[("xs", b2), "ident_b"], writes=[pk(pb)])
                yield
                av, sv = (2, 3) if kind == "c" else (0, 1)
                psv = PSB(pb).rearrange("p (k t) -> p k t", k=8)[:, :, 0:rows]
                K.op("dve", lambda e, psv=psv, b2=b2, rows=rows, av=av: e.tensor_tensor(
                    out=hT[b2][:, :, 0:rows], in0=psv, in1=vecs[:, av, :].unsqueeze(2).to_broadcast([128, 8, rows]), op=ALU.mult),
                    reads=[pk(pb), "vecs"], writes=[("hT", b2)])
                K.op("pool", lambda e, b2=b2, rows=rows, sv=sv: e.tensor_tensor(
                    out=hT[b2][:, :, 0:rows], in0=hT[b2][:, :, 0:rows], in1=vecs[:, sv, :].unsqueeze(2).to_broadcast([128, 8, rows]), op=ALU.add),
                    reads=[("hT", b2), "vecs"], writes=[("hT", b2)])
                yield
                zb = (ti // 8) % 2
                pz = 2 + b2
                for kc in range(8):
                    K.op("pe", lambda e, kc=kc, b2=b2, pz=pz, rows=rows: e.matmul(
                        PS(pz)[0:rows, :], lhsT=hT[b2][:, kc, 0:rows], rhs=w_in_bf[:, kc, 0:512], start=(kc == 0), stop=(kc == 7)),
                        reads=[("hT", b2), "w_in_bf"], writes=[pk(pz)])
                K.op("act", lambda e, zb=zb, s=s, pz=pz, rows=rows: e.activation(
                    out=Zs[zb][0:rows].rearrange("p g (s h) -> p g s h", s=8)[:, :, s, :],
                    in_=PS(pz)[0:rows, :].rearrange("p (g h) -> p g h", g=32), func=AF.Identity),
                     reads=[pk(pz)], writes=[("Zs", zb, s)])
                yield
                if kind == "x":
                    pv = 4 + b2
                    for g in range(4):
                        for kc in range(8):
                            K.op("pe", lambda e, g=g, kc=kc, b2=b2, pv=pv: e.matmul(
                                PS(pv, 128, g * 128), lhsT=w_in_bf[:, kc, 512 + g * 128: 512 + (g + 1) * 128], rhs=hT[b2][:, kc, :],
                                start=(kc == 0), stop=(kc == 7)), reads=[("hT", b2), "w_in_bf"], writes=[pk(pv)])
                    K.op("act", lambda e, b2=b2, pv=pv: e.activation(out=vT[b2].rearrange("p g t -> p (g t)"), in_=PS(pv), func=AF.Identity),
                         reads=[pk(pv)], writes=[("vT", b2)])
                    tl = j * 8 + s
                    for g in range(4):
                        K.op("pe", lambda e, g=g, b2=b2: e.matmul(
                            PS(6 + g // 2, 256, (g % 2) * 256), lhsT=vT[b2][:, g, :], rhs=AcAs[:, g, :], start=True, stop=True),
                            reads=[("vT", b2), "AcAs"], writes=[pk(6 + g // 2)])
                    K.op("dve", lambda e, tl=tl: e.tensor_copy(
                        out=PQ[:, tl, :].rearrange("p (r g m) -> p g r m", r=2, g=4),
                        in_=ps_t[:, 6 * 512:8 * 512].rearrange("p (g r m) -> p g r m", g=4, r=2)),
                         reads=[pk(6), pk(7)], writes=[("PQ", tl)])
                yield
                if s == 7:
                    nrow = rows
                    if kind == "c":
                        col_sets = [(0, 32), (544, 576)]
                    else:
                        col_sets = [(32 + 128 * j, 32 + 128 * (j + 1))]
                    for g8 in range(4):
                        pu = 6 + (g8 % 2)
                        for gi in range(8):
                            g = g8 * 8 + gi
                            K.op("pe", lambda e, g=g, gi=gi, zb=zb, pu=pu, nrow=nrow: e.transpose(
                                PSB(pu)[:, gi * 128: gi * 128 + nrow], Zs[zb][0:nrow, g, :], ident_b[0:nrow, 0:nrow]),
                                reads=[("Zs", zb, ss_) for ss_ in range(8)] + ["ident_b"], writes=[pk(pu)])
                        for (c0, c1) in col_sets:
                            cp("act" if g8 % 2 == 0 else "dve", U[:, g8 * 8:(g8 + 1) * 8, c0:c1],
                               PSB(pu).rearrange("p (g c) -> p g c", g=8)[:, :, 0:nrow], [pk(pu)], [("U", g8)])

            def p2_load(ti):
                kind, s, j = tiles[ti]
                rows = 32 if kind == "c" else 128
                src = ctx_r[s, :, :] if kind == "c" else x_r[s, j * 128:(j + 1) * 128, :]
                K.dma("sp", xt[ti % 4][0:rows, :], src, writes=[("xt", ti % 4)])
            p2_load(0); p2_load(1)
            for pr in range(len(tiles) // 2):
                if 2 * pr + 3 < len(tiles):
                    p2_load(2 * pr + 2); p2_load(2 * pr + 3)
                gens = [p2_tile(2 * pr), p2_tile(2 * pr + 1)]
                alive = [True, True]
                while any(alive):
                    for gi_, g_ in enumerate(gens):
                        if alive[gi_]:
                            try:
                                next(g_)
                            except StopIteration:
                                alive[gi_] = False
            K.barrier()
            A.top = p2mark

            chk(2)
            p3mark = A.top
            fnTt = [A.alloc([4, 128], BF16) for _ in range(2)]
            tab = A.alloc([2, 64, 128], BF16)
            fsb = [A.alloc([512], F32) for _ in range(2)]
            fmn = [A.alloc([512], F32) for _ in range(2)]
            fnb = [A.alloc([512], BF16) for _ in range(2)]
            fjunk = A.alloc([512], BF16)
            fstat = A.alloc([32, 4], F32)
            PQv = PQ.rearrange("p t (r n) -> p t r n", r=2)
            for r in range(2):
                for so_ in range(8):
                    K.dma("sp" if r == 0 else "act", tab[:, r, so_ * 8:(so_ + 1) * 8, :], c_tab[:, r, so_ * 8:(so_ + 1) * 8, :], writes=[("tab", r, so_)])
            hoist_lo = A.top
            Lrows = A.alloc([2, 128], F32)
            LR = A.alloc([32], F32); LI = A.alloc([32], F32); LS = A.alloc([32], F32)
            T = {n: A.alloc([32, 18], F32) for n in ("AK", "ANG", "ANGC", "TF", "SINK", "COSK", "MAGP", "MAGN", "LKR", "LKI")}
            TI = A.alloc([32, 18], I32)
            S = {n: A.alloc([32], F32) for n in ("dt", "a", "th", "nr", "den", "rden", "qre", "qim", "t1", "t2")}
            W = {n: A.alloc([32, 8], F32) for n in ("ire", "iim", "wre", "wim", "w2re", "w2im", "t1", "t2")}
            K.dma("sp", Lrows[0:32, 0, :].rearrange("g (d p) -> g d p", d=2), lam_re.rearrange("d g p -> g d p"), writes=["Lrows"])
            K.dma("sp", Lrows[0:32, 1, :].rearrange("g (d p) -> g d p", d=2), lam_im.rearrange("d g p -> g d p"), writes=["Lrows"])
            for d in range(2):
                K.dma("sp", LS[64 * d:64 * d + 64, :], log_step[d].partition_broadcast(64), writes=["LS"])
            for r, dst, key in ((0, LR, "LR"), (1, LI, "LI")):
                K.op("pe", lambda e, r=r: e.transpose(PS(0, 32), Lrows[0:32, r, :], ident_f[0:32, 0:32]), reads=["Lrows", "ident_f"], writes=[pk(0)])
                cp("dve", dst, PS(0, 32), [pk(0)], [key])
            def dve(fn, reads, writes, eng="dve"):
                K.op(eng, fn, reads=reads, writes=writes)
            K.op("act", lambda e: e.activation(out=S["dt"], in_=LS, func=AF.Exp), reads=["LS"], writes=["dt"])
            dve(lambda e: e.tensor_tensor(out=S["a"], in0=LR, in1=S["dt"], op=ALU.mult), ["LR", "dt"], ["a"])
            dve(lambda e: e.tensor_tensor(out=S["th"], in0=LI, in1=S["dt"], op=ALU.mult), ["LI", "dt"], ["th"])
            kidx_b = kidx.unsqueeze(1).to_broadcast([128, 32, 18])
            dve(lambda e: e.tensor_tensor(out=T["AK"], in0=S["a"].unsqueeze(2).to_broadcast([128, 32, 18]), in1=kidx_b, op=ALU.mult), ["a", "kidx"], ["AK"])
            dve(lambda e: e.tensor_tensor(out=T["ANG"], in0=S["th"].unsqueeze(2).to_broadcast([128, 32, 18]), in1=kidx_b, op=ALU.mult), ["th", "kidx"], ["ANG"])
            dve(lambda e: e.tensor_scalar(out=T["ANGC"], in0=T["ANG"], scalar1=math.pi / 2, scalar2=None, op0=ALU.add), ["ANG"], ["ANGC"])
            for src, dst in (("ANG", "SINK"), ("ANGC", "COSK")):
                dve(lambda e, src=src: e.tensor_scalar(out=TI, in0=T[src], scalar1=1.0 / TWO_PI, scalar2=None, op0=ALU.mult), [src], ["TI"])
                dve(lambda e: e.tensor_copy(out=T["TF"], in_=TI), ["TI"], ["TF"])
                dve(lambda e, src=src: e.scalar_tensor_tensor(out=T["TF"], in0=T["TF"], scalar=-TWO_PI, in1=T[src], op0=ALU.mult, op1=ALU.add), ["TF", src], ["TF"])
                dve(lambda e: e.tensor_scalar(out=T["TF"], in0=T["TF"], scalar1=-3.14159, scalar2=3.14159, op0=ALU.max, op1=ALU.min), ["TF"], ["TF"])
                K.op("act", lambda e, dst=dst: e.activation(out=T[dst], in_=T["TF"], func=AF.Sin), reads=["TF"], writes=[dst])
            K.op("act", lambda e: e.activation(out=T["MAGP"], in_=T["AK"], func=AF.Exp), reads=["AK"], writes=["MAGP"])
            K.op("act", lambda e: e.activation(out=T["MAGN"], in_=T["AK"], func=AF.Exp, scale=-1.0), reads=["AK"], writes=["MAGN"])
            dve(lambda e: e.tensor_tensor(out=T["LKR"], in0=T["MAGP"], in1=T["COSK"], op=ALU.mult), ["MAGP", "COSK"], ["LKR"])
            dve(lambda e: e.tensor_tensor(out=T["LKI"], in0=T["MAGP"], in1=T["SINK"], op=ALU.mult), ["MAGP", "SINK"], ["LKI"])
            l1r = T["LKR"][:, :, 16]; l1i = T["LKI"][:, :, 16]
            dve(lambda e: e.tensor_scalar(out=S["nr"], in0=l1r, scalar1=-1.0, scalar2=None, op0=ALU.add), ["LKR"], ["nr"])
            dve(lambda e: e.tensor_tensor(out=S["den"], in0=LR, in1=LR, op=ALU.mult), ["LR"], ["den"])
            dve(lambda e: e.tensor_tensor(out=S["t1"], in0=LI, in1=LI, op=ALU.mult), ["LI"], ["t1"])
            dve(lambda e: e.tensor_tensor(out=S["den"], in0=S["den"], in1=S["t1"], op=ALU.add), ["den", "t1"], ["den"])
            dve(lambda e: e.reciprocal(out=S["rden"], in_=S["den"]), ["den"], ["rden"])
            dve(lambda e: e.tensor_tensor(out=S["t1"], in0=S["nr"], in1=LR, op=ALU.mult), ["nr", "LR"], ["t1"])
            dve(lambda e: e.tensor_tensor(out=S["t2"], in0=l1i, in1=LI, op=ALU.mult), ["LKI", "LI"], ["t2"])
            dve(lambda e: e.tensor_tensor(out=S["t1"], in0=S["t1"], in1=S["t2"], op=ALU.add), ["t1", "t2"], ["t1"])
            dve(lambda e: e.tensor_tensor(out=S["qre"], in0=S["t1"], in1=S["rden"], op=ALU.mult), ["t1", "rden"], ["qre"])
            dve(lambda e: e.tensor_tensor(out=S["t1"], in0=l1i, in1=LR, op=ALU.mult), ["LKI", "LR"], ["t1"])
            dve(lambda e: e.tensor_tensor(out=S["t2"], in0=S["nr"], in1=LI, op=ALU.mult), ["nr", "LI"], ["t2"])
            dve(lambda e: e.tensor_tensor(out=S["t1"], in0=S["t1"], in1=S["t2"], op=ALU.subtract), ["t1", "t2"], ["t1"])
            dve(lambda e: e.tensor_tensor(out=S["qim"], in0=S["t1"], in1=S["rden"], op=ALU.mult), ["t1", "rden"], ["qim"])
            dve(lambda e: e.tensor_tensor(out=W["ire"], in0=T["MAGN"][:, :, 0:8], in1=T["COSK"][:, :, 0:8], op=ALU.mult), ["MAGN", "COSK"], ["ire"])
            dve(lambda e: e.scalar_tensor_tensor(out=W["iim"], in0=T["MAGN"][:, :, 0:8], scalar=-1.0, in1=T["SINK"][:, :, 0:8], op0=ALU.mult, op1=ALU.mult), ["MAGN", "SINK"], ["iim"])
            qreb = S["qre"].unsqueeze(2).to_broadcast([128, 32, 8]); qimb = S["qim"].unsqueeze(2).to_broadcast([128, 32, 8])

            def cmul(ore, oim, ar, ai, br, bi, keys_a, keys_b, ko):
                dve(lambda e: e.tensor_tensor(out=W["t1"], in0=ar, in1=br, op=ALU.mult), keys_a + keys_b, ["Wt1"])
                dve(lambda e: e.tensor_tensor(out=W["t2"], in0=ai, in1=bi, op=ALU.mult), keys_a + keys_b, ["Wt2"])
                dve(lambda e: e.tensor_tensor(out=ore, in0=W["t1"], in1=W["t2"], op=ALU.subtract), ["Wt1", "Wt2"], [ko + "re"])
                dve(lambda e: e.tensor_tensor(out=W["t1"], in0=ar, in1=bi, op=ALU.mult), keys_a + keys_b + [ko + "re"], ["Wt1"])
                dve(lambda e: e.tensor_tensor(out=W["t2"], in0=ai, in1=br, op=ALU.mult), keys_a + keys_b + [ko + "re"], ["Wt2"])
                dve(lambda e: e.tensor_tensor(out=oim, in0=W["t1"], in1=W["t2"], op=ALU.add), ["Wt1", "Wt2"], [ko + "im"])
            cmul(W["wre"], W["wim"], W["ire"], W["iim"], qreb, qimb, ["ire", "iim"], ["qre", "qim"], "w")
            cmul(W["w2re"], W["w2im"], T["LKR"][:, :, 8:16], T["LKI"][:, :, 8:16], qreb, qimb, ["LKR", "LKI"], ["qre", "qim"], "w2")
            def dft_a(ot):
                b2 = ot % 2
                jo, so = ot // 8, ot % 8
                pA, pB = 2 * b2, 2 * b2 + 1
                plan = []
                for it in range(32):
                    ji, si = it // 8, it % 8
                    m = (ji * so + jo * si) % 4
                    ss = so * 8 + si
                    if m == 0:
                        plan += [(pA, 0, 0, it, ss), (pB, 1, 1, it, ss)]
                    elif m == 1:
                        plan += [(pB, 1, 0, it, ss), (pB, 0, 1, it, ss)]
                    elif m == 2:
                        plan += [(pB, 0, 0, it, ss), (pA, 1, 1, it, ss)]
                    else:
                        plan += [(pA, 1, 0, it, ss), (pA, 0, 1, it, ss)]
                la = [p for p in plan if p[0] == pA]; lb = [p for p in plan if p[0] == pB]
                plan = []
                for q in range(max(len(la), len(lb))):
                    if q < len(la):
                        plan.append(la[q])
                    if q < len(lb):
                        plan.append(lb[q])
                cnt = {pA: len(la), pB: len(lb)}
                seen = {pA: 0, pB: 0}
                for (bk, tr, qr, it, ss) in plan:
                    st_ = (seen[bk] == 0)
                    seen[bk] += 1
                    sp_ = (seen[bk] == cnt[bk])
                    K.op("pe", lambda e, bk=bk, tr=tr, qr=qr, it=it, ss=ss, st_=st_, sp_=sp_: e.matmul(
                        PS(bk), lhsT=tab[:, tr, ss, :], rhs=PQv[:, it, qr, :], start=st_, stop=sp_),
                        reads=[("tab", tr, ss // 8), ("PQ", it)], writes=[pk(bk)])
                cp("act", fmn[b2], PS(pB), [pk(pB)], [("fmn", b2)])
                K.op("dve", lambda e, b2=b2, pA=pA: e.tensor_tensor(out=fsb[b2], in0=PS(pA), in1=fmn[b2], op=ALU.subtract),
                     reads=[pk(pA), ("fmn", b2)], writes=[("fsb", b2)])
                if ot == 0 and "d_f" in dbg_out:
                    K.dma("sp", dbg_out["d_f"], fsb[0], reads=[("fsb", 0)], writes=[("dbg", 5)])
                K.op("act", lambda e, b2=b2, ot=ot: e.activation(out=fjunk, in_=fsb[b2], func=AF.Square, accum_out=fstat[:, ot, 0:1]),
                     reads=[("fsb", b2)], writes=["fjunk", ("fstat", ot)])
                K.op("act", lambda e, ot=ot: e.activation(out=fstat[:, ot, 1:2], in_=fstat[:, ot, 0:1], func=AF.Sqrt, scale=1.0 / 512.0, bias=small[:, 0:1]),
                     reads=[("fstat", ot), "small"], writes=[("fstat", ot)])
                K.op("dve", lambda e, ot=ot: e.reciprocal(out=fstat[:, ot, 2:3], in_=fstat[:, ot, 1:2]), reads=[("fstat", ot)], writes=[("fstat", ot)])
                K.op("dve", lambda e, b2=b2, ot=ot: e.tensor_scalar(out=fnb[b2], in0=fsb[b2], scalar1=fstat[:, ot, 2:3], scalar2=None, op0=ALU.mult),
                     reads=[("fsb", b2), ("fstat", ot)], writes=[("fnb", b2)])

            def dft_b(ot):
                b2 = ot % 2
                pt = 4 + b2
                for fb in range(4):
                    K.op("pe", lambda e, fb=fb, b2=b2, pt=pt: e.transpose(PSB(pt)[:, fb * 128:(fb + 1) * 128], fnb[b2][:, fb * 128:(fb + 1) * 128], ident_b),
                         reads=[("fnb", b2), "ident_b"], writes=[pk(pt)])
                K.op("dve", lambda e, b2=b2, pt=pt: e.tensor_copy(out=fnTt[b2].rearrange("p f t -> p (f t)"), in_=PSB(pt)[:, 0:512]),
                     reads=[pk(pt)], writes=[("fnTt", b2)])
                K.dma("act", fT_scr[ot], fnTt[b2], reads=[("fnTt", b2)], writes=[("fTscr", ot)])
            for ot in range(32):
                dft_a(ot)
                if ot > 0:
                    dft_b(ot - 1)
            dft_b(31)
            K.barrier()
            A.top = p2start + 32 * 576

            chk(3)
            GamRe = A.alloc([32, 128], BF16)
            GamIm = A.alloc([32, 128], BF16)
            Mi = A.alloc([32, 128], BF16)
            ovl = A.top
            PhiT = A.alloc([32, 2, 128], BF16)
            RS = 32
            ring = A.alloc([RS, 4, 32], F32)
            rtmp = A.alloc([4, 32], F32); rC = A.alloc([2, 32], F32)
            L4 = A.alloc([4, 32], F32)
            p4mark = A.top
            Bre = A.alloc([32, 16], F32); Bim = A.alloc([32, 16], F32)
            Crows = A.alloc([2, 4, 128], F32)
            Cre = A.alloc([32, 16], F32); Cim = A.alloc([32, 16], F32)
            PH1 = A.alloc([16, 8, 16], F32); PH2 = A.alloc([16, 8, 16], F32)
            PhRe = A.alloc([32, 128], BF16); PhIm = A.alloc([32, 128], BF16)
            assert A.top <= hoist_lo, (A.top, hoist_lo)
            MT1 = PH1.rearrange("p g s h -> p (g s h)")[:, 0:512].rearrange("p (g m) -> p g m", g=4)
            MT2 = PH2.rearrange("p g s h -> p (g s h)")[:, 0:512].rearrange("p (g m) -> p g m", g=4)

            with nc.allow_non_contiguous_dma(reason="s5 small param layouts"):
                for d in range(2):
                    K.dma("sp", Bre[64 * d:64 * d + 64], b_re[d].rearrange("g p h -> p g h"), writes=["Bre"])
                    K.dma("act", Bim[64 * d:64 * d + 64], b_im[d].rearrange("g p h -> p g h"), writes=["Bim"])
                    for gb in range(4):
                        K.dma("sp", Crows[:, 0, gb, 64 * d:64 * d + 64], c_re[d, gb * 8:(gb + 1) * 8].rearrange("gl h p -> (gl h) p"), writes=["Crows"])
                        K.dma("act", Crows[:, 1, gb, 64 * d:64 * d + 64], c_im[d, gb * 8:(gb + 1) * 8].rearrange("gl h p -> (gl h) p"), writes=["Crows"])
            for r, dst, key in ((0, Cre, "Cre"), (1, Cim, "Cim")):
                for gb in range(4):
                    K.op("pe", lambda e, r=r, gb=gb: e.transpose(PS(1, 128, gb * 128), Crows[:, r, gb, :], ident_f), reads=["Crows", "ident_f"], writes=[pk(1)])
                cp("dve", dst.rearrange("p g h -> p (g h)"), PS(1), [pk(1)], [key])

            chk(3.1)

            def outer(dst_re, dst_im, wr, wi, wkeys, Xre, Xim, xkeys, okey, neg_im=False):
                for gh in range(2):
                    gs = slice(gh * 16, gh * 16 + 16)
                    wrb = wr[:, gs, :].unsqueeze(3).to_broadcast([128, 16, 8, 16]); wib = wi[:, gs, :].unsqueeze(3).to_broadcast([128, 16, 8, 16])
                    xrb = Xre[:, gs, :].unsqueeze(2).to_broadcast([128, 16, 8, 16]); xib = Xim[:, gs, :].unsqueeze(2).to_broadcast([128, 16, 8, 16])
                    dr = dst_re[:, gs, :].rearrange("p g (s h) -> p g s h", s=8); di = dst_im[:, gs, :].rearrange("p g (s h) -> p g s h", s=8)
                    dve(lambda e, wrb=wrb, xrb=xrb: e.tensor_tensor(out=PH1, in0=wrb, in1=xrb, op=ALU.mult), wkeys + xkeys, ["PH1"])
                    dve(lambda e, wib=wib, xib=xib: e.tensor_tensor(out=PH2, in0=wib, in1=xib, op=ALU.mult), wkeys + xkeys, ["PH2"])
                    dve(lambda e, dr=dr: e.tensor_tensor(out=dr, in0=PH1, in1=PH2, op=ALU.subtract), ["PH1", "PH2"], [okey + "re"])
                    dve(lambda e, wrb=wrb, xib=xib: e.tensor_tensor(out=PH1, in0=wrb, in1=xib, op=ALU.mult), wkeys + xkeys + [okey + "re"], ["PH1"])
                    dve(lambda e, wib=wib, xrb=xrb: e.tensor_tensor(out=PH2, in0=wib, in1=xrb, op=ALU.mult), wkeys + xkeys + [okey + "re"], ["PH2"])
                    if neg_im:
                        dve(lambda e, di=di: e.scalar_tensor_tensor(out=di, in0=PH1, scalar=-1.0, in1=PH2, op0=ALU.mult, op1=ALU.subtract), ["PH1", "PH2"], [okey + "im"])
                    else:
                        dve(lambda e, di=di: e.tensor_tensor(out=di, in0=PH1, in1=PH2, op=ALU.add), ["PH1", "PH2"], [okey + "im"])
            chk(3.2)
            outer(PhRe, PhIm, W["w2re"], W["w2im"], ["w2re", "w2im"], Bre, Bim, ["Bre", "Bim"], "Ph")
            for g in range(32):
                pb = 2 + (g // 4) % 2
                for ri, src, sk in ((0, PhRe, "Phre"), (1, PhIm, "Phim")):
                    K.op("pe", lambda e, g=g, ri=ri, src=src, pb=pb: e.transpose(
                        PSB(pb)[:, ((g % 4) * 2 + ri) * 128:((g % 4) * 2 + ri + 1) * 128], src[:, g, :], ident_b),
                        reads=[sk, "ident_b"], writes=[pk(pb)])
                if g % 4 == 3:
                    g0 = g - 3
                    cp("act", PhiT[:, g0:g0 + 4].rearrange("p g r m -> p (g r m)"), PSB(pb), [pk(pb)], ["PhiT"])
            chk(3.3)
            outer(PhRe, PhIm, W["wre"], W["wim"], ["wre", "wim"], Bre, Bim, ["Bre", "Bim"], "Ph")
            outer(GamRe, GamIm, T["LKR"][:, :, 0:8], T["LKI"][:, :, 0:8], ["LKR", "LKI"], Cre, Cim, ["Cre", "Cim"], "Gam", neg_im=True)
            l8r = T["LKR"][:, :, 17]; l8i = T["LKI"][:, :, 17]
            dve(lambda e: e.tensor_copy(out=L4[:, 0:2, :], in_=l8r.unsqueeze(1).to_broadcast([128, 2, 32])), ["LKR"], ["L4"])
            dve(lambda e: e.tensor_copy(out=L4[:, 2, :], in_=l8i), ["LKI"], ["L4"])
            dve(lambda e: e.tensor_scalar(out=L4[:, 3, :], in0=l8i, scalar1=-1.0, scalar2=None, op0=ALU.mult), ["LKI"], ["L4"])
            mfb = masks[:, 0:128].unsqueeze(1).to_broadcast([128, 4, 128]); mrb = masks[:, 128:256].unsqueeze(1).to_broadcast([128, 4, 128])
            for g4 in range(8):
                pf_, pr_ = 4 + 2 * (g4 % 2), 5 + 2 * (g4 % 2)
                for gi in range(4):
                    g = g4 * 4 + gi
                    for d, pb in ((0, pf_), (1, pr_)):
                        K.op("pe", lambda e, g=g, gi=gi, d=d, pb=pb: e.matmul(
                            PS(pb, 128, gi * 128), lhsT=PhRe[64 * d:64 * d + 64, g, :], rhs=GamRe[64 * d:64 * d + 64, g, :], start=True, stop=False),
                            reads=["Phre", "Gamre"], writes=[pk(pb)])
                        K.op("pe", lambda e, g=g, gi=gi, d=d, pb=pb: e.matmul(
                            PS(pb, 128, gi * 128), lhsT=PhIm[64 * d:64 * d + 64, g, :], rhs=GamIm[64 * d:64 * d + 64, g, :], start=False, stop=True),
                            reads=["Phim", "Gamim"], writes=[pk(pb)])
                dve(lambda e, pf_=pf_: e.tensor_tensor(out=MT1, in0=PS(pf_).rearrange("p (g m) -> p g m", g=4), in1=mfb, op=ALU.mult), [pk(pf_), "masks"], ["PH1"])
                dve(lambda e, pr_=pr_: e.tensor_tensor(out=MT2, in0=PS(pr_).rearrange("p (g m) -> p g m", g=4), in1=mrb, op=ALU.mult), [pk(pr_), "masks"], ["PH2"])
                dve(lambda e: e.tensor_tensor(out=MT1, in0=MT1, in1=MT2, op=ALU.add), ["PH1", "PH2"], ["PH1"])
                for gi in range(4):
                    g = g4 * 4 + gi
                    dve(lambda e, g=g, gi=gi: e.scalar_tensor_tensor(out=Mi[:, g, :], in0=ident_f, scalar=Dcol[:, g:g + 1], in1=MT1[:, gi, :], op0=ALU.mult, op1=ALU.add),
                        ["PH1", "ident_f", "Dcol"], [("Mi", g)])
            if "d_Mi" in dbg_out:
                dM = A.alloc([128], F32)
                dve(lambda e: e.tensor_copy(out=dM, in_=Mi[:, 0, :]), [("Mi", 0)], ["dM"])
                K.dma("sp", dbg_out["d_Mi"], dM, reads=["dM"], writes=[("dbg", 20)])
            K.barrier()
            chk(3.4)
            A.top = p4mark
            XV = A.alloc([2, 32, NE], BF16)
            A.alloc([512], BF16)
            wob_lo = A.top
            w_out_bf = A.alloc([8, 1024], BF16)
            w_glu_bf = A.alloc([4, 512], BF16)
            w_out_r = w_out.rearrange("(kc p) n -> p kc n", p=128)
            for kc in range(8):
                K.dma("pool", w_out_bf[:, kc, :], w_out_r[:, kc, :], writes=["w_out_bf"])
            K.dma("pool", w_glu_bf, w_glu.rearrange("(kc p) n -> p kc n", p=128), writes=["w_glu_bf"])
            for kc in range(8):
                K.op("dve", lambda e, kc=kc: e.tensor_scalar(out=w_out_bf[:, kc, :], in0=w_out_bf[:, kc, :], scalar1=cols[:, 80 + kc:81 + kc], scalar2=None, op0=ALU.mult),
                     reads=["w_out_bf", "cols"], writes=["w_out_bf"])
            for g in range(32):
                for ri in range(2):
                    idx = g * 2 + ri
                    pa = idx % 2
                    pbk = 2 + (idx // 16) % 2
                    for d in range(2):
                        rA = U[:, g, 0:512] if d == 0 else U[:, g, 575:63:-1]
                        rB = U[:, g, 512:543] if d == 0 else U[:, g, 63:32:-1]
                        K.op("pe", lambda e, g=g, ri=ri, d=d, pa=pa, rA=rA: e.matmul(
                            PS(pa)[64 * d:64 * d + 64, :], lhsT=PhiT[:, g, ri, 64 * d:64 * d + 64], rhs=rA, start=True, stop=True),
                            reads=["PhiT", ("U", g // 8)], writes=[pk(pa)])
                        K.op("pe", lambda e, g=g, ri=ri, d=d, pbk=pbk, rB=rB, idx=idx: e.matmul(
                            PS(pbk, 31, (idx % 16) * 32)[64 * d:64 * d + 64, :], lhsT=PhiT[:, g, ri, 64 * d:64 * d + 64], rhs=rB, start=True, stop=True),
                            reads=["PhiT", ("U", g // 8)], writes=[pk(pbk)])
                    cp("act" if idx % 2 == 0 else "dve", XV[:, ri, g, 0:512], PS(pa), [pk(pa)], ["XVin"])
                    if idx % 16 == 15:
                        g0 = (idx - 15) // 2
                        cp("dve", XV[:, :, g0:g0 + 8, 512:543].rearrange("p r g c -> p g r c"),
                           PS(pbk).rearrange("p (g r c) -> p g r c", g=8, r=2)[:, :, :, 0:31], [pk(pbk)], ["XVin"])
            if "d_V" in dbg_out:
                dV = A.alloc([NE], F32)
                dve(lambda e: e.tensor_copy(out=dV, in_=XV[:, 0, 0, :]), ["XVin"], ["dV"])
                K.dma("sp", dbg_out["d_V"], dV, reads=["dV"], writes=[("dbg", 21)])
            chk(3.5)
            XVOUT = []
            dve(lambda e: e.memset(ring[:, 0, :, :], 0.0), [], [("ring", 0)])
            NSTEP = 543
            CV = 16
            rk = lambda slot: ("ring", (slot % RS) // CV)
            for e_ in range(NSTEP):
                cur = ring[:, e_ % RS, :, :]
                nxt = ring[:, (e_ + 1) % RS, :, :].rearrange("p (c r) g -> p c r g", c=2)
                dve(lambda e, cur=cur: e.tensor_tensor(out=rtmp, in0=cur, in1=L4, op=ALU.mult), [rk(e_), "L4"], ["rtmp"])
                dve(lambda e: e.tensor_tensor(out=rC, in0=rtmp[:, 0:2, :], in1=rtmp[:, 3:1:-1, :], op=ALU.add), ["rtmp"], ["rC"])
                dve(lambda e, nxt=nxt, e_=e_: e.tensor_tensor(out=nxt, in0=rC.unsqueeze(1).to_broadcast([128, 2, 2, 32]),
                                                              in1=XV[:, :, :, e_].unsqueeze(1).to_broadcast([128, 2, 2, 32]), op=ALU.add), ["rC", "XVin"], [rk(e_ + 1)])
                if (e_ + 1) % CV == 0 or e_ == NSTEP - 1:
                    n = CV if (e_ + 1) % CV == 0 else (NSTEP % CV)
                    e0 = e_ + 1 - n
                    k = 0
                    while k < n:
                        rs = (e0 + 1 + k) % RS
                        run = min(n - k, RS - rs)
                        cp("act", XV[:, :, :, e0 + k:e0 + k + run], ring[:, rs:rs + run, 0:2, :].rearrange("p c r g -> p r g c"),
                           sorted(set([rk(rs + q) for q in range(run)])), [("XVout", e0 // CV, k)])
                        XVOUT.append(("XVout", e0 // CV, k))
                        k += run
            if "d_X" in dbg_out:
                dX = A.alloc([NE], F32)
                dve(lambda e: e.tensor_copy(out=dX, in_=XV[:, 0, 0, :]), XVOUT + ["XVin"], ["dX"])
                K.dma("sp", dbg_out["d_X"], dX, reads=["dX"], writes=[("dbg", 22)])
            K.barrier()
            chk(3.6)
            A2 = Arena(arena_t); A2.top = ovl
            Ylj = A2.alloc([32, 128], F32)
            Ytj = A2.alloc([8, 512], F32)
            assert A2.top <= p4mark
            y5_r = y5_scr.rearrange("(c s) f -> c s f", s=8)
            for j in range(4):
                for g in range(32):
                    pA = (g % 2) * 3
                    K.op("pe", lambda e, g=g, pA=pA, j=j: e.matmul(PS(pA, 128), lhsT=Mi[:, g, :], rhs=U[:, g, 32 + 128 * j:32 + 128 * (j + 1)], start=True, stop=True),
                         reads=[("Mi", g), ("U", g // 8)], writes=[pk(pA)])
                    for d in range(2):
                        pD = pA + 1 + d
                        for ri, Gm in ((0, GamRe), (1, GamIm)):
                            if d == 0:
                                rhs = XV[0:64, ri, g, 31 + 128 * j:31 + 128 * (j + 1)]
                            else:
                                hi = 542 - 128 * j
                                lo = hi - 128
                                rhs = XV[64:128, ri, g, hi:lo:-1]
                            K.op("pe", lambda e, g=g, pD=pD, d=d, Gm=Gm, rhs=rhs, ri=ri: e.matmul(
                                PS(pD, 128), lhsT=Gm[64 * d:64 * d + 64, g, :], rhs=rhs, start=(ri == 0), stop=(ri == 1)),
                                reads=["Gamre", "Gamim", "XVin"] + XVOUT, writes=[pk(pD)])
                    cp("act", Ylj[:, g, :], PS(pA, 128), [pk(pA)], [("Ylj", g)])
                    K.op("dve", lambda e, g=g, pA=pA: e.tensor_tensor(out=Ylj[:, g, :], in0=Ylj[:, g, :], in1=PS(pA + 1, 128), op=ALU.add),
                         reads=[("Ylj", g), pk(pA + 1)], writes=[("Ylj", g)])
                    K.op("dve", lambda e, g=g, pA=pA: e.tensor_tensor(out=Ylj[:, g, :], in0=Ylj[:, g, :], in1=PS(pA + 2, 128), op=ALU.add),
                         reads=[("Ylj", g), pk(pA + 2)], writes=[("Ylj", g)])
                if j == 0 and "d_Y" in dbg_out:
                    K.dma("sp", dbg_out["d_Y"], Ylj[:, 0, :], reads=[("Ylj", 0)], writes=[("dbg", 7)])
                for g4 in range(8):
                    pb = 6 + g4 % 2
                    for gi in range(4):
                        g = g4 * 4 + gi
                        K.op("pe", lambda e, g=g, gi=gi, pb=pb: e.transpose(PS(pb, 128, gi * 128), Ylj[:, g, :], ident_f),
                             reads=[("Ylj", g), "ident_f"], writes=[pk(pb)])
                    cp("act" if g4 % 2 == 0 else "dve", Ytj[:, :, g4 * 64:(g4 + 1) * 64].rearrange("p s (g h) -> p g s h", g=4),
                       PS(pb).rearrange("p (g s h) -> p g s h", g=4, s=8), [pk(pb)], ["Ytj"])
                K.dma("sp", y5_r[128 * j:128 * (j + 1), :, :], Ytj, reads=["Ytj"], writes=[("y5", j)])
            K.barrier()

            chk(4)
            A.top = p2start
            G = A.alloc([32, 32], F32)
            pjunk = A.alloc([1024], BF16)
            p5keep = A.top
            pjunk2 = A.alloc([1024], BF16)
            lgall = A.alloc([32, 36], F32)
            p5tmp = A.top
            D2 = lambda shape, dt: [A.alloc(shape, dt) for _ in range(4)]
            D4 = lambda shape, dt: [A.alloc(shape, dt) for _ in range(8)]
            Yt = D4([512], F32); fnTt5 = D4([4, 128], BF16)
            ygf = D2([512], F32); ygb = D2([512], BF16); ygT = D2([4, 128], BF16)
            sg = D2([512], F32); y2 = D2([512], F32); y2n = D2([512], BF16); y2nT = D2([4, 128], BF16)
            xr = D4([1024], F32); x1 = D2([1024], F32)
            xs2 = D2([1024], BF16); tT = D2([8, 128], BF16); tTf = D2([8, 128], F32)
            pstat = A.alloc([32, 8], F32)
            y_r = y.rearrange("(c s) d -> s c d", s=8)
            x1_r = x1_scr.rearrange("(c s) d -> s c d", s=8)
            y5_sr = y5_scr.rearrange("(c s) f -> s c f", s=8)
            pj = [pjunk, pjunk2, A.alloc([1024], BF16), A.alloc([1024], BF16)]
            assert A.top <= wob_lo, (A.top, wob_lo)

            def p5_loads(t_):
                j_, s_, b_ = t_ // 8, t_ % 8, t_ % 8
                K.dma("sp", Yt[b_], y5_sr[s_, j_ * 128:(j_ + 1) * 128, :], reads=[("y5", j_)], writes=[("Yt", b_)])
                K.dma("sp", fnTt5[b_], fT_scr[t_], reads=[("fTscr", t_)], writes=[("fnTt5", b_)])
                K.dma("sp", xr[b_], x_r[s_, j_ * 128:(j_ + 1) * 128, :], writes=[("xr", b_)])
            def p5_tile(tl):
                j, s = tl // 8, tl % 8
                tl = j * 8 + s
                b2 = tl % 4
                bT, bG, bO = 2 * b2, 2 * b2 + 1, 2 * b2 + 1
                b4 = tl % 8
                b3 = tl % 4
                K.op("act", lambda e, b2=b2, b4=b4: e.activation(out=ygf[b2], in_=Yt[b4], func=AF.Gelu_apprx_tanh), reads=[("Yt", b4)], writes=[("ygf", b2)])
                K.op("pool", lambda e, b2=b2: e.tensor_copy(out=ygb[b2], in_=ygf[b2]), reads=[("ygf", b2)], writes=[("ygb", b2)])
                yield
                for fb in range(4):
                    K.op("pe", lambda e, fb=fb, b2=b2, bT=bT: e.transpose(PSB(bT)[:, fb * 128:(fb + 1) * 128], ygb[b2][:, fb * 128:(fb + 1) * 128], ident_b),
                         reads=[("ygb", b2), "ident_b"], writes=[pk(bT)])
                cp("dve", ygT[b2].rearrange("p f t -> p (f t)"), PSB(bT)[:, 0:512], [pk(bT)], [("ygT", b2)])
                yield
                for fb in range(4):
                    K.op("pe", lambda e, fb=fb, b2=b2, bG=bG: e.matmul(PS(bG), lhsT=ygT[b2][:, fb, :], rhs=w_glu_bf[:, fb, :], start=(fb == 0), stop=(fb == 3)),
                         reads=[("ygT", b2), "w_glu_bf"], writes=[pk(bG)])
                K.op("act", lambda e, b2=b2, bG=bG: e.activation(out=sg[b2], in_=PS(bG), func=AF.Sigmoid), reads=[pk(bG)], writes=[("sg", b2)])
                K.op("dve", lambda e, b2=b2: e.tensor_tensor(out=y2[b2], in0=ygf[b2], in1=sg[b2], op=ALU.mult), reads=[("ygf", b2), ("sg", b2)], writes=[("y2", b2)])
                yield
                rms_prep(100 + tl, ("y2", b2), y2[b2], 128, 512.0, pstat[:, tl, 0:4], pj[b2][:, 0:512], y2n[b2], ("y2n", b2), junk_key=("pj", b2))
                yield
                for fb in range(4):
                    K.op("pe", lambda e, fb=fb, b2=b2, bT=bT: e.transpose(PSB(bT)[:, 512 + fb * 128:512 + (fb + 1) * 128], y2n[b2][:, fb * 128:(fb + 1) * 128], ident_b),
                         reads=[("y2n", b2), "ident_b"], writes=[pk(bT)])
                cp("dve", y2nT[b2].rearrange("p f t -> p (f t)"), PSB(bT)[:, 512:1024], [pk(bT)], [("y2nT", b2)])
                yield
                for hf in range(2):
                    pb = bO
                    for kb in range(8):
                        lhsT = y2nT[b2][:, kb, :] if kb < 4 else fnTt5[b4][:, kb - 4, :]
                        K.op("pe", lambda e, hf=hf, kb=kb, pb=pb, lhsT=lhsT: e.matmul(
                            PS(pb), lhsT=lhsT, rhs=w_out_bf[:, kb, hf * 512:(hf + 1) * 512], start=(kb == 0), stop=(kb == 7)),
                            reads=[("y2nT", b2), ("fnTt5", b4), "w_out_bf"], writes=[pk(pb)])
                    K.op("dve", lambda e, hf=hf, pb=pb, b2=b2: e.tensor_tensor(out=x1[b2][:, hf * 512:(hf + 1) * 512], in0=PS(pb), in1=g1_bc[:, hf * 512:(hf + 1) * 512], op=ALU.mult),
                         reads=[pk(pb), "g1_bc"], writes=[("x1", b2)])
                yield
                K.op("pool", lambda e, b2=b2, b4=b4: e.tensor_tensor(out=x1[b2], in0=x1[b2], in1=xr[b4], op=ALU.add), reads=[("x1", b2), ("xr", b4)], writes=[("x1", b2)])
                K.dma("sp", x1_r[s, j * 128:(j + 1) * 128, :], x1[b2], reads=[("x1", b2)], writes=[("x1scr", tl)])
                if tl == 0 and "d_x1" in dbg_out:
                    K.dma("sp", dbg_out["d_x1"], x1[0], reads=[("x1", 0)], writes=[("dbg", 8)])
                yield
                rms_prep(200 + tl, ("x1", b2), x1[b2], 128, 1024.0, pstat[:, tl, 4:8], pj[b2], xs2[b2], ("xs2", b2), junk_key=("pj", b2))
                yield
                for kc in range(8):
                    K.op("pe", lambda e, kc=kc, b2=b2, bT=bT: e.transpose(PSB(bT)[:, kc * 128:(kc + 1) * 128], xs2[b2][:, kc * 128:(kc + 1) * 128], ident_b),
                         reads=[("xs2", b2), "ident_b"], writes=[pk(bT)])
                psv = PSB(bT).rearrange("p (k t) -> p k t", k=8)
                K.op("dve", lambda e, psv=psv, b3=b3: e.tensor_tensor(out=tTf[b3], in0=psv, in1=vecs[:, 4, :].unsqueeze(2).to_broadcast([128, 8, 128]), op=ALU.mult),
                     reads=[pk(bT), "vecs"], writes=[("tTf", b3)])
                yield
                K.op("pool", lambda e, b3=b3: e.tensor_tensor(out=tTf[b3], in0=tTf[b3], in1=vecs[:, 5, :].unsqueeze(2).to_broadcast([128, 8, 128]), op=ALU.add),
                     reads=[("tTf", b3), "vecs"], writes=[("tTf", b3)])
                cp("act", tT[b2], tTf[b3], [("tTf", b3)], [("tT", b2)])
                K.dma("sp", tT_scr[:, :, tl * 128:(tl + 1) * 128].rearrange("k p t -> p k t"), tT[b2], reads=[("tT", b2)], writes=[("tTscr", tl)])
                yield
                for kc in range(8):
                    K.op("pe", lambda e, kc=kc, b3=b3, bG=bG: e.matmul(PS(bG, 36), lhsT=tTf[b3][:, kc, :], rhs=wrt[:, kc, :], start=(kc == 0), stop=(kc == 7)),
                         reads=[("tTf", b3), "wrt"], writes=[pk(bG)])
                K.op("dve", lambda e, tl=tl, bG=bG: e.tensor_tensor(out=lgall[:, tl, :], in0=PS(bG, 36), in1=brt_bc, op=ALU.add), reads=[pk(bG), "brt_bc"], writes=["lgall"])

            for t_ in range(4):
                p5_loads(t_)
            for pr in range(8):
                if pr + 1 < 8:
                    for t_ in range(4):
                        p5_loads(4 * pr + 4 + t_)
                gens = [p5_tile(4 * pr + q) for q in range(4)]
                alive = [True] * 4
                while any(alive):
                    for gi_, g_ in enumerate(gens):
                        if alive[gi_]:
                            try:
                                next(g_)
                            except StopIteration:
                                alive[gi_] = False
            K.barrier()
            A.top = p5tmp
            NTL = 32
            ra = lambda n: A.alloc([NTL, n], F32)
            gmax = ra(1); sel = ra(4); exg = ra(4); gsum = ra(1); gw = ra(1); t8 = ra(32); es_ = ra(8); m1 = ra(1); mk1 = ra(8)
            es2 = ra(8); m2 = ra(1); mk2 = ra(8); dd = ra(1); exd = ra(1); den = ra(1); w1 = ra(1); w2 = ra(1); comb = ra(8); cb2 = ra(8)
            gl3 = lgall[:, :, 0:4]; el4 = lgall[:, :, 4:36].rearrange("p t (g e) -> p t g e", g=4)

            def R(fn, eng="dve"):
                K.op(eng, fn, reads=["rt", "lgall"], writes=["rt"])
            bc4 = lambda a: a[:, :, 0:1].to_broadcast([128, NTL, 4])
            bc8 = lambda a: a[:, :, 0:1].to_broadcast([128, NTL, 8])
            R(lambda e: e.tensor_reduce(out=gmax[:, :, 0], in_=gl3, axis=AX.X, op=ALU.max))
            R(lambda e: e.tensor_tensor(out=sel, in0=gl3, in1=bc4(gmax), op=ALU.is_equal))
            R(lambda e: e.tensor_tensor(out=exg, in0=gl3, in1=bc4(gmax), op=ALU.subtract))
            R(lambda e: e.activation(out=exg, in_=exg, func=AF.Exp), eng="act")
            R(lambda e: e.tensor_reduce(out=gsum[:, :, 0], in_=exg, axis=AX.X, op=ALU.add))
            R(lambda e: e.reciprocal(out=gw, in_=gsum))
            R(lambda e: e.tensor_tensor(out=t8.rearrange("p t (g e) -> p t g e", g=4), in0=el4, in1=sel.unsqueeze(3).to_broadcast([128, NTL, 4, 8]), op=ALU.mult))
            R(lambda e: e.tensor_reduce(out=es_, in_=t8.rearrange("p t (g e) -> p t e g", g=4), axis=AX.X, op=ALU.add))
            R(lambda e: e.tensor_reduce(out=m1[:, :, 0], in_=es_, axis=AX.X, op=ALU.max))
            R(lambda e: e.tensor_tensor(out=mk1, in0=es_, in1=bc8(m1), op=ALU.is_equal))
            R(lambda e: e.scalar_tensor_tensor(out=es2, in0=mk1, scalar=-1e30, in1=es_, op0=ALU.mult, op1=ALU.add))
            R(lambda e: e.tensor_reduce(out=m2[:, :, 0], in_=es2, axis=AX.X, op=ALU.max))
            R(lambda e: e.tensor_tensor(out=mk2, in0=es2, in1=bc8(m2), op=ALU.is_equal))
            R(lambda e: e.tensor_tensor(out=dd, in0=m2, in1=m1, op=ALU.subtract))
            R(lambda e: e.activation(out=exd, in_=dd, func=AF.Exp), eng="act")
            R(lambda e: e.tensor_scalar(out=den, in0=exd, scalar1=1.0, scalar2=None, op0=ALU.add))
            R(lambda e: e.reciprocal(out=w1, in_=den))
            R(lambda e: e.tensor_tensor(out=w1, in0=w1, in1=gw, op=ALU.mult))
            R(lambda e: e.tensor_tensor(out=w2, in0=w1, in1=exd, op=ALU.mult))
            R(lambda e: e.tensor_tensor(out=comb, in0=mk1, in1=bc8(w1), op=ALU.mult))
            R(lambda e: e.tensor_tensor(out=cb2, in0=mk2, in1=bc8(w2), op=ALU.mult))
            R(lambda e: e.tensor_tensor(out=comb, in0=comb, in1=cb2, op=ALU.add))
            K.op("dve", lambda e: e.tensor_tensor(out=G.rearrange("p t (g e) -> p t g e", g=4),
                                                  in0=sel.unsqueeze(3).to_broadcast([128, NTL, 4, 8]),
                                                  in1=comb.unsqueeze(2).to_broadcast([128, NTL, 4, 8]), op=ALU.mult),
                 reads=["rt"], writes=[("G", tl_) for tl_ in range(32)])
            if "d_G" in dbg_out:
                K.dma("sp", dbg_out["d_G"], G[:, 0, :], reads=[("G", 0)], writes=[("dbg", 9)])
            K.barrier()

            chk(5)
            A.top = p5keep
            tTh2 = [A.alloc([8, 2048], BF16) for _ in range(2)]
            acc = A.alloc([16, 1024], F32)
            wg = [A.alloc([8, 256], BF16) for _ in range(2)]
            wu = [A.alloc([8, 256], BF16) for _ in range(2)]
            wd = [A.alloc([2, 1024], BF16) for _ in range(2)]
            sl = [A.alloc([512], F32) for _ in range(2)]
            aT = [A.alloc([2, 512], BF16) for _ in range(2)]
            ob = [A.alloc([1024], F32) for _ in range(2)]
            ostat = A.alloc([32, 4], F32)
            ojunk = pjunk
            g2b = A.alloc([1024], BF16)
            K.op("dve", lambda e: e.tensor_copy(out=g2b, in_=g2_bc), reads=["g2_bc"], writes=["g2b"])
            def acc_load(hh, tt):
                tl = hh * 16 + tt
                j, s = tl // 8, tl % 8
                K.dma("act", acc[:, tt, :], x1_r[s, j * 128:(j + 1) * 128, :], reads=[("x1scr", tl)], writes=[("acc", tt)])

            for hh in range(2):
                for tg_ in range(4):
                    c0_ = hh * 2048 + tg_ * 512
                    K.dma("sp", tTh2[hh][:, :, tg_ * 512:(tg_ + 1) * 512], tT_scr[:, :, c0_:c0_ + 512].rearrange("k p t -> p k t"),
                          reads=[("tTscr", tl) for tl in range(hh * 16 + tg_ * 4, hh * 16 + tg_ * 4 + 4)], writes=[("tTh", hh, tg_)])
            for tt in range(16):
                acc_load(0, tt)

            def wload(ex_, wi):
                b2 = wi % 2
                K.dma("pool", wg[b2], w_gate[ex_].rearrange("(kc p) f -> p kc f", p=128), writes=[("wg", b2)])
                K.dma("pool", wu[b2], w_up[ex_].rearrange("(kc p) f -> p kc f", p=128), writes=[("wu", b2)])
                K.dma("pool", wd[b2], w_down[ex_].rearrange("(fb p) n -> p fb n", p=128), writes=[("wd", b2)])
                K.op("pool", lambda e, b2=b2: e.tensor_tensor(out=wd[b2], in0=wd[b2], in1=g2b.unsqueeze(1).to_broadcast([128, 2, 1024]), op=ALU.mult),
                     reads=[("wd", b2), "g2b"], writes=[("wd", b2)])

            def hgu(hh, ex_, tg, it_):
                b2 = (hh * 32 + ex_) % 2
                ab = it_ % 2
                tTh = tTh2[hh]
                for fb in range(2):
                    pg, pu_ = 0 + fb, 2 + fb
                    for kc in range(8):
                        K.op("pe", lambda e, kc=kc, fb=fb, b2=b2, tg=tg, pg=pg, tTh=tTh: e.matmul(
                            PS(pg), lhsT=wg[b2][:, kc, fb * 128:(fb + 1) * 128], rhs=tTh[:, kc, tg * 512:(tg + 1) * 512], start=(kc == 0), stop=(kc == 7)),
                            reads=[("wg", b2), ("tTh", hh, tg)], writes=[pk(pg)])
                    for kc in range(8):
                        K.op("pe", lambda e, kc=kc, fb=fb, b2=b2, tg=tg, pu_=pu_, tTh=tTh: e.matmul(
                            PS(pu_), lhsT=wu[b2][:, kc, fb * 128:(fb + 1) * 128], rhs=tTh[:, kc, tg * 512:(tg + 1) * 512], start=(kc == 0), stop=(kc == 7)),
                            reads=[("wu", b2), ("tTh", hh, tg)], writes=[pk(pu_)])
                    K.op("act", lambda e, fb=fb, pg=pg: e.activation(out=sl[fb], in_=PS(pg), func=AF.Silu), reads=[pk(pg)], writes=[("sl", fb)])
                    K.op("dve", lambda e, fb=fb, pu_=pu_, ab=ab: e.tensor_tensor(out=aT[ab][:, fb, :], in0=sl[fb], in1=PS(pu_), op=ALU.mult),
                         reads=[("sl", fb), pk(pu_)], writes=[("aT", ab, fb)])

            def down(hh, ex_, tg, it_):
                b2 = (hh * 32 + ex_) % 2
                ab = it_ % 2
                for t4 in range(4):
                    tt = tg * 4 + t4
                    tl = hh * 16 + tt
                    for hf in range(2):
                        po = 4 + (t4 * 2 + hf) % 4
                        for fb in range(2):
                            K.op("pe", lambda e, fb=fb, hf=hf, po=po, ab=ab, t4=t4, b2=b2: e.matmul(
                                PS(po), lhsT=aT[ab][:, fb, t4 * 128:(t4 + 1) * 128], rhs=wd[b2][:, fb, hf * 512:(hf + 1) * 512], start=(fb == 0), stop=(fb == 1)),
                                reads=[("aT", ab, 0), ("aT", ab, 1), ("wd", b2)], writes=[pk(po)])
                        K.op("dve", lambda e, po=po, tt=tt, hf=hf, tl=tl, ex_=ex_: e.scalar_tensor_tensor(
                            out=acc[:, tt, hf * 512:(hf + 1) * 512], in0=PS(po), scalar=G[:, tl, ex_:ex_ + 1], in1=acc[:, tt, hf * 512:(hf + 1) * 512],
                            op0=ALU.mult, op1=ALU.add), reads=[pk(po), ("G", tl), ("acc", tt)], writes=[("acc", tt)])

            def final_norm(hh, tt):
                tl = hh * 16 + tt
                j, s = tl // 8, tl % 8
                b2 = tt % 2
                K.op("act", lambda e, tt=tt, tl=tl: e.activation(out=ojunk, in_=acc[:, tt, :], func=AF.Square, accum_out=ostat[:, tl, 0:1]),
                     reads=[("acc", tt)], writes=["ojunk", ("ostat", tl)])
                K.op("act", lambda e, tl=tl: e.activation(out=ostat[:, tl, 1:2], in_=ostat[:, tl, 0:1], func=AF.Sqrt, scale=1.0 / 1024.0, bias=small[:, 0:1]),
                     reads=[("ostat", tl), "small"], writes=[("ostat", tl)])
                K.op("dve", lambda e, tl=tl: e.reciprocal(out=ostat[:, tl, 2:3], in_=ostat[:, tl, 1:2]), reads=[("ostat", tl)], writes=[("ostat", tl)])
                K.op("dve", lambda e, tt=tt, tl=tl, b2=b2: e.scalar_tensor_tensor(out=ob[b2], in0=acc[:, tt, :], scalar=ostat[:, tl, 2:3], in1=fin_bc, op0=ALU.mult, op1=ALU.mult),
                     reads=[("acc", tt), ("ostat", tl), "fin_bc"], writes=[("ob", b2)])
                K.dma("sp", y_r[s, j * 128:(j + 1) * 128, :], ob[b2], reads=[("ob", b2)], writes=[("y", tl)])

            items = [(hh, ex_, tg) for hh in range(2) for ex_ in range(32) for tg in range(4)]
            wload(0, 0)
            hgu(0, 0, 0, 0)
            for it_ in range(len(items)):
                hh, ex_, tg = items[it_]
                if it_ + 1 < len(items):
                    nh, ne, ntg = items[it_ + 1]
                    if ntg == 0:
                        wload(ne, nh * 32 + ne)
                    hgu(nh, ne, ntg, it_ + 1)
                down(hh, ex_, tg, it_)
                if ex_ == 31:
                    for t4 in range(4):
                        final_norm(hh, tg * 4 + t4)
                        if hh == 0:
                            acc_load(1, tg * 4 + t4)
        except _Stop:
            pass
        K.finish()

        @block.tensor
        def _(e):
            for f in K.eng["pe"]["prog"]:
                f(e)

        @block.scalar
        def _(e):
            for f in K.eng["act"]["prog"]:
                f(e)

        @block.vector
        def _(e):
            for f in K.eng["dve"]["prog"]:
                f(e)

        @block.gpsimd
        def _(e):
            for f in K.eng["pool"]["prog"]:
                f(e)

        @block.sync
        def _(e):
            for f in K.eng["sp"]["prog"]:
                f(e)
    return nc


_CONST = None


def _consts():
    global _CONST
    if _CONST is not None:
        return _CONST
    c = {}
    c["c_ident"] = np.eye(128, dtype=np.float32)
    s_in = np.arange(128) // 16
    mf = (s_in[:, None] <= s_in[None, :]).astype(np.float32)
    mr = (s_in[:, None] >= s_in[None, :]).astype(np.float32)
    c["c_masks"] = np.concatenate([mf, mr], axis=1)
    kid = np.zeros((128, 18), np.float32)
    s = np.arange(8)
    kid[0:64, 0:8] = s + 1
    kid[64:128, 0:8] = 8 - s
    kid[:, 8:16] = 8 - kid[:, 0:8]
    kid[:, 16] = 1
    kid[:, 17] = 8
    c["c_kidx"] = kid
    rep = np.zeros((16, 128), np.float32)
    for p in range(128):
        rep[p % 16, p] = 1.0
    c["c_rep16"] = rep
    jj = np.arange(128)
    ang = 2 * np.pi * np.outer(jj, jj) / 128.0
    sc = 1.0 / math.sqrt(L * 128.0)
    c["c_dftc"] = np.concatenate([np.cos(ang) * sc, np.sin(ang) * sc], axis=1).astype(np.float32)
    ii = np.arange(128)
    tab = np.zeros((128, 2, 64, 128), dtype=ml_dtypes.bfloat16)
    for s_ in range(8):
        for s2 in range(8):
            mm = ((8 * ii[:, None] + s_) * (8 * ii[None, :] + s2)) % L
            a = 2 * np.pi * mm / L
            tab[:, 0, s2 * 8 + s_, :] = np.cos(a).astype(ml_dtypes.bfloat16)
            tab[:, 1, s2 * 8 + s_, :] = np.sin(a).astype(ml_dtypes.bfloat16)
    c["c_tab"] = tab
    _CONST = c
    return c


_NC_CACHE = {}


def _get_nc(dbg=()):
    key = tuple(dbg)
    if key not in _NC_CACHE:
        _NC_CACHE[key] = build_nc(dbg)
    return _NC_CACHE[key]


def make_in_maps(inputs, cores):
    c = _consts()
    f = lambda a: np.ascontiguousarray(np.asarray(a, dtype=np.float32))
    shared = {
        "w_ada": f(inputs["w_ada"][0]), "b_ada": f(inputs["b_ada"][0]), "w_in": f(inputs["w_in"][0]),
        "lam_re": f(inputs["s5_lam_re"][0]), "lam_im": f(inputs["s5_lam_im"][0]),
        "b_re": f(inputs["s5_b_re"][0]), "b_im": f(inputs["s5_b_im"][0]),
        "c_re": f(inputs["s5_c_re"][0]), "c_im": f(inputs["s5_c_im"][0]),
        "log_step": f(inputs["s5_log_step"][0]), "s5_d": f(inputs["s5_d"][0]),
        "w_glu": f(inputs["s5_w_glu"][0]), "four_w": f(inputs["fourier_w"][0]), "w_out": f(inputs["w_out"][0]),
        "w_rt": f(np.concatenate([np.asarray(inputs["moe_w_group"][0]), np.asarray(inputs["moe_w_router"][0]).reshape(D, 32)], axis=1)),
        "b_rt": f(np.concatenate([np.asarray(inputs["moe_b_group"][0]), np.asarray(inputs["moe_b_router"][0]).reshape(32)])),
        "w_gate": f(inputs["moe_w_gate"][0]), "w_up": f(inputs["moe_w_up"][0]), "w_down": f(inputs["moe_w_down"][0]),
        "fin_g": f(inputs["final_norm_g"]),
    }
    shared.update(c)
    maps = []
    for b in cores:
        rows = np.zeros((128, 128), np.float32)
        rows[0:8] = np.asarray(inputs["c"][b]).reshape(8, 128)
        rows[8:16] = np.asarray(inputs["c_ctx"]).reshape(8, 128)
        rows[16:24] = np.asarray(inputs["norm1_g"][0]).reshape(8, 128)
        rows[24:32] = np.asarray(inputs["norm2_g"][0]).reshape(8, 128)
        rows[32:80] = np.asarray(inputs["b_ada"][0]).reshape(48, 128)
        rows[80:84] = np.asarray(inputs["mix_norm_s5_g"][0]).reshape(4, 128)
        rows[84:88] = np.asarray(inputs["mix_norm_f_g"][0]).reshape(4, 128)
        m = dict(shared)
        m["x"] = f(inputs["x"][b])
        m["ctx"] = f(inputs["ctx"][b])
        m["rows_in"] = rows
        maps.append(m)
    return maps


def kernel(**inputs):
    nc = _get_nc()
    maps = make_in_maps(inputs, list(range(8)))
    res = run_bass_kernel_spmd(nc, maps, core_ids=list(range(8)))
    return np.stack([np.asarray(r["y"], dtype=np.float32) for r in res.results], axis=0)
```
